# Optimizing a Trainium2 kernel written in Bass

```python
import jax, jax.numpy as jnp
from jax import lax
import numpy as np

D_MODEL = 1024
BATCH = 8
SEQ = 4096
DEPTH = 2

CTX_LEN = 256
GRID_W = 64
MIX_WIDTH = D_MODEL
ATTN_WIDTH = MIX_WIDTH // 2
CONV_WIDTH = MIX_WIDTH - ATTN_WIDTH
HEAD_DIM = 64
N_HEADS = ATTN_WIDTH // HEAD_DIM
NA_KH = 8
NA_KW = 16
CONV_K = 3
FF_DIM = 3584
N_EXPERTS = 8
TOP_K = 2
EPS = 1e-6
N_DENSE = (DEPTH + 1) // 2
N_MOE = DEPTH // 2

Q_OFF = 0
K_OFF = ATTN_WIDTH
V_OFF = 2 * ATTN_WIDTH
B_OFF = 3 * ATTN_WIDTH
C_OFF = B_OFF + CONV_WIDTH
U_OFF = C_OFF + CONV_WIDTH
IN_WIDTH = U_OFF + CONV_WIDTH

kernel_name = 'hybrid_na_shortconv_moe_dit'


def rms_norm(x, g):
    xf = x.astype(jnp.float32)
    y = xf * lax.rsqrt(jnp.mean(xf * xf, axis=-1, keepdims=True) + EPS)
    return (y * g.astype(jnp.float32)).astype(x.dtype)


def adaln(c_vec, w, b):
    m = jax.nn.silu(c_vec) @ w + b
    return jnp.split(m, 6, axis=-1)


def heads(t):
    return t.reshape(t.shape[0], t.shape[1], N_HEADS, HEAD_DIM)


def short_gated_conv(p, w):
    bg = p[..., B_OFF:C_OFF]
    cg = p[..., C_OFF:U_OFF]
    u = p[..., U_OFF:IN_WIDTH]
    v = cg * u
    L = v.shape[1]
    pad = CONV_K // 2
    vp = jnp.pad(v, ((0, 0), (pad, CONV_K - 1 - pad), (0, 0)))
    conv = sum(w[i] * vp[:, i:i + L] for i in range(CONV_K))
    return bg * conv


def ctx_attention(q, k, v):
    s = jnp.einsum('bqhd,bkhd->bhqk', q, k).astype(jnp.float32) * (HEAD_DIM ** -0.5)
    p = jax.nn.softmax(s, axis=-1).astype(v.dtype)
    o = jnp.einsum('bhqk,bkhd->bqhd', p, v)
    return o.reshape(o.shape[0], o.shape[1], ATTN_WIDTH)


def neighbourhood_attention(q, k, v, k_c, v_c, rpb):
    b, s = q.shape[0], q.shape[1]
    rows = s // GRID_W
    kh = min(NA_KH, rows)
    scale = HEAD_DIM ** -0.5
    qg = q.reshape(b, rows, GRID_W, N_HEADS, HEAD_DIM)
    kg = k.reshape(b, rows, GRID_W, N_HEADS, HEAD_DIM)
    vg = v.reshape(b, rows, GRID_W, N_HEADS, HEAD_DIM)
    cols = jnp.arange(GRID_W)
    col_start = jnp.clip(cols - NA_KW // 2, 0, GRID_W - NA_KW)
    col_idx = col_start[:, None] + jnp.arange(NA_KW)[None, :]
    dx_idx = col_idx - cols[:, None] + (NA_KW - 1)
    n_loc = kh * NA_KW

    def row_block(r):
        rs = jnp.clip(r - kh // 2, 0, rows - kh)
        q_r = lax.dynamic_index_in_dim(qg, r, axis=1, keepdims=False)
        k_band = lax.dynamic_slice_in_dim(kg, rs, kh, axis=1)
        v_band = lax.dynamic_slice_in_dim(vg, rs, kh, axis=1)
        k_win = k_band[:, :, col_idx]
        v_win = v_band[:, :, col_idx]
        dy_idx = rs + jnp.arange(kh) - r + (NA_KH - 1)
        bias = rpb[:, dy_idx[None, :, None], dx_idx[:, None, :]]
        s_loc = jnp.einsum('bwhd,biwjhd->bhwij', q_r, k_win).astype(jnp.float32) * scale
        s_loc = (s_loc + bias.astype(jnp.float32)[None]).reshape(b, N_HEADS, GRID_W, n_loc)
        s_ctx = jnp.einsum('bwhd,bchd->bhwc', q_r, k_c).astype(jnp.float32) * scale
        p = jax.nn.softmax(jnp.concatenate([s_loc, s_ctx], axis=-1), axis=-1).astype(v.dtype)
        p_loc = p[..., :n_loc].reshape(b, N_HEADS, GRID_W, kh, NA_KW)
        p_ctx = p[..., n_loc:]
        return (jnp.einsum('bhwij,biwjhd->bwhd', p_loc, v_win)
                + jnp.einsum('bhwc,bchd->bwhd', p_ctx, v_c))

    out = lax.map(row_block, jnp.arange(rows))
    return out.transpose(1, 0, 2, 3, 4).reshape(b, s, ATTN_WIDTH)


def merge_groups(attn_out, conv_out, g_out, w_out):
    a = rms_norm(attn_out, g_out[:ATTN_WIDTH])
    cv = rms_norm(conv_out, g_out[ATTN_WIDTH:])
    return jnp.concatenate([a, cv], axis=-1) @ w_out


def swiglu(h, w_gu, w_down):
    g, u = jnp.split(h @ w_gu, 2, axis=-1)
    return (jax.nn.silu(g) * u) @ w_down


def moe_swiglu(h, w_router, w_gu, w_down):
    logits = (h @ w_router).astype(jnp.float32)
    top_v, top_i = lax.top_k(logits, TOP_K)
    top_w = jax.nn.softmax(top_v, axis=-1)
    gates = jnp.sum(jax.nn.one_hot(top_i, N_EXPERTS, dtype=jnp.float32) * top_w[..., None], axis=-2).astype(h.dtype)
    y = jnp.zeros_like(h)
    for e in range(N_EXPERTS):
        y = y + gates[..., e:e + 1] * swiglu(h, w_gu[e], w_down[e])
    return y


def setup_inputs(seed: int = 0) -> dict:
    key = jax.random.key(seed)
    ks = jax.random.split(key, 17)
    f32 = jnp.float32

    def nrm(k, shape, s):
        return jax.random.normal(k, shape, f32) * s

    return {
        'x': nrm(ks[0], (BATCH, SEQ, D_MODEL), 1.0),
        'c': nrm(ks[1], (BATCH, D_MODEL), 1.0),
        'ctx': nrm(ks[2], (BATCH, CTX_LEN, D_MODEL), 1.0),
        'c_ctx': nrm(ks[3], (D_MODEL,), 1.0),
        'w_ada': nrm(ks[4], (DEPTH, D_MODEL, 6 * D_MODEL), 0.5 * D_MODEL ** -0.5),
        'b_ada': nrm(ks[5], (DEPTH, 6 * D_MODEL), 0.02),
        'norm_g': 1.0 + nrm(ks[6], (DEPTH, 4, D_MODEL), 0.05),
        'w_in': nrm(ks[7], (DEPTH, D_MODEL, IN_WIDTH), D_MODEL ** -0.5),
        'rpb': nrm(ks[8], (DEPTH, N_HEADS, 2 * NA_KH - 1, 2 * NA_KW - 1), 0.1),
        'conv_w': nrm(ks[9], (DEPTH, CONV_K, CONV_WIDTH), CONV_K ** -0.5),
        'out_norm_g': 1.0 + nrm(ks[10], (DEPTH, MIX_WIDTH), 0.05),
        'w_out': nrm(ks[11], (DEPTH, MIX_WIDTH, D_MODEL), MIX_WIDTH ** -0.5),
        'w_gu_dense': nrm(ks[12], (N_DENSE, D_MODEL, 2 * FF_DIM), D_MODEL ** -0.5),
        'w_down_dense': nrm(ks[13], (N_DENSE, FF_DIM, D_MODEL), FF_DIM ** -0.5),
        'w_router': nrm(ks[14], (N_MOE, D_MODEL, N_EXPERTS), D_MODEL ** -0.5),
        'w_gu_moe': nrm(ks[15], (N_MOE, N_EXPERTS, D_MODEL, 2 * FF_DIM), D_MODEL ** -0.5),
        'w_down_moe': nrm(ks[16], (N_MOE, N_EXPERTS, FF_DIM, D_MODEL), FF_DIM ** -0.5),
    }


def reference(x, c, ctx, c_ctx, w_ada, b_ada, norm_g, w_in, rpb, conv_w, out_norm_g, w_out,
              w_gu_dense, w_down_dense, w_router, w_gu_moe, w_down_moe):
    def channel_mixer(l, h):
        if l % 2 == 0:
            return swiglu(h, w_gu_dense[l // 2], w_down_dense[l // 2])
        return moe_swiglu(h, w_router[l // 2], w_gu_moe[l // 2], w_down_moe[l // 2])

    for l in range(DEPTH):
        last = l == DEPTH - 1
        sh_m, sc_m, gt_m, sh_f, sc_f, gt_f = adaln(c[:, None, :], w_ada[l], b_ada[l])
        csh_m, csc_m, cgt_m, csh_f, csc_f, cgt_f = adaln(c_ctx, w_ada[l], b_ada[l])
        g_pre_m, g_post_m, g_pre_f, g_post_f = norm_g[l, 0], norm_g[l, 1], norm_g[l, 2], norm_g[l, 3]

        hx = rms_norm(x, g_pre_m) * (1 + sc_m) + sh_m
        hc = rms_norm(ctx, g_pre_m) * (1 + csc_m) + csh_m
        px = hx @ w_in[l]
        if last:
            pc = hc @ w_in[l][:, K_OFF:B_OFF]
            k_c, v_c = heads(pc[..., :ATTN_WIDTH]), heads(pc[..., ATTN_WIDTH:])
        else:
            pc = hc @ w_in[l]
            k_c, v_c = heads(pc[..., K_OFF:V_OFF]), heads(pc[..., V_OFF:B_OFF])

        attn_x = neighbourhood_attention(heads(px[..., Q_OFF:K_OFF]), heads(px[..., K_OFF:V_OFF]),
                                         heads(px[..., V_OFF:B_OFF]), k_c, v_c, rpb[l])
        conv_x = short_gated_conv(px, conv_w[l])
        yx = merge_groups(attn_x, conv_x, out_norm_g[l], w_out[l])
        x = x + gt_m * rms_norm(yx, g_post_m)

        if not last:
            attn_c = ctx_attention(heads(pc[..., Q_OFF:K_OFF]), k_c, v_c)
            conv_c = short_gated_conv(pc, conv_w[l])
            yc = merge_groups(attn_c, conv_c, out_norm_g[l], w_out[l])
            ctx = ctx + cgt_m * rms_norm(yc, g_post_m)

        hx = rms_norm(x, g_pre_f) * (1 + sc_f) + sh_f
        x = x + gt_f * rms_norm(channel_mixer(l, hx), g_post_f)
        if not last:
            hc = rms_norm(ctx, g_pre_f) * (1 + csc_f) + csh_f
            ctx = ctx + cgt_f * rms_norm(channel_mixer(l, hc), g_post_f)
    return x
```

```python
import numpy as np
from contextlib import ExitStack
import concourse.bass as bass
import concourse.mybir as mybir
from concourse.bass_utils import run_bass_kernel_spmd

F32 = mybir.dt.float32
BF16 = mybir.dt.bfloat16
U32 = mybir.dt.uint32
I32 = mybir.dt.int32
NSEG = 23
PIPE_LAGS = (1, 2)
NSLOT = NSEG * 512
AF = mybir.ActivationFunctionType
ALU = mybir.AluOpType
AX = mybir.AxisListType

D = 1024
S_LEN = 4096
CTX = 256
DEPTH = 2
FF = 3584
NE = 8
INW = 3072
NEG = -30000.0


class Buf:
    __slots__ = ("w", "r", "dsem", "psum")

    def __init__(self):
        self.w = None
        self.r = []
        self.dsem = None
        self.psum = False


class Sched:
    def __init__(self, nc, es):
        self.nc = nc
        self.es = es
        self.engs = {}
        for name, e in (("pe", nc.tensor), ("act", nc.scalar), ("dve", nc.vector),
                        ("pool", nc.gpsimd), ("sp", nc.sync)):
            sem = es.enter_context(nc.semaphore("prog_" + name))
            self.engs[name] = dict(e=e, sem=sem, count=0, seen={})
        self.free_dsems = {"sp": [], "pool": []}
        self.all_dsems = []
        self.phase_dsems = []
        self.persist = []

    def buf(self, dma=False, persist=False):
        b = Buf()
        if dma:
            kind = "pool" if dma == "pool" else "sp"
            if self.free_dsems[kind] and not persist:
                d = self.free_dsems[kind].pop()
            else:
                sem = self.es.enter_context(self.nc.semaphore("dsem%d" % (len(self.all_dsems) + len(self.persist))))
                d = dict(sem=sem, count=0, kind=kind)
                (self.persist if persist else self.all_dsems).append(d)
            b.dsem = d
            if not persist:
                self.phase_dsems.append(d)
        return b

    def _wait(self, eng, deps):
        best = {}
        for sem, val in deps:
            k = id(sem)
            if k not in best or best[k][1] < val:
                best[k] = (sem, val)
        for k, (sem, val) in best.items():
            if eng["seen"].get(k, 0) < val:
                eng["e"].wait_ge(sem, val)
                eng["seen"][k] = val

    @staticmethod
    def _deps(reads, writes):
        deps = []
        for b in reads:
            if b.w is not None:
                deps.append(b.w)
            if b.psum:
                deps.extend(b.r)
        for b in writes:
            if b.w is not None:
                deps.append(b.w)
            deps.extend(b.r)
        return deps

    @staticmethod
    def _mark(tok, reads, writes):
        for b in reads:
            b.r.append(tok)
            if len(b.r) > 24:
                best = {}
                for s, v in b.r:
                    if id(s) not in best or best[id(s)][1] < v:
                        best[id(s)] = (s, v)
                b.r = list(best.values())
        for b in writes:
            b.w = tok
            b.r = []

    def op(self, engname, fn, reads=(), writes=()):
        eng = self.engs[engname]
        deps = self._deps(reads, writes)
        if engname == "pe":
            deps = [d for d in deps if d[0] is not eng["sem"]]
        self._wait(eng, deps)
        ins = fn(eng["e"])
        eng["count"] += 1
        ins.then_inc(eng["sem"], 1)
        eng["seen"].pop(id(eng["sem"]), None)
        self._mark((eng["sem"], eng["count"]), reads, writes)
        return ins

    def dma(self, engname, out, in_, reads=(), writes=(), sembuf=None):
        eng = self.engs[engname]
        self._wait(eng, self._deps(reads, writes))
        ins = eng["e"].dma_start(out=out, in_=in_)
        d = sembuf.dsem
        assert d["kind"] == ("pool" if engname == "pool" else "sp"), (d["kind"], engname)
        d["count"] += 16
        ins.then_inc(d["sem"], 16)
        self._mark((d["sem"], d["count"]), reads, writes)
        return ins

    def dma_fn(self, engname, fn, reads=(), writes=(), sembuf=None):
        eng = self.engs[engname]
        self._wait(eng, self._deps(reads, writes))
        ins = fn(eng["e"])
        d = sembuf.dsem
        assert d["kind"] == ("pool" if engname == "pool" else "sp"), (d["kind"], engname)
        d["count"] += 16
        ins.then_inc(d["sem"], 16)
        self._mark((d["sem"], d["count"]), reads, writes)
        return ins

    def barrier(self):
        deps = [(e["sem"], e["count"]) for e in self.engs.values() if e["count"] > 0]
        deps += [(d["sem"], d["count"]) for d in self.all_dsems if d["count"] > 0]
        for eng in self.engs.values():
            self._wait(eng, deps)

    def end_phase(self):
        self.barrier()
        for d in self.phase_dsems:
            self.free_dsems[d["kind"]].append(d)
        self.phase_dsems = []


class TT:
    def __init__(self, t, b):
        self.t = t
        self.b = b


def _bc_rows(handle_ap, row, n, parts=128):
    return handle_ap[row:row + 1, 0:n].partition_broadcast(parts)


def build_nc(dbg=None):
    nc = bass.Bass("TRN2", target_bir_lowering=False)
    di = lambda name, shape, dt=F32: nc.dram_tensor(name, list(shape), dt, kind="ExternalInput").ap()
    x_in = di("x", [S_LEN, D])
    ctx_in = di("ctx", [CTX, D])
    cvec = di("cvec", [128, 16])
    w_ada = di("w_ada", [DEPTH, D, 6 * D])
    b_ada = di("b_ada", [DEPTH, 6 * D])
    norm_g = di("norm_g", [DEPTH * 4, D])
    w_in = di("w_in", [DEPTH, D, INW])
    nab_mid = di("nab_mid", [DEPTH, 128, 8, 576])
    nab_edge = di("nab_edge", [DEPTH, 4, 8, 128, 512])
    convw = di("convw", [DEPTH, 128, 12])
    gout = di("gout", [DEPTH, D])
    goutc = di("goutc", [DEPTH, 128, 4])
    w_out = di("w_out", [DEPTH, D, D])
    w_gu_d = di("w_gu_dense", [1, D, 2 * FF])
    w_dn_d = di("w_down_dense", [1, FF, D])
    w_rt = di("w_router", [128, 8, NE])
    w_gu_m = di("w_gu_moe", [NE, D, 2 * FF])
    w_dn_m = di("w_down_moe", [NE, FF, D])
    y_out = nc.dram_tensor("y", [S_LEN, D], F32, kind="ExternalOutput").ap()

    ds = lambda name, shape, dt: nc.dram_tensor(name, list(shape), dt).ap()
    QTd = ds("QTd", [4, 128, S_LEN], BF16)
    KTd = ds("KTd", [4, 128, S_LEN], BF16)
    Vd = ds("Vd", [S_LEN, 512], BF16)
    cnTd = ds("cnTd", [4, 128, S_LEN], BF16)
    QcTd = ds("QcTd", [4, 128, CTX], BF16)
    KcTd = ds("KcTd", [4, 128, CTX], BF16)
    Vcd = ds("Vcd", [CTX, 512], BF16)
    cncTd = ds("cncTd", [4, 128, CTX], BF16)
    mods = ds("mods", [12, D], F32)
    WGd = ds("WGd", [NE * 7 * 128, 8 * 512], BF16)
    WUd = ds("WUd", [NE * 7 * 128, 8 * 512], BF16)
    WDd = ds("WDd", [NE * 7 * 128, 4 * D], BF16)
    hs = ds("hs", [NSLOT, D], BF16)
    ys = ds("ys", [NSLOT, D], F32)
    ctxs = ds("ctxs", [CTX, D], F32)
    dbg_out = None
    if dbg is not None:
        dbg_out = nc.dram_tensor("dbg", list(dbg["shape"]), dbg.get("dt", F32), kind="ExternalOutput").ap()

    ges = ExitStack()
    with ges:
        S = Sched(nc, ges)

        uid = [0]

        def alloc(es, name, shape, dt, dma=False, psum=False):
            uid[0] += 1
            name = "%s_%d" % (name, uid[0])
            if psum:
                t = es.enter_context(nc.psum_tensor(name, list(shape), dt))
            else:
                t = es.enter_context(nc.sbuf_tensor(name, list(shape), dt))
            tt = TT(t, S.buf(dma=dma))
            tt.b.psum = bool(psum)
            return tt

        def allocn(es, n, name, shape, dt, dma=False, psum=False):
            return [alloc(es, "%s%d" % (name, i), shape, dt, dma=dma, psum=psum) for i in range(n)]

        ident = alloc(ges, "ident", [128, 128], BF16)
        ident32 = alloc(ges, "ident32", [128, 128], F32)
        ones = alloc(ges, "ones", [128, 128], BF16)
        epsb = alloc(ges, "epsb", [128, 1], F32)
        for idt in (ident, ident32):
            S.op("pool", lambda e: e.memset(idt.t[:], 0.0), writes=[idt.b])
            S.op("pool", lambda e: e.affine_select(out=idt.t[:], in_=idt.t[:], pattern=[[-1, 128]],
                                                   compare_op=ALU.not_equal, fill=1.0, base=0,
                                                   channel_multiplier=1), reads=[idt.b], writes=[idt.b])
        S.op("pool", lambda e: e.memset(ones.t[:], 1.0), writes=[ones.b])
        S.op("pool", lambda e: e.memset(epsb.t[:], 1e-6), writes=[epsb.b])

        ones32 = alloc(ges, "ones32", [128, 128], F32)
        tri32 = alloc(ges, "tri32", [128, 128], F32)
        S.op("pool", lambda e: e.memset(ones32.t[:], 1.0), writes=[ones32.b])
        S.op("pool", lambda e: e.memset(tri32.t[:], 1.0), writes=[tri32.b])
        S.op("pool", lambda e: e.affine_select(out=tri32.t[:], in_=tri32.t[:], pattern=[[1, 128]],
                                               compare_op=ALU.is_gt, fill=0.0, base=0,
                                               channel_multiplier=-1), reads=[tri32.b], writes=[tri32.b])
        zt = alloc(ges, "zt", [128, D], BF16)
        S.op("pool", lambda e: e.memset(zt.t[:], 0.0), writes=[zt.b])
        hsz = S.buf(dma="pool", persist=True)
        convb = S.buf(dma="pool", persist=True)
        conv_pieces = []
        for r in range(NSLOT // 512):
            conv_pieces.append((hs[r * 512:(r + 1) * 512, :].rearrange("(r p) d -> p r d", p=128),
                                zt.t[:].unsqueeze(1).to_broadcast([128, 4, D]), [zt.b], hsz))
        for ex in range(NE):
            for c in range(7):
                r0 = (ex * 7 + c) * 128
                conv_pieces.append((WGd[r0:r0 + 128, :].rearrange("p (c f) -> p c f", c=8),
                                    w_gu_m[ex, :, c * 512:(c + 1) * 512].rearrange("(c p) f -> p c f", p=128), [], convb))
                conv_pieces.append((WUd[r0:r0 + 128, :].rearrange("p (c f) -> p c f", c=8),
                                    w_gu_m[ex, :, FF + c * 512:FF + (c + 1) * 512].rearrange("(c p) f -> p c f", p=128),
                                    [], convb))
                conv_pieces.append((WDd[r0:r0 + 128, :].rearrange("p (j d) -> p j d", j=4),
                                    w_dn_m[ex, c * 512:(c + 1) * 512, :].rearrange("(j p) d -> p j d", p=128), [], convb))
        conv_pos = [0]

        def conv_step(n):
            for _ in range(n):
                if conv_pos[0] >= len(conv_pieces):
                    return
                o, i_, rd, sb = conv_pieces[conv_pos[0]]
                conv_pos[0] += 1
                S.dma("pool", o, i_, reads=rd, sembuf=sb)
                sb.w = (sb.dsem["sem"], sb.dsem["count"])

        def rstd_from_ss(ss, rstd, scale):
            S.op("act", lambda e: e.activation(out=rstd.t[:], in_=ss.t[:], func=AF.Sqrt, scale=scale,
                                               bias=epsb.t[:]), reads=[ss.b, epsb.b], writes=[rstd.b])
            S.op("dve", lambda e: e.reciprocal(out=rstd.t[:], in_=rstd.t[:]), reads=[rstd.b], writes=[rstd.b])

        def phase_ada(l):
            es = ExitStack()
            with es:
                cv = alloc(es, "cv", [128, 16], F32, dma=True)
                sc = alloc(es, "sc", [128, 16], F32)
                scb = alloc(es, "scb", [128, 16], BF16)
                wch = allocn(es, 2, "wch", [128, 8, 512], BF16, dma="pool")
                row = alloc(es, "row", [1, 2, 6 * D], F32)
                bad = alloc(es, "bad", [1, 6 * D], F32, dma=True)
                ng = alloc(es, "ng", [1, 4, D], F32, dma=True)
                ps = allocn(es, 2, "adaps", [128, 512], F32, psum=True)
                stb = S.buf(dma=True)
                S.dma("sp", cv.t[:], cvec[:, :], writes=[cv.b], sembuf=cv.b)
                S.dma("sp", bad.t[:], b_ada[l:l + 1, :], writes=[bad.b], sembuf=bad.b)
                S.dma("sp", ng.t[:], norm_g[4 * l:4 * l + 4, :].rearrange("(o r) d -> o r d", o=1),
                      writes=[ng.b], sembuf=ng.b)
                S.op("act", lambda e: e.activation(out=sc.t[:], in_=cv.t[:], func=AF.Silu),
                     reads=[cv.b], writes=[sc.b])
                S.op("dve", lambda e: e.tensor_copy(out=scb.t[:], in_=sc.t[:]), reads=[sc.b], writes=[scb.b])
                for j in range(12):
                    w = wch[j % 2]
                    S.dma("pool", w.t[:], w_ada[l, :, j * 512:(j + 1) * 512].rearrange("(c p) f -> p c f", p=128),
                          writes=[w.b], sembuf=w.b)
                    for src in range(2):
                        p = ps[src]
                        for ch in range(8):
                            S.op("pe", lambda e: e.matmul(p.t[0:1, :], lhsT=scb.t[:, src * 8 + ch:src * 8 + ch + 1],
                                                          rhs=w.t[:, ch, :], start=(ch == 0), stop=(ch == 7)),
                                 reads=[scb.b, w.b], writes=[p.b])
                        S.op("dve", lambda e: e.tensor_tensor(out=row.t[0:1, src, j * 512:(j + 1) * 512],
                                                              in0=p.t[0:1, :], in1=bad.t[0:1, j * 512:(j + 1) * 512],
                                                              op=ALU.add),
                             reads=[p.b, bad.b], writes=[row.b])
                for src in range(2):
                    for (k, gi) in ((1, 0), (4, 2)):
                        S.op("dve", lambda e: e.scalar_tensor_tensor(
                            out=row.t[0:1, src, k * D:(k + 1) * D], in0=row.t[0:1, src, k * D:(k + 1) * D],
                            scalar=1.0, in1=ng.t[0:1, gi, :], op0=ALU.add, op1=ALU.mult),
                            reads=[row.b, ng.b], writes=[row.b])
                    for (k, gi) in ((2, 1), (5, 3)):
                        S.op("dve", lambda e: e.tensor_tensor(
                            out=row.t[0:1, src, k * D:(k + 1) * D], in0=row.t[0:1, src, k * D:(k + 1) * D],
                            in1=ng.t[0:1, gi, :], op=ALU.mult), reads=[row.b, ng.b], writes=[row.b])
                S.dma("sp", mods.rearrange("(o r) d -> o (r d)", o=1), row.t[0:1, :, :].rearrange("o s d -> o (s d)"),
                      reads=[row.b], sembuf=stb)
                S.end_phase()

        def load_bc(es, name, rowidx, n=D, src=None):
            t = alloc(es, name, [128, n], F32, dma=True)
            srcap = mods if src is None else src
            S.dma("sp", t.t[:], _bc_rows(srcap, rowidx, n), writes=[t.b], sembuf=t.b)
            return t

        def norm_tile(xt, gs, sh, ss, rstd, junk, tmp, hb, hb32=None):
            S.op("act", lambda e: e.activation(out=junk.t[:], in_=xt.t[:], func=AF.Square, accum_out=ss.t[:]),
                 reads=[xt.b], writes=[junk.b, ss.b])
            rstd_from_ss(ss, rstd, 1.0 / D)
            S.op("dve", lambda e: e.scalar_tensor_tensor(out=tmp.t[:], in0=xt.t[:], scalar=rstd.t[:, 0:1],
                                                         in1=gs.t[:], op0=ALU.mult, op1=ALU.mult),
                 reads=[xt.b, rstd.b, gs.b], writes=[tmp.b])
            if hb32 is not None:
                S.op("pool", lambda e: e.tensor_tensor(out=hb32.t[:], in0=tmp.t[:], in1=sh.t[:], op=ALU.add),
                     reads=[tmp.b, sh.b], writes=[hb32.b])
                S.op("pool", lambda e: e.tensor_copy(out=hb.t[:], in_=hb32.t[:]), reads=[hb32.b], writes=[hb.b])
            else:
                S.op("pool", lambda e: e.tensor_tensor(out=hb.t[:], in0=tmp.t[:], in1=sh.t[:], op=ALU.add),
                     reads=[tmp.b, sh.b], writes=[hb.b])

        def phase_a(l, last):
            es = ExitStack()
            with es:
                win = alloc(es, "win", [128, 8, INW], BF16)
                win_b = [S.buf(dma="pool") for _ in range(6)]
                for j in ([1, 2, 3, 4, 5, 0] if last else [3, 4, 5, 0, 1, 2]):
                    S.dma("pool", win.t[:, :, j * 512:(j + 1) * 512],
                          w_in[l, :, j * 512:(j + 1) * 512].rearrange("(c p) f -> p c f", p=128),
                          writes=[win_b[j]], sembuf=win_b[j])
                gs_x = load_bc(es, "gs_x", 1)
                sh_x = load_bc(es, "sh_x", 0)
                gs_c = load_bc(es, "gs_c", 6 + 1)
                sh_c = load_bc(es, "sh_c", 6 + 0)
                cw = alloc(es, "cw", [128, 12], F32, dma=True)
                S.dma("sp", cw.t[:], convw[l], writes=[cw.b], sembuf=cw.b)
                goc = alloc(es, "goc", [128, 4], F32, dma=True)
                S.dma("sp", goc.t[:], goutc[l], writes=[goc.b], sembuf=goc.b)

                xts = allocn(es, 4, "xt", [128, D], F32, dma=True)
                junk = alloc(es, "junk", [128, D], BF16)
                tmp = allocn(es, 2, "tmp", [128, D], F32)
                hbs = allocn(es, 4, "hb", [128, D], BF16)
                sss = allocn(es, 2, "ss", [128, 1], F32)
                rstds = allocn(es, 2, "rstd", [128, 1], F32)
                hT = allocn(es, 2, "hT", [128, 8, 512], BF16)
                tps = allocn(es, 2, "tp", [128, 8, 128], BF16, psum=True)
                mps = allocn(es, 4, "mp", [128, 512], F32, psum=True)
                sps = alloc(es, "sps", [128, 512], F32, psum=True)
                qks = allocn(es, 2, "qks", [128, 8, 512], BF16, dma=True)
                vst = allocn(es, 2, "vst", [128, 512], BF16, dma=True)
                BT = allocn(es, 2, "BT", [128, 4, 512], BF16)
                vT = allocn(es, 2, "vT", [128, 4, 514], F32)
                csb = allocn(es, 2, "csb", [128, 512], F32)
                cacc = allocn(es, 4, "cacc", [128, 512], F32)
                co = alloc(es, "co", [128, 4, 512], F32)
                sq = allocn(es, 4, "sq", [128, 512], BF16)
                rbc = alloc(es, "rbc", [128, 512], F32)
                cn = allocn(es, 2, "cn", [128, 4, 512], BF16, dma=True)
                cnt = dict(mp=0)

                csrc = ctx_in if l == 0 else ctxs
                xsrc = x_in if l == 0 else y_out
                G = [dict(src=csrc, dst=dict(Q=QcTd, K=KcTd, V=Vcd), tok0=0, T=CTX, gs=gs_c, sh=sh_c,
                          want_q=not last, want_conv=not last, lat=False, cdst=cncTd)]
                for g in range(8):
                    G.append(dict(src=xsrc, dst=dict(Q=QTd, K=KTd, V=Vd), tok0=g * 512, T=512, gs=gs_x, sh=sh_x,
                                  want_q=True, want_conv=True, lat=True, cdst=cnTd))
                n0 = 0
                for k, gd in enumerate(G):
                    gd["k"] = k
                    gd["n0"] = n0
                    n0 += gd["T"] // 128

                def load_part(gd):
                    for i in range(gd["T"] // 128):
                        n = gd["n0"] + i
                        xt = xts[n % 4]
                        S.dma("sp", xt.t[:], gd["src"][gd["tok0"] + i * 128:gd["tok0"] + (i + 1) * 128, :],
                              writes=[xt.b], sembuf=xt.b)

                def norm_part(gd):
                    for i in range(gd["T"] // 128):
                        n = gd["n0"] + i
                        xt = xts[n % 4]
                        k2 = n % 2
                        norm_tile(xt, gd["gs"], gd["sh"], sss[k2], rstds[k2], junk, tmp[k2], hbs[n % 4])

                def trans_part(gd):
                    h = hT[gd["k"] % 2]
                    for i in range(gd["T"] // 128):
                        n = gd["n0"] + i
                        tp = tps[n % 2]
                        hb_ = hbs[n % 4]
                        for c in range(8):
                            S.op("pe", lambda e: e.transpose(out=tp.t[:, c, :], in_=hb_.t[:, c * 128:(c + 1) * 128],
                                                             identity=ident.t[:]),
                                 reads=[hb_.b, ident.b], writes=[tp.b])
                        S.op("act", lambda e: e.activation(out=h.t[:, :, i * 128:(i + 1) * 128], in_=tp.t[:],
                                                           func=AF.Copy), reads=[tp.b], writes=[h.b])

                def fmm(gd, ft):
                    T = gd["T"]
                    h = hT[gd["k"] % 2]
                    p = mps[cnt["mp"] % 4]
                    cnt["mp"] += 1
                    for c in range(8):
                        S.op("pe", lambda e: e.matmul(p.t[:, 0:T], lhsT=win.t[:, c, ft * 128:(ft + 1) * 128],
                                                      rhs=h.t[:, c, 0:T], start=(c == 0), stop=(c == 7)),
                             reads=[win_b[ft // 4], h.b], writes=[p.b])
                    return p

                def mm_bcu(gd):
                    T = gd["T"]
                    bt = BT[gd["k"] % 2]
                    vt = vT[gd["k"] % 2]
                    for c in range(4):
                        p = fmm(gd, 12 + c)
                        S.op("act", lambda e: e.activation(out=bt.t[:, c, 0:T], in_=p.t[:, 0:T], func=AF.Copy),
                             reads=[p.b], writes=[bt.b])
                        p = fmm(gd, 16 + c)
                        cs = csb[c % 2]
                        S.op("act", lambda e: e.activation(out=cs.t[:, 0:T], in_=p.t[:, 0:T], func=AF.Copy),
                             reads=[p.b], writes=[cs.b])
                        p = fmm(gd, 20 + c)
                        S.op("dve", lambda e: e.tensor_tensor(out=vt.t[:, c, 1:T + 1], in0=p.t[:, 0:T], in1=cs.t[:, 0:T],
                                                              op=ALU.mult), reads=[p.b, cs.b], writes=[vt.b])

                def mm_qk(gd):
                    T, tok0, dst = gd["T"], gd["tok0"], gd["dst"]
                    h = hT[gd["k"] % 2]
                    st = qks[gd["k"] % 2]
                    for ft in range(8):
                        if ft < 4 and not gd["want_q"]:
                            continue
                        p = fmm(gd, ft)
                        if ft < 4:
                            S.op("act", lambda e: e.activation(out=st.t[:, ft, 0:T], in_=p.t[:, 0:T], func=AF.Copy,
                                                               scale=0.125), reads=[p.b], writes=[st.b])
                        else:
                            S.op("dve", lambda e: e.tensor_copy(out=st.t[:, ft, 0:T], in_=p.t[:, 0:T]),
                                 reads=[p.b], writes=[st.b])
                    if gd["want_q"]:
                        S.dma("sp", dst["Q"][:, :, tok0:tok0 + T].rearrange("c p t -> p c t"), st.t[:, 0:4, 0:T],
                              reads=[st.b], sembuf=st.b)
                    S.dma("sp", dst["K"][:, :, tok0:tok0 + T].rearrange("c p t -> p c t"), st.t[:, 4:8, 0:T],
                          reads=[st.b], sembuf=st.b)

                def mm_v(gd):
                    T, tok0, dst = gd["T"], gd["tok0"], gd["dst"]
                    h = hT[gd["k"] % 2]
                    for i in range(T // 128):
                        p = mps[cnt["mp"] % 4]
                        cnt["mp"] += 1
                        for c in range(8):
                            S.op("pe", lambda e: e.matmul(p.t[:, :], lhsT=h.t[:, c, i * 128:(i + 1) * 128],
                                                          rhs=win.t[:, c, 1024:1536], start=(c == 0), stop=(c == 7)),
                                 reads=[win_b[2], h.b], writes=[p.b])
                        v = vst[i % 2]
                        S.op("act", lambda e: e.activation(out=v.t[:], in_=p.t[:], func=AF.Copy),
                             reads=[p.b], writes=[v.b])
                        S.dma("sp", dst["V"][tok0 + i * 128:tok0 + (i + 1) * 128, :], v.t[:], reads=[v.b], sembuf=v.b)

                def conv_ew(gd):
                    T = gd["T"]
                    bt = BT[gd["k"] % 2]
                    vt = vT[gd["k"] % 2]
                    for c in range(4):
                        a = cacc[c]
                        S.op("act", lambda e: e.activation(out=a.t[:, 0:T], in_=vt.t[:, c, 1:T + 1], func=AF.Copy,
                                                           scale=cw.t[:, c * 3 + 1:c * 3 + 2]),
                             reads=[vt.b, cw.b], writes=[a.b])
                    for c in range(4):
                        a = cacc[c]
                        S.op("dve", lambda e: e.scalar_tensor_tensor(out=a.t[:, 0:T], in0=vt.t[:, c, 0:T],
                                                                     scalar=cw.t[:, c * 3:c * 3 + 1], in1=a.t[:, 0:T],
                                                                     op0=ALU.mult, op1=ALU.add),
                             reads=[vt.b, cw.b, a.b], writes=[a.b])
                        S.op("dve", lambda e: e.scalar_tensor_tensor(out=a.t[:, 0:T], in0=vt.t[:, c, 2:T + 2],
                                                                     scalar=cw.t[:, c * 3 + 2:c * 3 + 3], in1=a.t[:, 0:T],
                                                                     op0=ALU.mult, op1=ALU.add),
                             reads=[vt.b, cw.b, a.b], writes=[a.b])
                    for c in range(4):
                        a = cacc[c]
                        S.op("pool", lambda e: e.tensor_tensor(out=co.t[:, c, 0:T], in0=a.t[:, 0:T], in1=bt.t[:, c, 0:T],
                                                               op=ALU.mult), reads=[a.b, bt.b], writes=[co.b])

                def conv_sq(gd):
                    T = gd["T"]
                    for c in range(4):
                        s = sq[c]
                        S.op("act", lambda e: e.activation(out=s.t[:, 0:T], in_=co.t[:, c, 0:T], func=AF.Square),
                             reads=[co.b], writes=[s.b])

                def conv_fin(gd):
                    T, tok0 = gd["T"], gd["tok0"]
                    for c in range(4):
                        s = sq[c]
                        S.op("pe", lambda e: e.matmul(sps.t[:, 0:T], lhsT=ones.t[:], rhs=s.t[:, 0:T],
                                                      start=(c == 0), stop=(c == 3)),
                             reads=[ones.b, s.b], writes=[sps.b])
                    S.op("act", lambda e: e.activation(out=rbc.t[:, 0:T], in_=sps.t[:, 0:T], func=AF.Sqrt,
                                                       scale=1.0 / 512, bias=epsb.t[:]),
                         reads=[sps.b, epsb.b], writes=[rbc.b])
                    S.op("dve", lambda e: e.reciprocal(out=rbc.t[:, 0:T], in_=rbc.t[:, 0:T]), reads=[rbc.b], writes=[rbc.b])
                    o = cn[gd["k"] % 2]
                    for c in range(4):
                        S.op("dve", lambda e: e.scalar_tensor_tensor(out=o.t[:, c, 0:T], in0=co.t[:, c, 0:T],
                                                                     scalar=goc.t[:, c:c + 1], in1=rbc.t[:, 0:T],
                                                                     op0=ALU.mult, op1=ALU.mult),
                             reads=[co.b, goc.b, rbc.b], writes=[o.b])
                    S.dma("sp", gd["cdst"][:, :, tok0:tok0 + T].rearrange("c p t -> p c t"), o.t[:, :, 0:T],
                          reads=[o.b], sembuf=o.b)

                load_part(G[0])
                norm_part(G[0])
                trans_part(G[0])
                if len(G) > 1:
                    load_part(G[1])
                prev_lat = None
                for k, gd in enumerate(G):
                    nxt = G[k + 1] if k + 1 < len(G) else None
                    cv = None
                    if nxt is not None:
                        norm_part(nxt)
                        if k + 2 < len(G):
                            load_part(G[k + 2])
                    if gd["want_conv"]:
                        mm_bcu(gd)
                    if gd["want_conv"]:
                        vt = vT[k % 2]
                        if not gd["lat"]:
                            S.op("pool", lambda e: e.memset(vt.t[:, :, 0:1], 0.0), writes=[vt.b])
                            S.op("pool", lambda e: e.memset(vt.t[:, :, CTX + 1:CTX + 2], 0.0), writes=[vt.b])
                            cv = gd
                        elif prev_lat is None:
                            S.op("pool", lambda e: e.memset(vt.t[:, :, 0:1], 0.0), writes=[vt.b])
                        else:
                            pv = vT[prev_lat["k"] % 2]
                            S.op("pool", lambda e: e.tensor_copy(out=vt.t[:, :, 0:1], in_=pv.t[:, :, 512:513]),
                                 reads=[pv.b], writes=[vt.b])
                            S.op("pool", lambda e: e.tensor_copy(out=pv.t[:, :, 513:514], in_=vt.t[:, :, 1:2]),
                                 reads=[vt.b], writes=[pv.b])
                            cv = prev_lat
                        if cv is not None:
                            conv_ew(cv)
                    mm_qk(gd)
                    if cv is not None:
                        conv_sq(cv)
                    if nxt is not None:
                        trans_part(nxt)
                    mm_v(gd)
                    if cv is not None:
                        conv_fin(cv)
                    if gd["lat"]:
                        prev_lat = gd
                        conv_step(3)
                pv = vT[prev_lat["k"] % 2]
                S.op("pool", lambda e: e.memset(pv.t[:, :, 513:514], 0.0), writes=[pv.b])
                conv_ew(prev_lat)
                conv_sq(prev_lat)
                conv_fin(prev_lat)
                S.end_phase()

        def phase_b(l, last):
            es = ExitStack()
            with es:
                KT = alloc(es, "KT", [128, 4, S_LEN], BF16, dma=True)
                V = alloc(es, "V", [128, 32, 512], BF16, dma=True)
                KcT = alloc(es, "KcT", [128, 4, CTX], BF16, dma=True)
                Vc = alloc(es, "Vc", [128, 2, 512], BF16, dma=True)
                nbm = alloc(es, "nbm", [128, 8, 576], F32, dma=True)

                def init_loads_first():
                    S.dma("sp", KcT.t[:], KcTd.rearrange("c p t -> p c t"), writes=[KcT.b], sembuf=KcT.b)
                    S.dma("sp", Vc.t[:], Vcd.rearrange("(t p) f -> p t f", p=128), writes=[Vc.b], sembuf=Vc.b)

                def init_loads_rest():
                    S.dma("sp", KT.t[:, 0, :], KTd[0], writes=[KT.b], sembuf=KT.b)
                    S.dma("sp", V.t[:, 0:8, :], Vd[0:1024, :].rearrange("(t p) f -> p t f", p=128),
                          writes=[V.b], sembuf=V.b)
                    for c in range(1, 4):
                        S.dma("sp", KT.t[:, c, :], KTd[c], writes=[KT.b], sembuf=KT.b)
                    S.dma("sp", nbm.t[:], nab_mid[l], writes=[nbm.b], sembuf=nbm.b)
                    for q in range(1, 4):
                        S.dma("sp", V.t[:, q * 8:(q + 1) * 8, :],
                              Vd[q * 1024:(q + 1) * 1024, :].rearrange("(t p) f -> p t f", p=128),
                              writes=[V.b], sembuf=V.b)
                wo = alloc(es, "wo", [128, 8, D], BF16, dma="pool")
                for j in range(2):
                    S.dma("pool", wo.t[:, :, j * 512:(j + 1) * 512],
                          w_out[l, :, j * 512:(j + 1) * 512].rearrange("(c p) f -> p c f", p=128),
                          writes=[wo.b], sembuf=wo.b)
                ggt_x = load_bc(es, "ggt_x", 2)
                ggt_c = load_bc(es, "ggt_c", 6 + 2) if not last else None
                goa = load_bc(es, "goa", l, n=512, src=gout)

                NB = 3
                QA = allocn(es, 2, "QA", [128, 4, 512], BF16, dma=True)
                QB = allocn(es, 2, "QB", [128, 4, 512], BF16, dma=True)
                for qz in QA:
                    S.op("pool", lambda e: e.memset(qz.t[64:128, :, :], 0.0), writes=[qz.b])
                for qz in QB:
                    S.op("pool", lambda e: e.memset(qz.t[0:64, :, :], 0.0), writes=[qz.b])
                mT = allocn(es, 2, "mT", [128, 8, 512], BF16, dma=True)
                nbe = allocn(es, 4, "nbe", [128, 512], F32, dma=True)
                psA = allocn(es, 2, "psA", [128, 512], F32, psum=True)
                psB = allocn(es, 2, "psB", [128, 512], F32, psum=True)
                psT = allocn(es, 2, "psT", [128, 8, 128], BF16, psum=True)
                psO = alloc(es, "psO", [128, 512], F32, psum=True)
                psY = alloc(es, "psY", [128, 512], F32, psum=True)
                Ssb = allocn(es, NB, "Ssb", [128, 832], F32)
                Sctx_b = [S.buf() for _ in range(NB)]
                Psb = allocn(es, NB, "Psb", [128, 832], BF16)
                PT = allocn(es, NB, "PT", [128, 7, 128], BF16)
                nmx = allocn(es, NB, "nmx", [128, 1], F32)
                rsum = allocn(es, 3, "rsum", [128, 8], F32)
                rinv = allocn(es, 2, "rinv", [128, 8], F32)
                Osb = allocn(es, 2, "Osb", [128, 512], F32)
                On = allocn(es, 2, "On", [128, 512], BF16)
                junk = alloc(es, "junkb", [128, 512], BF16)
                ss = allocn(es, 2, "ssb", [128, 2], F32)
                ss1 = allocn(es, 2, "ss1b", [128, 1], F32)
                rstd = allocn(es, 2, "rstdb", [128, 1], F32)
                xr = allocn(es, 2, "xr", [128, D], F32, dma=True)
                ysb = allocn(es, 2, "ysb", [128, D], F32)

                def rstd_lnexp(ssv, rs_, scale):
                    S.op("act", lambda e: e.activation(out=rs_.t[:], in_=ssv.t[:], func=AF.Ln, scale=scale,
                                                       bias=epsb.t[:]), reads=[ssv.b, epsb.b], writes=[rs_.b])
                    S.op("act", lambda e: e.activation(out=rs_.t[:], in_=rs_.t[:], func=AF.Exp, scale=-0.5),
                         reads=[rs_.b], writes=[rs_.b])

                def stage_a(u):
                    i = u["i"]
                    a, b = psA[i % 2], psB[i % 2]
                    q_ap, pb, chunk = u["q"], u["pb"], u["chunk"]
                    if u["loc"] is not None:
                        tok0, nloc, tile0 = u["loc"]
                        S.op("pe", lambda e: e.matmul(a.t[:, 0:512], lhsT=q_ap,
                                                      rhs=KT.t[:, chunk, tok0:tok0 + 512], start=True, stop=True),
                             reads=[u["qb"], KT.b], writes=[a.b])
                        if nloc > 512:
                            S.op("pe", lambda e: e.matmul(b.t[:, 0:64], lhsT=q_ap,
                                                          rhs=KT.t[:, chunk, tok0 + 512:tok0 + 576],
                                                          start=True, stop=True),
                                 reads=[u["qb"], KT.b], writes=[b.b])
                    S.op("pe", lambda e: e.matmul(b.t[:, 64:320], lhsT=q_ap, rhs=KcT.t[:, chunk, :],
                                                  start=True, stop=True), reads=[u["qb"], KcT.b], writes=[b.b])

                def stage_b1(u):
                    i, h = u["i"], u["h"]
                    a, b = psA[i % 2], psB[i % 2]
                    s, mx = Ssb[i % NB], nmx[i % NB]
                    sc_b = Sctx_b[i % NB]
                    nloc = u["loc"][1] if u["loc"] is not None else 0
                    ntot = nloc + CTX
                    S.op("act", lambda e: e.activation(out=s.t[:, nloc:ntot], in_=b.t[:, 64:320], func=AF.Copy),
                         reads=[b.b], writes=[sc_b])
                    if u["loc"] is not None:
                        if u["edge"] is None:
                            bt, bap = nbm.b, nbm.t[:, h, :]
                        else:
                            nb = nbe[u["nbi"] % 4]
                            bt, bap = nb.b, nb.t[:, :]
                        S.op("dve", lambda e: e.tensor_tensor(out=s.t[:, 0:512], in0=a.t[:, 0:512], in1=bap[:, 0:512],
                                                              op=ALU.add), reads=[a.b, bt], writes=[s.b])
                        if nloc > 512:
                            S.op("dve", lambda e: e.tensor_tensor(out=s.t[:, 512:576], in0=b.t[:, 0:64],
                                                                  in1=bap[:, 512:576], op=ALU.add),
                                 reads=[b.b, bt], writes=[s.b])
                    S.op("dve", lambda e: e.reduce_max(out=mx.t[:], in_=s.t[:, 0:ntot], axis=AX.X, negate=True),
                         reads=[s.b, sc_b], writes=[mx.b])

                def stage_b2(u):
                    i, h = u["i"], u["h"]
                    s, pp, mx = Ssb[i % NB], Psb[i % NB], nmx[i % NB]
                    sc_b = Sctx_b[i % NB]
                    rs = rsum[u["tile"] % 3]
                    nloc = u["loc"][1] if u["loc"] is not None else 0
                    ntot = nloc + CTX
                    S.op("act", lambda e: e.activation(out=pp.t[:, 0:ntot], in_=s.t[:, 0:ntot], func=AF.Exp,
                                                       bias=mx.t[:, 0:1], accum_out=rs.t[:, h:h + 1]),
                         reads=[s.b, sc_b, mx.b], writes=[pp.b, rs.b])

                def chunks_of(u):
                    nloc = 0
                    chunks = []
                    if u["loc"] is not None:
                        tok0, nloc, tile0 = u["loc"]
                        for k in range(4):
                            chunks.append((k * 128, 128, V, tile0 + k))
                        if nloc > 512:
                            chunks.append((512, 64, V, tile0 + 4))
                    chunks.append((nloc, 128, Vc, 0))
                    chunks.append((nloc + 128, 128, Vc, 1))
                    return chunks

                def stage_c1(u):
                    i = u["i"]
                    pp = Psb[i % NB]
                    pst = psT[i % 2]
                    for j, (off, sz, vt, ti) in enumerate(chunks_of(u)):
                        S.op("pe", lambda e: e.transpose(out=pst.t[0:sz, j, :], in_=pp.t[:, off:off + sz],
                                                         identity=ident.t[:]),
                             reads=[pp.b, ident.b], writes=[pst.b])

                def stage_c1b(u):
                    i = u["i"]
                    pt = PT[i % NB]
                    pst = psT[i % 2]
                    nch = len(chunks_of(u))
                    if i % 2 == 0:
                        S.op("act", lambda e: e.activation(out=pt.t[:, 0:nch, :], in_=pst.t[:, 0:nch, :], func=AF.Copy),
                             reads=[pst.b], writes=[pt.b])
                    else:
                        S.op("dve", lambda e: e.tensor_copy(out=pt.t[:, 0:nch, :], in_=pst.t[:, 0:nch, :]),
                             reads=[pst.b], writes=[pt.b])

                def stage_c2(u):
                    i, h = u["i"], u["h"]
                    pt = PT[i % NB]
                    chunks = chunks_of(u)
                    nch = len(chunks)
                    for j, (off, sz, vt, ti) in enumerate(chunks):
                        S.op("pe", lambda e: e.matmul(psO.t[:, h * 64:(h + 1) * 64], lhsT=pt.t[0:sz, j, :],
                                                      rhs=vt.t[0:sz, ti, h * 64:(h + 1) * 64],
                                                      start=(j == 0), stop=(j == nch - 1)),
                             reads=[pt.b, vt.b], writes=[psO.b])

                def tail1(ti, m, col0, src_rows, dst_rows, ggt, L):
                    k2 = ti % 2
                    rs = rsum[ti % 3]
                    S.dma("sp", xr[k2].t[:], src_rows, writes=[xr[k2].b], sembuf=xr[k2].b)
                    S.op("dve", lambda e: e.reciprocal(out=rinv[k2].t[:], in_=rs.t[:]),
                         reads=[rs.b], writes=[rinv[k2].b])
                    S.op("dve", lambda e: e.tensor_tensor(
                        out=Osb[k2].t[:].rearrange("p (h d) -> p h d", h=8),
                        in0=psO.t[:].rearrange("p (h d) -> p h d", h=8),
                        in1=rinv[k2].t[:].unsqueeze(2).to_broadcast([128, 8, 64]), op=ALU.mult),
                        reads=[psO.b, rinv[k2].b], writes=[Osb[k2].b])
                    S.op("act", lambda e: e.activation(out=junk.t[:], in_=Osb[k2].t[:], func=AF.Square,
                                                       accum_out=ss1[k2].t[:]),
                         reads=[Osb[k2].b], writes=[junk.b, ss1[k2].b])
                    rstd_lnexp(ss1[k2], rstd[k2], 1.0 / 512)

                def tail1b(ti, m, col0, src_rows, dst_rows, ggt, L):
                    k2 = ti % 2
                    S.op("dve", lambda e: e.scalar_tensor_tensor(out=On[k2].t[:], in0=Osb[k2].t[:],
                                                                 scalar=rstd[k2].t[:, 0:1], in1=goa.t[:],
                                                                 op0=ALU.mult, op1=ALU.mult),
                         reads=[Osb[k2].b, rstd[k2].b, goa.b], writes=[On[k2].b])

                def tail2(ti, m, col0, src_rows, dst_rows, ggt, L):
                    k2 = ti % 2
                    pst = psT[L % 2]
                    for c in range(4):
                        S.op("pe", lambda e: e.transpose(out=pst.t[:, c, :], in_=On[k2].t[:, c * 128:(c + 1) * 128],
                                                         identity=ident.t[:]),
                             reads=[On[k2].b, ident.b], writes=[pst.b])
                    S.op("act", lambda e: e.activation(out=m.t[:, 0:4, col0:col0 + 128], in_=pst.t[:, 0:4, :], func=AF.Copy),
                         reads=[pst.b], writes=[m.b])

                def tail3(hf, ti, m, col0, src_rows, dst_rows, ggt, L):
                    k2 = ti % 2
                    for k in range(8):
                        S.op("pe", lambda e: e.matmul(psY.t[:], lhsT=m.t[:, k, col0:col0 + 128],
                                                      rhs=wo.t[:, k, hf * 512:(hf + 1) * 512],
                                                      start=(k == 0), stop=(k == 7)),
                             reads=[m.b, wo.b], writes=[psY.b])
                    S.op("act", lambda e: e.activation(out=ysb[k2].t[:, hf * 512:(hf + 1) * 512], in_=psY.t[:],
                                                       func=AF.Copy), reads=[psY.b], writes=[ysb[k2].b])
                    S.op("act", lambda e: e.activation(out=junk.t[:], in_=ysb[k2].t[:, hf * 512:(hf + 1) * 512],
                                                       func=AF.Square, accum_out=ss[k2].t[:, hf:hf + 1]),
                         reads=[ysb[k2].b], writes=[junk.b, ss[k2].b])

                def tail4(ti, m, col0, src_rows, dst_rows, ggt, L):
                    k2 = ti % 2
                    S.op("dve", lambda e: e.tensor_tensor(out=ss1[k2].t[:], in0=ss[k2].t[:, 0:1], in1=ss[k2].t[:, 1:2],
                                                          op=ALU.add), reads=[ss[k2].b], writes=[ss1[k2].b])
                    rstd_lnexp(ss1[k2], rstd[k2], 1.0 / D)

                def tail4b(ti, m, col0, src_rows, dst_rows, ggt, L):
                    k2 = ti % 2
                    S.op("dve", lambda e: e.scalar_tensor_tensor(
                        out=ysb[k2].t[:], in0=ysb[k2].t[:], scalar=rstd[k2].t[:, 0:1],
                        in1=ggt.t[:], op0=ALU.mult, op1=ALU.mult),
                        reads=[ysb[k2].b, rstd[k2].b, ggt.b], writes=[ysb[k2].b])
                    S.op("pool", lambda e: e.tensor_tensor(out=xr[k2].t[:], in0=ysb[k2].t[:], in1=xr[k2].t[:], op=ALU.add),
                         reads=[ysb[k2].b, xr[k2].b], writes=[xr[k2].b])
                    S.dma("sp", dst_rows, xr[k2].t[:], reads=[xr[k2].b], sembuf=xr[k2].b)

                units = []
                tails = {}
                groups = []
                tile_no = 0
                nbi = 0
                if not last:
                    csrc = ctx_in if l == 0 else ctxs
                    qa, qb_, m = QA[0], QB[0], mT[0]
                    groups.append((len(units),
                                   [(qa, lambda qa=qa: qa.t[0:64, :, 0:CTX], QcTd[:, 0:64, :].rearrange("c p t -> p c t")),
                                    (qb_, lambda qb_=qb_: qb_.t[64:128, :, 0:CTX], QcTd[:, 64:128, :].rearrange("c p t -> p c t"))],
                                   (m, lambda m=m: m.t[:, 4:8, 0:CTX], cncTd.rearrange("c p t -> p c t"))))
                    for ti in range(2):
                        for h in range(8):
                            pb = (h % 2) * 64
                            q = qa if h % 2 == 0 else qb_
                            units.append(dict(q=q.t[:, h // 2, ti * 128:(ti + 1) * 128], qb=q.b, pb=pb, chunk=h // 2,
                                              h=h, loc=None, edge=None, tile=tile_no))
                        tails[len(units) - 1] = (tile_no, m, ti * 128, csrc[ti * 128:(ti + 1) * 128, :],
                                                 ctxs[ti * 128:(ti + 1) * 128, :], ggt_c)
                        tile_no += 1
                xsrc = x_in if l == 0 else y_out
                for g in range(8):
                    qa, qb_, m = QA[(g + 1) % 2], QB[(g + 1) % 2], mT[(g + 1) % 2]
                    groups.append((len(units),
                                   [(qa, lambda qa=qa: qa.t[0:64, :, :],
                                     QTd[:, 0:64, g * 512:(g + 1) * 512].rearrange("c p t -> p c t")),
                                    (qb_, lambda qb_=qb_: qb_.t[64:128, :, :],
                                     QTd[:, 64:128, g * 512:(g + 1) * 512].rearrange("c p t -> p c t"))],
                                   (m, lambda m=m: m.t[:, 4:8, :], cnTd[:, :, g * 512:(g + 1) * 512].rearrange("c p t -> p c t"))))
                    for pi in range(4):
                        p = g * 4 + pi
                        if 2 <= p <= 29:
                            bs, nloc, edge = 2 * p - 4, 576, None
                        elif p < 2:
                            bs, nloc, edge = 0, 512, p
                        else:
                            bs, nloc, edge = 56, 512, p - 28
                        for h in range(8):
                            pb = (h % 2) * 64
                            q = qa if h % 2 == 0 else qb_
                            u = dict(q=q.t[:, h // 2, pi * 128:(pi + 1) * 128], qb=q.b, pb=pb, chunk=h // 2, h=h,
                                     loc=(bs * 64, nloc, bs // 2), edge=edge, tile=tile_no)
                            if edge is not None:
                                u["nbi"] = nbi
                                nbi += 1
                            units.append(u)
                        tails[len(units) - 1] = (tile_no, m, pi * 128, xsrc[p * 128:(p + 1) * 128, :],
                                                 y_out[p * 128:(p + 1) * 128, :], ggt_x)
                        tile_no += 1
                for i, u in enumerate(units):
                    u["i"] = i
                NU = len(units)
                pre = {}

                def at(step, fn):
                    pre.setdefault(max(step, 0), []).append(fn)

                def mk_load(spec):
                    tt, dst_fn, src_ap = spec
                    return lambda: S.dma("sp", dst_fn(), src_ap, writes=[tt.b], sembuf=tt.b)

                for gi_, (first, qspecs, mspec) in enumerate(groups):
                    if gi_ == 0:
                        for qs in qspecs:
                            at(0, mk_load(qs))
                        at(0, mk_load(mspec))
                    else:
                        pf = groups[gi_ - 1][0]
                        for qs in qspecs:
                            at(pf, mk_load(qs))
                        at(pf + 8, mk_load(mspec))
                for u in units:
                    if u["edge"] is not None:
                        def ld(u=u):
                            nb = nbe[u["nbi"] % 4]
                            S.dma("sp", nb.t[:], nab_edge[l, u["edge"], u["h"]], writes=[nb.b], sembuf=nb.b)
                        at(u["i"] - 2, ld)
                post = {}
                for L, targs in tails.items():
                    post.setdefault(L + 4, []).append(lambda targs=targs, L=L: tail1(*targs, L))
                    post.setdefault(L + 5, []).append(lambda targs=targs, L=L: tail1b(*targs, L))
                    post.setdefault(L + 6, []).append(lambda targs=targs, L=L: tail2(*targs, L))
                    post.setdefault(L + 7, []).append(lambda targs=targs, L=L: tail3(0, *targs, L))
                    post.setdefault(L + 8, []).append(lambda targs=targs, L=L: tail3(1, *targs, L))
                    post.setdefault(L + 10, []).append(lambda targs=targs, L=L: tail4(*targs, L))
                    post.setdefault(L + 11, []).append(lambda targs=targs, L=L: (tail4b(*targs, L), conv_step(2)))
                init_loads_first()
                for st in range(NU + 12):
                    for fn in pre.get(st, []):
                        fn()
                    if st == 0:
                        init_loads_rest()
                    if 0 <= st - 4 < NU:
                        stage_c1b(units[st - 4])
                    if st < NU:
                        stage_a(units[st])
                    if 0 <= st - 1 < NU:
                        stage_b1(units[st - 1])
                    if 0 <= st - 2 < NU:
                        stage_b2(units[st - 2])
                    if 0 <= st - 3 < NU:
                        stage_c1(units[st - 3])
                    if 0 <= st - 4 < NU:
                        stage_c2(units[st - 4])
                    for fn in post.get(st, []):
                        fn()
                S.end_phase()

        def phase_f(l, last):
            es = ExitStack()
            with es:
                gsT = alloc(es, "fgs", [128, D], F32, dma=True)
                shT = alloc(es, "fsh", [128, D], F32, dma=True)
                ggt_x = load_bc(es, "fggt_x", 5)
                ggt_c = load_bc(es, "fggt_c", 6 + 5) if not last else None

                def load_rows(base):
                    S.dma("sp", gsT.t[:], _bc_rows(mods, base + 4, D), writes=[gsT.b], sembuf=gsT.b)
                    S.dma("sp", shT.t[:], _bc_rows(mods, base + 3, D), writes=[shT.b], sembuf=shT.b)

                tiles = []
                if not last:
                    for t in range(CTX // 128):
                        tiles.append(dict(rows=ctxs[t * 128:(t + 1) * 128, :], ctx=True))
                for p in range(S_LEN // 128):
                    tiles.append(dict(rows=y_out[p * 128:(p + 1) * 128, :], ctx=False))
                ngr = 4
                base_n, extra = len(tiles) // ngr, len(tiles) % ngr
                groups = []
                pos = 0
                for k in range(ngr):
                    n = base_n + (1 if k < extra else 0)
                    groups.append(dict(k=k, tiles=tiles[pos:pos + n]))
                    pos += n
                NTM = max(len(g["tiles"]) for g in groups)
                TM = NTM * 128
                for g in groups:
                    nt = len(g["tiles"])
                    g["nt"] = nt
                    g["splits"] = [(0, 512), (512, 512)] if nt == 8 else [(i * 384, 384) for i in range(nt * 128 // 384)]
                    assert sum(w for _, w in g["splits"]) == nt * 128
                    bl = []
                    for i, td in enumerate(g["tiles"]):
                        if bl and len(bl[-1]) < 4 and g["tiles"][bl[-1][0]]["ctx"] == td["ctx"]:
                            bl[-1].append(i)
                        else:
                            bl.append([i])
                    g["batches"] = bl

                xts = allocn(es, 4, "fxt", [128, D], F32, dma=True)
                junk = alloc(es, "fjunk", [128, D], BF16)
                tmp = allocn(es, 2, "ftmp", [128, D], F32)
                hb = allocn(es, 4, "fhb", [128, D], BF16)
                ss4 = allocn(es, 2, "fss4", [128, 4], F32)
                rs4 = allocn(es, 2, "frs4", [128, 4], F32)
                sse = alloc(es, "fsse", [128, NTM], F32)
                rse = alloc(es, "frse", [128, NTM], F32)
                hT = allocn(es, 2, "fhT", [128, 8, TM], BF16)
                tps = allocn(es, 2, "ftp", [128, 8, 128], BF16, psum=True)
                psG = allocn(es, 2, "psG", [128, 512], F32, psum=True)
                psU = allocn(es, 2, "psU", [128, 512], F32, psum=True)
                psD = allocn(es, 2, "psD", [128, 512], F32, psum=True)
                wg = allocn(es, 2, "wg", [128, 8, 512], BF16, dma="pool")
                wu = allocn(es, 2, "wu", [128, 8, 512], BF16, dma="pool")
                wd = allocn(es, 2, "wd", [128, 4, D], BF16, dma="pool")
                sg = allocn(es, 2, "sg", [128, 512], F32)
                aT = allocn(es, 2, "aT", [128, 4, TM], BF16)
                acc = alloc(es, "facc", [128, NTM, D], F32)
                cnt = dict(x=0, h=0, b=0, w=0, gu=0, d=0, a=0, t=0)
                cur_kind = [None]

                def load_batch(g, bi):
                    xl = []
                    for i in g["batches"][bi]:
                        xt = xts[cnt["x"] % 4]
                        cnt["x"] += 1
                        xl.append(xt)
                        S.dma("sp", xt.t[:], g["tiles"][i]["rows"], writes=[xt.b], sembuf=xt.b)
                    g.setdefault("xl", {})[bi] = xl

                def norm_batch(g, bi):
                    idx = g["batches"][bi]
                    kind = g["tiles"][idx[0]]["ctx"]
                    if cur_kind[0] != kind:
                        load_rows(6 if kind else 0)
                        cur_kind[0] = kind
                    kb = cnt["b"] % 2
                    cnt["b"] += 1
                    ssb, rsb = ss4[kb], rs4[kb]
                    xl = g["xl"][bi]
                    for j, i in enumerate(idx):
                        xt = xl[j]
                        S.op("act", lambda e: e.activation(out=junk.t[:], in_=xt.t[:], func=AF.Square,
                                                           accum_out=ssb.t[:, j:j + 1]),
                             reads=[xt.b], writes=[junk.b, ssb.b])
                    nbt = len(idx)
                    S.op("act", lambda e: e.activation(out=rsb.t[:, 0:nbt], in_=ssb.t[:, 0:nbt], func=AF.Sqrt,
                                                       scale=1.0 / D, bias=epsb.t[:]),
                         reads=[ssb.b, epsb.b], writes=[rsb.b])
                    S.op("dve", lambda e: e.reciprocal(out=rsb.t[:, 0:nbt], in_=rsb.t[:, 0:nbt]),
                         reads=[rsb.b], writes=[rsb.b])
                    hl = []
                    for j, i in enumerate(idx):
                        xt = xl[j]
                        tm = tmp[j % 2]
                        h_ = hb[cnt["h"] % 4]
                        cnt["h"] += 1
                        hl.append((i, h_))
                        S.op("dve", lambda e: e.scalar_tensor_tensor(out=tm.t[:], in0=xt.t[:], scalar=rsb.t[:, j:j + 1],
                                                                     in1=gsT.t[:], op0=ALU.mult, op1=ALU.mult),
                             reads=[xt.b, rsb.b, gsT.b], writes=[tm.b])
                        S.op("pool", lambda e: e.tensor_tensor(out=h_.t[:], in0=tm.t[:], in1=shT.t[:], op=ALU.add),
                             reads=[tm.b, shT.b], writes=[h_.b])
                    g.setdefault("hl", {})[bi] = hl

                def trans_batch(g, bi):
                    h = hT[g["k"] % 2]
                    for (i, h_) in g["hl"][bi]:
                        tp = tps[cnt["t"] % 2]
                        cnt["t"] += 1
                        for c in range(8):
                            S.op("pe", lambda e: e.transpose(out=tp.t[:, c, :], in_=h_.t[:, c * 128:(c + 1) * 128],
                                                             identity=ident.t[:]),
                                 reads=[h_.b, ident.b], writes=[tp.b])
                        S.op("act", lambda e: e.activation(out=h.t[:, :, i * 128:(i + 1) * 128], in_=tp.t[:],
                                                           func=AF.Copy), reads=[tp.b], writes=[h.b])

                def epilogue(g):
                    nt = g["nt"]
                    for i in range(nt):
                        S.op("act", lambda e: e.activation(out=junk.t[:], in_=acc.t[:, i, :], func=AF.Square,
                                                           accum_out=sse.t[:, i:i + 1]),
                             reads=[acc.b], writes=[junk.b, sse.b])
                    S.op("act", lambda e: e.activation(out=rse.t[:, 0:nt], in_=sse.t[:, 0:nt], func=AF.Sqrt,
                                                       scale=1.0 / D, bias=epsb.t[:]),
                         reads=[sse.b, epsb.b], writes=[rse.b])
                    S.op("dve", lambda e: e.reciprocal(out=rse.t[:, 0:nt], in_=rse.t[:, 0:nt]),
                         reads=[rse.b], writes=[rse.b])

                    def issue(i):
                        xt = xts[cnt["x"] % 4]
                        cnt["x"] += 1
                        S.dma("sp", xt.t[:], g["tiles"][i]["rows"], writes=[xt.b], sembuf=xt.b)
                        return xt
                    q = [issue(i) for i in range(min(3, nt))]
                    for i in range(nt):
                        xt = q.pop(0)
                        tm = tmp[i % 2]
                        ggt = ggt_c if g["tiles"][i]["ctx"] else ggt_x
                        S.op("dve", lambda e: e.scalar_tensor_tensor(out=tm.t[:], in0=acc.t[:, i, :],
                                                                     scalar=rse.t[:, i:i + 1], in1=ggt.t[:],
                                                                     op0=ALU.mult, op1=ALU.mult),
                             reads=[acc.b, rse.b, ggt.b], writes=[tm.b])
                        S.op("pool", lambda e: e.tensor_tensor(out=xt.t[:], in0=tm.t[:], in1=xt.t[:], op=ALU.add),
                             reads=[tm.b, xt.b], writes=[xt.b])
                        S.dma("sp", g["tiles"][i]["rows"], xt.t[:], reads=[xt.b], sembuf=xt.b)
                        if i + 3 < nt:
                            q.append(issue(i + 3))

                wgu, wdn = w_gu_d[l // 2], w_dn_d[l // 2]

                def wload(c):
                    kw = cnt["w"] % 2
                    cnt["w"] += 1
                    S.dma("pool", wg[kw].t[:], wgu[:, c * 512:(c + 1) * 512].rearrange("(c p) f -> p c f", p=128),
                          writes=[wg[kw].b], sembuf=wg[kw].b)
                    S.dma("pool", wu[kw].t[:],
                          wgu[:, FF + c * 512:FF + (c + 1) * 512].rearrange("(c p) f -> p c f", p=128),
                          writes=[wu[kw].b], sembuf=wu[kw].b)
                    S.dma("pool", wd[kw].t[:], wdn[c * 512:(c + 1) * 512, :].rearrange("(j p) d -> p j d", p=128),
                          writes=[wd[kw].b], sembuf=wd[kw].b)
                    return kw

                def gu_block(g, kw, a, j, c0, cwd):
                    h = hT[g["k"] % 2]
                    kk = cnt["gu"] % 2
                    cnt["gu"] += 1
                    pg, pu, sgt = psG[kk], psU[kk], sg[kk]
                    for ch in range(8):
                        S.op("pe", lambda e: e.matmul(pg.t[:, 0:cwd], lhsT=wg[kw].t[:, ch, j * 128:(j + 1) * 128],
                                                      rhs=h.t[:, ch, c0:c0 + cwd],
                                                      start=(ch == 0), stop=(ch == 7)),
                             reads=[wg[kw].b, h.b], writes=[pg.b])
                    for ch in range(8):
                        S.op("pe", lambda e: e.matmul(pu.t[:, 0:cwd], lhsT=wu[kw].t[:, ch, j * 128:(j + 1) * 128],
                                                      rhs=h.t[:, ch, c0:c0 + cwd],
                                                      start=(ch == 0), stop=(ch == 7)),
                             reads=[wu[kw].b, h.b], writes=[pu.b])
                    S.op("act", lambda e: e.activation(out=sgt.t[:, 0:cwd], in_=pg.t[:, 0:cwd], func=AF.Silu),
                         reads=[pg.b], writes=[sgt.b])
                    S.op("dve", lambda e: e.tensor_tensor(out=a.t[:, j, c0:c0 + cwd],
                                                          in0=pu.t[:, 0:cwd], in1=sgt.t[:, 0:cwd], op=ALU.mult),
                         reads=[pu.b, sgt.b], writes=[a.b])

                def down(g, c, kw, a):
                    for i in range(g["nt"]):
                        for dh in range(2):
                            pd = psD[cnt["d"] % 2]
                            cnt["d"] += 1
                            for j in range(4):
                                S.op("pe", lambda e: e.matmul(pd.t[:], lhsT=a.t[:, j, i * 128:(i + 1) * 128],
                                                              rhs=wd[kw].t[:, j, dh * 512:(dh + 1) * 512],
                                                              start=(j == 0), stop=(j == 3)),
                                     reads=[a.b, wd[kw].b], writes=[pd.b])
                            av = acc.t[:, i, dh * 512:(dh + 1) * 512]
                            if c == 0:
                                S.op("dve", lambda e: e.tensor_copy(out=av, in_=pd.t[:]), reads=[pd.b], writes=[acc.b])
                            else:
                                S.op("dve", lambda e: e.tensor_tensor(out=av, in0=pd.t[:], in1=av, op=ALU.add),
                                     reads=[pd.b, acc.b], writes=[acc.b])

                g0 = groups[0]
                for bi in range(len(g0["batches"])):
                    load_batch(g0, bi)
                    norm_batch(g0, bi)
                    trans_batch(g0, bi)
                pend_d = None
                NCH = 7 * len(groups)
                kw_of = {0: wload(0), 1: wload(1)}
                for k, g in enumerate(groups):
                    nxt = groups[k + 1] if k + 1 < len(groups) else None
                    nbn = len(nxt["batches"]) if nxt is not None else 0
                    for c in range(7):
                        t = 7 * k + c
                        kw = kw_of[t]
                        a = aT[cnt["a"] % 2]
                        cnt["a"] += 1
                        blocks = [(j, c0, cwd) for j in range(4) for (c0, cwd) in g["splits"]]
                        gu_block(g, kw, a, *blocks[0])
                        if pend_d is not None:
                            down(*pend_d)
                            if t + 1 < NCH:
                                kw_of[t + 1] = wload((t + 1) % 7)
                        conv_step(1)
                        if c == 0 and k > 0:
                            epilogue(groups[k - 1])
                        if 0 <= c - 2 < nbn:
                            trans_batch(nxt, c - 2)
                        for blk in blocks[1:]:
                            gu_block(g, kw, a, *blk)
                        if 0 <= c - 1 < nbn:
                            norm_batch(nxt, c - 1)
                        if c < nbn:
                            load_batch(nxt, c)
                        pend_d = (g, c, kw, a)
                down(*pend_d)
                epilogue(groups[-1])
                S.end_phase()


        def phase_moe(l):
            es0 = ExitStack()
            with es0:
                slot = alloc(es0, "slot", [128, 32, 2], U32)
                gates = alloc(es0, "gates", [128, 32, 2], F32)
                idxw = alloc(es0, "idxw", [128, NSEG * 7], U32)
                es = ExitStack()
                with es:
                    gs_x = load_bc(es, "mgs_x", 4)
                    sh_x = load_bc(es, "msh_x", 3)
                    hb_all = alloc(es, "hb_all", [128, 32, D], BF16)
                    xts = allocn(es, 4, "mxt", [128, D], F32, dma=True)
                    junk = alloc(es, "mjunk", [128, D], BF16)
                    tmp = allocn(es, 2, "mtmp", [128, D], F32)
                    hb32 = allocn(es, 3, "mhb32", [128, D], F32)
                    sss = allocn(es, 2, "mss", [128, 1], F32)
                    rstds = allocn(es, 2, "mrstd", [128, 1], F32)
                    tp32 = allocn(es, 2, "mtp32", [128, 4, 128], F32, psum=True)
                    pl = allocn(es, 2, "mpl", [128, 512], F32, psum=True)
                    hT32 = allocn(es, 2, "mhT32", [128, 8, 128], F32)
                    wr = alloc(es, "mwr", [128, 8, NE], F32, dma=True)
                    S.dma("sp", wr.t[:], w_rt, writes=[wr.b], sembuf=wr.b)
                    M1 = alloc(es, "M1", [128, 32, NE], F32)
                    M2 = alloc(es, "M2", [128, 32, NE], F32)
                    M12 = allocn(es, 2, "M12", [128, NE], F32)
                    rt = alloc(es, "rt", [128, 32, 2 * NE], F32)
                    cum = alloc(es, "cum", [128, 32, NE], F32)
                    rank = alloc(es, "rank", [128, 32, NE], F32)
                    lg = allocn(es, 2, "lg", [128, NE], F32)
                    l2 = allocn(es, 2, "l2", [128, NE], F32)
                    m1 = allocn(es, 2, "m1", [128, 1], F32)
                    m2 = allocn(es, 2, "m2", [128, 1], F32)
                    dd = alloc(es, "dd", [128, 32], F32)
                    scb = S.buf(dma="pool")
                    V_ = lambda fn, r, w: S.op("dve", fn, reads=r, writes=w)
                    G_ = lambda fn, r, w: S.op("pool", fn, reads=r, writes=w)
                    NT = 32

                    def r_load(i):
                        xt = xts[i % 4]
                        S.dma("sp", xt.t[:], y_out[i * 128:(i + 1) * 128, :], writes=[xt.b], sembuf=xt.b)

                    def r_norm(i):
                        k2 = i % 2
                        xt = xts[i % 4]
                        h32 = hb32[i % 3]
                        S.op("act", lambda e: e.activation(out=junk.t[:], in_=xt.t[:], func=AF.Square,
                                                           accum_out=sss[k2].t[:]),
                             reads=[xt.b], writes=[junk.b, sss[k2].b])
                        rstd_from_ss(sss[k2], rstds[k2], 1.0 / D)
                        V_(lambda e: e.scalar_tensor_tensor(out=tmp[k2].t[:], in0=xt.t[:], scalar=rstds[k2].t[:, 0:1],
                                                            in1=gs_x.t[:], op0=ALU.mult, op1=ALU.mult),
                           [xt.b, rstds[k2].b, gs_x.b], [tmp[k2].b])
                        G_(lambda e: e.tensor_tensor(out=h32.t[:], in0=tmp[k2].t[:], in1=sh_x.t[:], op=ALU.add),
                           [tmp[k2].b, sh_x.b], [h32.b])
                        S.op("act", lambda e: e.activation(out=hb_all.t[:, i, :], in_=h32.t[:], func=AF.Copy),
                             reads=[h32.b], writes=[hb_all.b])

                    def r_trans(i):
                        k2 = i % 2
                        h32 = hb32[i % 3]
                        for half in range(2):
                            tpp = tp32[half]
                            for c in range(4):
                                cc = half * 4 + c
                                S.op("pe", lambda e: e.transpose(out=tpp.t[:, c, :], in_=h32.t[:, cc * 128:(cc + 1) * 128],
                                                                 identity=ident32.t[:]),
                                     reads=[h32.b, ident32.b], writes=[tpp.b])
                            S.op("act", lambda e: e.activation(out=hT32[k2].t[:, half * 4:half * 4 + 4, :], in_=tpp.t[:],
                                                               func=AF.Copy), reads=[tpp.b], writes=[hT32[k2].b])

                    def r_route(i):
                        k2 = i % 2
                        p = pl[k2]
                        for c in range(8):
                            S.op("pe", lambda e: e.matmul(p.t[:, 0:NE], lhsT=hT32[k2].t[:, c, :], rhs=wr.t[:, c, :],
                                                          start=(c == 0), stop=(c == 7)),
                                 reads=[hT32[k2].b, wr.b], writes=[p.b])
                        lgk, l2k, m1k, m2k = lg[k2], l2[k2], m1[k2], m2[k2]
                        V_(lambda e: e.tensor_copy(out=lgk.t[:], in_=p.t[:, 0:NE]), [p.b], [lgk.b])
                        V_(lambda e: e.reduce_max(out=m1k.t[:], in_=lgk.t[:], axis=AX.X), [lgk.b], [m1k.b])
                        V_(lambda e: e.tensor_scalar(out=M1.t[:, i, :], in0=lgk.t[:], scalar1=m1k.t[:, 0:1], scalar2=None,
                                                     op0=ALU.is_ge), [lgk.b, m1k.b], [M1.b])
                        V_(lambda e: e.scalar_tensor_tensor(out=l2k.t[:], in0=M1.t[:, i, :], scalar=-1e30, in1=lgk.t[:],
                                                            op0=ALU.mult, op1=ALU.add), [M1.b, lgk.b], [l2k.b])
                        V_(lambda e: e.reduce_max(out=m2k.t[:], in_=l2k.t[:], axis=AX.X), [l2k.b], [m2k.b])
                        V_(lambda e: e.tensor_scalar(out=M2.t[:, i, :], in0=l2k.t[:], scalar1=m2k.t[:, 0:1], scalar2=None,
                                                     op0=ALU.is_ge), [l2k.b, m2k.b], [M2.b])
                        V_(lambda e: e.tensor_tensor(out=dd.t[:, i:i + 1], in0=m1k.t[:], in1=m2k.t[:], op=ALU.subtract),
                           [m1k.b, m2k.b], [dd.b])
                        mk = M12[k2]
                        V_(lambda e: e.tensor_tensor(out=mk.t[:], in0=M1.t[:, i, :], in1=M2.t[:, i, :], op=ALU.add),
                           [M1.b, M2.b], [mk.b])
                        S.op("pe", lambda e: e.matmul(p.t[:, 64:64 + NE], lhsT=tri32.t[:], rhs=mk.t[:], start=True, stop=True),
                             reads=[tri32.b, mk.b], writes=[p.b])
                        S.op("pe", lambda e: e.matmul(p.t[:, 64 + NE:64 + 2 * NE], lhsT=ones32.t[:], rhs=mk.t[:],
                                                      start=True, stop=True),
                             reads=[ones32.b, mk.b], writes=[p.b])
                        V_(lambda e: e.tensor_copy(out=rt.t[:, i, :], in_=p.t[:, 64:64 + 2 * NE]), [p.b], [rt.b])

                    for i in range(min(3, NT)):
                        r_load(i)
                    for st in range(NT + 2):
                        if st + 3 < NT:
                            r_load(st + 3)
                        if st < NT:
                            r_norm(st)
                        if 0 <= st - 1 < NT:
                            r_trans(st - 1)
                        if 0 <= st - 2 < NT:
                            r_route(st - 2)
                    S.op("act", lambda e: e.activation(out=gates.t[:, :, 0], in_=dd.t[:, :], func=AF.Sigmoid),
                         reads=[dd.b], writes=[gates.b])
                    V_(lambda e: e.tensor_scalar(out=gates.t[:, :, 1], in0=gates.t[:, :, 0], scalar1=-1.0, scalar2=1.0,
                                                 op0=ALU.mult, op1=ALU.add), [gates.b], [gates.b])
                    G_(lambda e: e.memset(cum.t[:, 0, :], 0.0), [], [cum.b])
                    for i in range(1, NT):
                        V_(lambda e: e.tensor_tensor(out=cum.t[:, i, :], in0=cum.t[:, i - 1, :], in1=rt.t[:, i - 1, NE:2 * NE],
                                                     op=ALU.add), [cum.b, rt.b], [cum.b])
                    V_(lambda e: e.tensor_tensor(out=rank.t[:], in0=rt.t[:, :, 0:NE], in1=cum.t[:], op=ALU.add),
                       [rt.b, cum.b], [rank.b])
                    nb = alloc(es, "nb", [128, NE], F32)
                    pad = alloc(es, "pad", [128, NE], F32)
                    pend = alloc(es, "pend", [128, NE], F32)
                    off = alloc(es, "off", [128, NE], F32)
                    V_(lambda e: e.tensor_tensor(out=nb.t[:], in0=cum.t[:, NT - 1, :], in1=rt.t[:, NT - 1, NE:2 * NE],
                                                 op=ALU.add), [cum.b, rt.b], [nb.b])
                    cmp0 = alloc(es, "cmp0", [128, NE], F32)
                    V_(lambda e: e.tensor_single_scalar(out=pad.t[:], in_=nb.t[:], scalar=0.0, op=ALU.is_gt), [nb.b], [pad.b])
                    for kq in range(1, 8):
                        V_(lambda e: e.tensor_single_scalar(out=cmp0.t[:], in_=nb.t[:], scalar=float(512 * kq), op=ALU.is_gt),
                           [nb.b], [cmp0.b])
                        V_(lambda e: e.tensor_tensor(out=pad.t[:], in0=pad.t[:], in1=cmp0.t[:], op=ALU.add),
                           [pad.b, cmp0.b], [pad.b])
                    V_(lambda e: e.tensor_single_scalar(out=pad.t[:], in_=pad.t[:], scalar=512.0, op=ALU.mult), [pad.b], [pad.b])
                    V_(lambda e: e.tensor_copy(out=pend.t[:, 0:1], in_=pad.t[:, 0:1]), [pad.b], [pend.b])
                    for ex in range(1, NE):
                        V_(lambda e: e.tensor_tensor(out=pend.t[:, ex:ex + 1], in0=pend.t[:, ex - 1:ex], in1=pad.t[:, ex:ex + 1],
                                                     op=ALU.add), [pend.b, pad.b], [pend.b])
                    V_(lambda e: e.tensor_tensor(out=off.t[:], in0=pend.t[:], in1=pad.t[:], op=ALU.subtract),
                       [pend.b, pad.b], [off.b])
                    V_(lambda e: e.tensor_tensor(out=rank.t[:], in0=rank.t[:],
                                                 in1=off.t[:].unsqueeze(1).to_broadcast([128, 32, NE]), op=ALU.add),
                       [rank.b, off.b], [rank.b])
                    prod = alloc(es, "prod", [128, 32, NE], F32)
                    slotf = alloc(es, "slotf", [128, 32, 2], F32)
                    for k, Mk in enumerate((M1, M2)):
                        V_(lambda e: e.tensor_tensor(out=prod.t[:], in0=rank.t[:], in1=Mk.t[:], op=ALU.mult),
                           [rank.b, Mk.b], [prod.b])
                        V_(lambda e: e.reduce_sum(out=slotf.t[:, :, k], in_=prod.t[:], axis=AX.X), [prod.b], [slotf.b])
                    V_(lambda e: e.tensor_copy(out=slot.t[:], in_=slotf.t[:]), [slotf.b], [slot.b])
                    eseg = alloc(es, "eseg", [128, NSEG], F32)
                    cmpt = alloc(es, "cmpt", [128, NE], F32)
                    pci = alloc(es, "pci", [128, 7], I32)
                    pcf = alloc(es, "pcf", [128, 7], F32)
                    idxf = alloc(es, "idxf", [128, NSEG, 7], F32)
                    for sg_ in range(NSEG):
                        V_(lambda e: e.tensor_single_scalar(out=cmpt.t[:], in_=pend.t[:], scalar=float(512 * sg_), op=ALU.is_le),
                           [pend.b], [cmpt.b])
                        V_(lambda e: e.reduce_sum(out=eseg.t[:, sg_:sg_ + 1], in_=cmpt.t[:], axis=AX.X), [cmpt.b], [eseg.b])
                    V_(lambda e: e.tensor_scalar(out=eseg.t[:], in0=eseg.t[:], scalar1=float(NE - 1), scalar2=896.0,
                                                 op0=ALU.min, op1=ALU.mult), [eseg.b], [eseg.b])
                    G_(lambda e: e.iota(pci.t[:], pattern=[[128, 7]], base=0, channel_multiplier=1), [], [pci.b])
                    V_(lambda e: e.tensor_copy(out=pcf.t[:], in_=pci.t[:]), [pci.b], [pcf.b])
                    for sg_ in range(NSEG):
                        V_(lambda e: e.tensor_scalar(out=idxf.t[:, sg_, :], in0=pcf.t[:], scalar1=eseg.t[:, sg_:sg_ + 1],
                                                     scalar2=None, op0=ALU.add), [pcf.b, eseg.b], [idxf.b])
                    V_(lambda e: e.tensor_copy(out=idxw.t[:], in_=idxf.t[:].rearrange("p s c -> p (s c)")), [idxf.b], [idxw.b])
                    for i in range(32):
                        for k in range(2):
                            S.dma_fn("pool", lambda e: e.indirect_dma_start(
                                out=hs, out_offset=bass.IndirectOffsetOnAxis(ap=slot.t[:, i, k:k + 1], axis=0),
                                in_=hb_all.t[:, i, :], in_offset=None),
                                reads=[slot.b, hb_all.b, hsz], sembuf=scb)
                    if dbg is not None and dbg.get("moe_dump"):
                        S.dma("sp", dbg_out[:, 0:64], slot.t[:].rearrange("p i k -> p (i k)").bitcast(F32), reads=[slot.b], sembuf=xts[0].b)
                        S.dma("sp", dbg_out[:, 64:128], gates.t[:].rearrange("p i k -> p (i k)"), reads=[gates.b], sembuf=xts[0].b)
                        S.dma("sp", dbg_out[:, 128:128 + NSEG * 7], idxw.t[:].bitcast(F32), reads=[idxw.b], sembuf=xts[0].b)
                    S.end_phase()
                conv_step(10 ** 6)
                es = ExitStack()
                with es:
                    hsl = allocn(es, 4, "hsl", [128, D], BF16, dma=True)
                    hT = allocn(es, 2, "shT", [128, 8, 512], BF16)
                    tp = allocn(es, 2, "stp", [128, 8, 128], BF16, psum=True)
                    psG = allocn(es, 2, "spsG", [128, 512], F32, psum=True)
                    psU = allocn(es, 2, "spsU", [128, 512], F32, psum=True)
                    psD = allocn(es, 2, "spsD", [128, 512], F32, psum=True)
                    wg = allocn(es, 2, "swg", [128, 8, 512], BF16, dma="pool")
                    wu = allocn(es, 2, "swu", [128, 8, 512], BF16, dma="pool")
                    wd = allocn(es, 2, "swd", [128, 4, D], BF16, dma="pool")
                    sg = allocn(es, 2, "ssg", [128, 512], F32)
                    aT = allocn(es, 2, "saT", [128, 4, 512], BF16)
                    acc = allocn(es, 2, "sacc", [128, 4, D], F32, dma=True)
                    cnt = dict(x=0, w=0, gu=0, d=0, a=0)

                    def prep_load(seg):
                        for i in range(4):
                            hl = hsl[i]
                            S.dma("sp", hl.t[:], hs[seg * 512 + i * 128:seg * 512 + (i + 1) * 128, :], writes=[hl.b], sembuf=hl.b)

                    def prep_trans(seg):
                        h = hT[seg % 2]
                        for i in range(4):
                            hl = hsl[i]
                            t = tp[i % 2]
                            for c in range(8):
                                S.op("pe", lambda e: e.transpose(out=t.t[:, c, :], in_=hl.t[:, c * 128:(c + 1) * 128],
                                                                 identity=ident.t[:]),
                                     reads=[hl.b, ident.b], writes=[t.b])
                            S.op("act", lambda e: e.activation(out=h.t[:, :, i * 128:(i + 1) * 128], in_=t.t[:], func=AF.Copy),
                                 reads=[t.b], writes=[h.b])

                    def wload(seg, c):
                        k = cnt["w"] % 2
                        cnt["w"] += 1
                        ia = idxw.t[:, seg * 7 + c:seg * 7 + c + 1]
                        for (dst, src) in ((wg[k], WGd), (wu[k], WUd), (wd[k], WDd)):
                            S.dma_fn("pool", lambda e: e.indirect_dma_start(
                                out=dst.t[:].rearrange("p a b -> p (a b)"), out_offset=None, in_=src,
                                in_offset=bass.IndirectOffsetOnAxis(ap=ia, axis=0)),
                                reads=[idxw.b, convb], writes=[dst.b], sembuf=dst.b)
                        return k

                    def gu(seg, k, a, j):
                        h = hT[seg % 2]
                        kk = cnt["gu"] % 2
                        cnt["gu"] += 1
                        pg, pu, sgt = psG[kk], psU[kk], sg[kk]
                        for ch in range(8):
                            S.op("pe", lambda e: e.matmul(pg.t[:], lhsT=wg[k].t[:, ch, j * 128:(j + 1) * 128],
                                                          rhs=h.t[:, ch, :], start=(ch == 0), stop=(ch == 7)),
                                 reads=[wg[k].b, h.b], writes=[pg.b])
                        for ch in range(8):
                            S.op("pe", lambda e: e.matmul(pu.t[:], lhsT=wu[k].t[:, ch, j * 128:(j + 1) * 128],
                                                          rhs=h.t[:, ch, :], start=(ch == 0), stop=(ch == 7)),
                                 reads=[wu[k].b, h.b], writes=[pu.b])
                        S.op("act", lambda e: e.activation(out=sgt.t[:], in_=pg.t[:], func=AF.Silu),
                             reads=[pg.b], writes=[sgt.b])
                        S.op("dve", lambda e: e.tensor_tensor(out=a.t[:, j, :], in0=pu.t[:], in1=sgt.t[:], op=ALU.mult),
                             reads=[pu.b, sgt.b], writes=[a.b])

                    def down(seg, c, k, a):
                        ac = acc[seg % 2]
                        for i in range(4):
                            for dh in range(2):
                                pd = psD[cnt["d"] % 2]
                                cnt["d"] += 1
                                for j in range(4):
                                    S.op("pe", lambda e: e.matmul(pd.t[:], lhsT=a.t[:, j, i * 128:(i + 1) * 128],
                                                                  rhs=wd[k].t[:, j, dh * 512:(dh + 1) * 512],
                                                                  start=(j == 0), stop=(j == 3)),
                                         reads=[a.b, wd[k].b], writes=[pd.b])
                                av = ac.t[:, i, dh * 512:(dh + 1) * 512]
                                if c == 0:
                                    S.op("dve", lambda e: e.tensor_copy(out=av, in_=pd.t[:]), reads=[pd.b], writes=[ac.b])
                                else:
                                    S.op("dve", lambda e: e.tensor_tensor(out=av, in0=pd.t[:], in1=av, op=ALU.add),
                                         reads=[pd.b, ac.b], writes=[ac.b])
                        if c == 6:
                            S.dma("sp", ys[seg * 512:(seg + 1) * 512, :].rearrange("(i p) d -> p i d", p=128), ac.t[:],
                                  reads=[ac.b], sembuf=ac.b)

                    prep_load(0)
                    prep_trans(0)
                    pend_d = None
                    for seg in range(NSEG):
                        for c in range(7):
                            k = wload(seg, c)
                            a = aT[cnt["a"] % 2]
                            cnt["a"] += 1
                            gu(seg, k, a, 0)
                            if pend_d is not None:
                                down(*pend_d)
                            for j in range(1, 4):
                                gu(seg, k, a, j)
                            pend_d = (seg, c, k, a)
                            if seg + 1 < NSEG:
                                if c == 1:
                                    prep_load(seg + 1)
                                if c == 4:
                                    prep_trans(seg + 1)
                    down(*pend_d)
                    S.end_phase()
                es = ExitStack()
                with es:
                    ggt_x = load_bc(es, "cggt_x", 5)
                    NBUF = 4
                    r1 = allocn(es, NBUF, "r1", [128, D], F32, dma="pool")
                    r2 = allocn(es, NBUF, "r2", [128, D], F32, dma="pool")
                    xts = allocn(es, NBUF, "cxt", [128, D], F32, dma=True)
                    a1 = allocn(es, 2, "a1", [128, D], F32)
                    junk = alloc(es, "cjunk", [128, D], BF16)
                    sss = allocn(es, 2, "css", [128, 1], F32)
                    rstds = allocn(es, 2, "crstd", [128, 1], F32)
                    tmp = allocn(es, 2, "ctmp", [128, D], F32)

                    def c_issue(i):
                        kb = i % NBUF
                        for (r, k) in ((r1[kb], 0), (r2[kb], 1)):
                            S.dma_fn("pool", lambda e: e.indirect_dma_start(
                                out=r.t[:], out_offset=None, in_=ys,
                                in_offset=bass.IndirectOffsetOnAxis(ap=slot.t[:, i, k:k + 1], axis=0)),
                                reads=[slot.b], writes=[r.b], sembuf=r.b)
                        xt = xts[kb]
                        S.dma("sp", xt.t[:], y_out[i * 128:(i + 1) * 128, :], writes=[xt.b], sembuf=xt.b)

                    for i in range(NBUF - 1):
                        c_issue(i)
                    for i in range(32):
                        if i + NBUF - 1 < 32:
                            c_issue(i + NBUF - 1)
                        k2 = i % 2
                        kb = i % NBUF
                        xt = xts[kb]
                        S.op("act", lambda e: e.activation(out=a1[k2].t[:], in_=r1[kb].t[:], func=AF.Copy,
                                                           scale=gates.t[:, i, 0:1]),
                             reads=[r1[kb].b, gates.b], writes=[a1[k2].b])
                        S.op("dve", lambda e: e.scalar_tensor_tensor(out=a1[k2].t[:], in0=r2[kb].t[:], scalar=gates.t[:, i, 1:2],
                                                                     in1=a1[k2].t[:], op0=ALU.mult, op1=ALU.add),
                             reads=[r2[kb].b, gates.b, a1[k2].b], writes=[a1[k2].b])
                        S.op("act", lambda e: e.activation(out=junk.t[:], in_=a1[k2].t[:], func=AF.Square,
                                                           accum_out=sss[k2].t[:]),
                             reads=[a1[k2].b], writes=[junk.b, sss[k2].b])
                        rstd_from_ss(sss[k2], rstds[k2], 1.0 / D)
                        S.op("dve", lambda e: e.scalar_tensor_tensor(out=tmp[k2].t[:], in0=a1[k2].t[:],
                                                                     scalar=rstds[k2].t[:, 0:1], in1=ggt_x.t[:],
                                                                     op0=ALU.mult, op1=ALU.mult),
                             reads=[a1[k2].b, rstds[k2].b, ggt_x.b], writes=[tmp[k2].b])
                        S.op("pool", lambda e: e.tensor_tensor(out=xt.t[:], in0=tmp[k2].t[:], in1=xt.t[:], op=ALU.add),
                             reads=[tmp[k2].b, xt.b], writes=[xt.b])
                        S.dma("sp", y_out[i * 128:(i + 1) * 128, :], xt.t[:], reads=[xt.b], sembuf=xt.b)
                    S.end_phase()

        stop_after = dbg.get("stop") if dbg else None
        done = False
        for l in range(DEPTH):
            last = (l == DEPTH - 1)
            for name, fn in (("ada", lambda: phase_ada(l)), ("a", lambda: phase_a(l, last)),
                             ("b", lambda: phase_b(l, last)),
                             ("f", (lambda: phase_moe(l)) if (l % 2 == 1 and last) else (lambda: phase_f(l, last)))):
                fn()
                if stop_after == (l, name):
                    done = True
                    break
            if done:
                break
        if dbg is not None:
            es = ExitStack()
            with es:
                srcd = dict(mods=mods, QTd=QTd, KTd=KTd, Vd=Vd, cnTd=cnTd, QcTd=QcTd, KcTd=KcTd, Vcd=Vcd,
                            cncTd=cncTd, ctxs=ctxs)[dbg["src"]]
                b = S.buf(dma=True)
                S.dma("sp", dbg_out, srcd, sembuf=b)
                S.end_phase()
    return nc


def _host_inputs(inputs):
    f = lambda a: np.ascontiguousarray(np.asarray(a, dtype=np.float32))
    x = f(inputs["x"]); c = f(inputs["c"]); ctx = f(inputs["ctx"]); c_ctx = f(inputs["c_ctx"])
    rpb = f(inputs["rpb"])
    conv_w = f(inputs["conv_w"])
    gout = f(inputs["out_norm_g"])
    q = np.arange(128)
    qr_off = q // 64
    qc = q % 64
    cs = np.clip(qc - 8, 0, 48)

    def table(p, bs, nrows):
        qr = 2 * p + qr_off
        rs = np.clip(qr - 4, 0, 56)
        kk = np.arange(nrows * 64)
        kr = bs + kk // 64
        kc = kk % 64
        valid = ((kr[None, :] >= rs[:, None]) & (kr[None, :] < rs[:, None] + 8) &
                 (kc[None, :] >= cs[:, None]) & (kc[None, :] < cs[:, None] + 16))
        dy = np.clip(kr[None, :] - qr[:, None] + 7, 0, 14)
        dx = np.clip(kc[None, :] - qc[:, None] + 15, 0, 30)
        g = rpb[:, :, dy, dx]
        return np.where(valid[None, None], g, np.float32(NEG)).astype(np.float32)

    mid = table(10, 16, 9)
    nab_mid = np.ascontiguousarray(mid.transpose(0, 2, 1, 3))
    edges = [table(0, 0, 8), table(1, 0, 8), table(30, 56, 8), table(31, 56, 8)]
    nab_edge = np.ascontiguousarray(np.stack(edges, axis=1))
    convw = np.ascontiguousarray(conv_w.reshape(DEPTH, 3, 4, 128).transpose(0, 3, 2, 1).reshape(DEPTH, 128, 12))
    goutc = np.ascontiguousarray(gout[:, 512:].reshape(DEPTH, 4, 128).transpose(0, 2, 1))
    w_rt = np.ascontiguousarray(f(inputs["w_router"])[0].reshape(8, 128, NE).transpose(1, 0, 2))
    shared = {
        "w_ada": f(inputs["w_ada"]), "b_ada": f(inputs["b_ada"]),
        "norm_g": f(inputs["norm_g"]).reshape(DEPTH * 4, D),
        "w_in": f(inputs["w_in"]), "nab_mid": nab_mid, "nab_edge": nab_edge, "convw": convw,
        "gout": gout, "goutc": goutc, "w_out": f(inputs["w_out"]),
        "w_gu_dense": f(inputs["w_gu_dense"]), "w_down_dense": f(inputs["w_down_dense"]),
        "w_router": w_rt, "w_gu_moe": f(inputs["w_gu_moe"])[0], "w_down_moe": f(inputs["w_down_moe"])[0],
    }
    maps = []
    for b in range(x.shape[0]):
        cv = np.concatenate([c[b].reshape(8, 128).T, c_ctx.reshape(8, 128).T], axis=1)
        m = dict(shared)
        m["x"] = x[b]
        m["ctx"] = ctx[b]
        m["cvec"] = np.ascontiguousarray(cv)
        maps.append(m)
    return maps


def kernel(**inputs):
    maps = _host_inputs(inputs)
    nc = build_nc()
    res = run_bass_kernel_spmd(nc, maps, core_ids=list(range(len(maps))))
    return np.stack([np.asarray(r["y"], dtype=np.float32) for r in res.results], axis=0)
```

```python
import numpy as np
from contextlib import ExitStack
import concourse.bass as bass
import concourse.mybir as mybir
from concourse.bass_utils import run_bass_kernel_spmd

F32 = mybir.dt.float32
BF16 = mybir.dt.bfloat16
U32 = mybir.dt.uint32
I32 = mybir.dt.int32
NSEG = 23
PIPE_LAGS = (1, 2)
NSLOT = NSEG * 512
AF = mybir.ActivationFunctionType
ALU = mybir.AluOpType
AX = mybir.AxisListType

D = 1024
S_LEN = 4096
CTX = 256
DEPTH = 2
FF = 3584
NE = 8
INW = 3072
NEG = -30000.0


class Buf:
    __slots__ = ("w", "r", "dsem", "psum")

    def __init__(self):
        self.w = None
        self.r = []
        self.dsem = None
        self.psum = False


class Sched:
    def __init__(self, nc, es):
        self.nc = nc
        self.es = es
        self.engs = {}
        for name, e in (("pe", nc.tensor), ("act", nc.scalar), ("dve", nc.vector),
                        ("pool", nc.gpsimd), ("sp", nc.sync)):
            sem = es.enter_context(nc.semaphore("prog_" + name))
            self.engs[name] = dict(e=e, sem=sem, count=0, seen={})
        self.free_dsems = {"sp": [], "pool": []}
        self.all_dsems = []
        self.phase_dsems = []
        self.persist = []

    def buf(self, dma=False, persist=False):
        b = Buf()
        if dma:
            kind = "pool" if dma == "pool" else "sp"
            if self.free_dsems[kind] and not persist:
                d = self.free_dsems[kind].pop()
            else:
                sem = self.es.enter_context(self.nc.semaphore("dsem%d" % (len(self.all_dsems) + len(self.persist))))
                d = dict(sem=sem, count=0, kind=kind)
                (self.persist if persist else self.all_dsems).append(d)
            b.dsem = d
            if not persist:
                self.phase_dsems.append(d)
        return b

    def _wait(self, eng, deps):
        best = {}
        for sem, val in deps:
            k = id(sem)
            if k not in best or best[k][1] < val:
                best[k] = (sem, val)
        for k, (sem, val) in best.items():
            if eng["seen"].get(k, 0) < val:
                eng["e"].wait_ge(sem, val)
                eng["seen"][k] = val

    @staticmethod
    def _deps(reads, writes):
        deps = []
        for b in reads:
            if b.w is not None:
                deps.append(b.w)
            if b.psum:
                deps.extend(b.r)
        for b in writes:
            if b.w is not None:
                deps.append(b.w)
            deps.extend(b.r)
        return deps

    @staticmethod
    def _mark(tok, reads, writes):
        for b in reads:
            b.r.append(tok)
            if len(b.r) > 24:
                best = {}
                for s, v in b.r:
                    if id(s) not in best or best[id(s)][1] < v:
                        best[id(s)] = (s, v)
                b.r = list(best.values())
        for b in writes:
            b.w = tok
            b.r = []

    def op(self, engname, fn, reads=(), writes=()):
        eng = self.engs[engname]
        deps = self._deps(reads, writes)
        if engname == "pe":
            deps = [d for d in deps if d[0] is not eng["sem"]]
        self._wait(eng, deps)
        ins = fn(eng["e"])
        eng["count"] += 1
        ins.then_inc(eng["sem"], 1)
        eng["seen"].pop(id(eng["sem"]), None)
        self._mark((eng["sem"], eng["count"]), reads, writes)
        return ins

    def dma(self, engname, out, in_, reads=(), writes=(), sembuf=None):
        eng = self.engs[engname]
        self._wait(eng, self._deps(reads, writes))
        ins = eng["e"].dma_start(out=out, in_=in_)
        d = sembuf.dsem
        assert d["kind"] == ("pool" if engname == "pool" else "sp"), (d["kind"], engname)
        d["count"] += 16
        ins.then_inc(d["sem"], 16)
        self._mark((d["sem"], d["count"]), reads, writes)
        return ins

    def dma_fn(self, engname, fn, reads=(), writes=(), sembuf=None):
        eng = self.engs[engname]
        self._wait(eng, self._deps(reads, writes))
        ins = fn(eng["e"])
        d = sembuf.dsem
        assert d["kind"] == ("pool" if engname == "pool" else "sp"), (d["kind"], engname)
        d["count"] += 16
        ins.then_inc(d["sem"], 16)
        self._mark((d["sem"], d["count"]), reads, writes)
        return ins

    def barrier(self):
        deps = [(e["sem"], e["count"]) for e in self.engs.values() if e["count"] > 0]
        deps += [(d["sem"], d["count"]) for d in self.all_dsems if d["count"] > 0]
        for eng in self.engs.values():
            self._wait(eng, deps)

    def end_phase(self):
        self.barrier()
        for d in self.phase_dsems:
            self.free_dsems[d["kind"]].append(d)
        self.phase_dsems = []


class TT:
    def __init__(self, t, b):
        self.t = t
        self.b = b


def _bc_rows(handle_ap, row, n, parts=128):
    return handle_ap[row:row + 1, 0:n].partition_broadcast(parts)


def build_nc(dbg=None):
    nc = bass.Bass("TRN2", target_bir_lowering=False)
    di = lambda name, shape, dt=F32: nc.dram_tensor(name, list(shape), dt, kind="ExternalInput").ap()
    x_in = di("x", [S_LEN, D])
    ctx_in = di("ctx", [CTX, D])
    cvec = di("cvec", [128, 16])
    w_ada = di("w_ada", [DEPTH, D, 6 * D])
    b_ada = di("b_ada", [DEPTH, 6 * D])
    norm_g = di("norm_g", [DEPTH * 4, D])
    w_in = di("w_in", [DEPTH, D, INW])
    nab_mid = di("nab_mid", [DEPTH, 128, 8, 576])
    nab_edge = di("nab_edge", [DEPTH, 4, 8, 128, 512])
    convw = di("convw", [DEPTH, 128, 12])
    gout = di("gout", [DEPTH, D])
    goutc = di("goutc", [DEPTH, 128, 4])
    w_out = di("w_out", [DEPTH, D, D])
    w_gu_d = di("w_gu_dense", [1, D, 2 * FF])
    w_dn_d = di("w_down_dense", [1, FF, D])
    w_rt = di("w_router", [128, 8, NE])
    w_gu_m = di("w_gu_moe", [NE, D, 2 * FF])
    w_dn_m = di("w_down_moe", [NE, FF, D])
    y_out = nc.dram_tensor("y", [S_LEN, D], F32, kind="ExternalOutput").ap()

    ds = lambda name, shape, dt: nc.dram_tensor(name, list(shape), dt).ap()
    QTd = ds("QTd", [4, 128, S_LEN], BF16)
    KTd = ds("KTd", [4, 128, S_LEN], BF16)
    Vd = ds("Vd", [S_LEN, 512], BF16)
    cnTd = ds("cnTd", [4, 128, S_LEN], BF16)
    QcTd = ds("QcTd", [4, 128, CTX], BF16)
    KcTd = ds("KcTd", [4, 128, CTX], BF16)
    Vcd = ds("Vcd", [CTX, 512], BF16)
    cncTd = ds("cncTd", [4, 128, CTX], BF16)
    mods = ds("mods", [12, D], F32)
    WGd = ds("WGd", [NE * 7 * 128, 8 * 512], BF16)
    WUd = ds("WUd", [NE * 7 * 128, 8 * 512], BF16)
    WDd = ds("WDd", [NE * 7 * 128, 4 * D], BF16)
    hs = ds("hs", [NSLOT, D], BF16)
    ys = ds("ys", [NSLOT, D], F32)
    ctxs = ds("ctxs", [CTX, D], F32)
    dbg_out = None
    if dbg is not None:
        dbg_out = nc.dram_tensor("dbg", list(dbg["shape"]), dbg.get("dt", F32), kind="ExternalOutput").ap()

    ges = ExitStack()
    with ges:
        S = Sched(nc, ges)

        uid = [0]

        def alloc(es, name, shape, dt, dma=False, psum=False):
            uid[0] += 1
            name = "%s_%d" % (name, uid[0])
            if psum:
                t = es.enter_context(nc.psum_tensor(name, list(shape), dt))
            else:
                t = es.enter_context(nc.sbuf_tensor(name, list(shape), dt))
            tt = TT(t, S.buf(dma=dma))
            tt.b.psum = bool(psum)
            return tt

        def allocn(es, n, name, shape, dt, dma=False, psum=False):
            return [alloc(es, "%s%d" % (name, i), shape, dt, dma=dma, psum=psum) for i in range(n)]

        ident = alloc(ges, "ident", [128, 128], BF16)
        ident32 = alloc(ges, "ident32", [128, 128], F32)
        ones = alloc(ges, "ones", [128, 128], BF16)
        epsb = alloc(ges, "epsb", [128, 1], F32)
        for idt in (ident, ident32):
            S.op("pool", lambda e: e.memset(idt.t[:], 0.0), writes=[idt.b])
            S.op("pool", lambda e: e.affine_select(out=idt.t[:], in_=idt.t[:], pattern=[[-1, 128]],
                                                   compare_op=ALU.not_equal, fill=1.0, base=0,
                                                   channel_multiplier=1), reads=[idt.b], writes=[idt.b])
        S.op("pool", lambda e: e.memset(ones.t[:], 1.0), writes=[ones.b])
        S.op("pool", lambda e: e.memset(epsb.t[:], 1e-6), writes=[epsb.b])

        ones32 = alloc(ges, "ones32", [128, 128], F32)
        tri32 = alloc(ges, "tri32", [128, 128], F32)
        S.op("pool", lambda e: e.memset(ones32.t[:], 1.0), writes=[ones32.b])
        S.op("pool", lambda e: e.memset(tri32.t[:], 1.0), writes=[tri32.b])
        S.op("pool", lambda e: e.affine_select(out=tri32.t[:], in_=tri32.t[:], pattern=[[1, 128]],
                                               compare_op=ALU.is_gt, fill=0.0, base=0,
                                               channel_multiplier=-1), reads=[tri32.b], writes=[tri32.b])
        zt = alloc(ges, "zt", [128, D], BF16)
        S.op("pool", lambda e: e.memset(zt.t[:], 0.0), writes=[zt.b])
        hsz = S.buf(dma="pool", persist=True)
        convb = S.buf(dma="pool", persist=True)
        conv_pieces = []
        for r in range(NSLOT // 512):
            conv_pieces.append((hs[r * 512:(r + 1) * 512, :].rearrange("(r p) d -> p r d", p=128),
                                zt.t[:].unsqueeze(1).to_broadcast([128, 4, D]), [zt.b], hsz))
        for ex in range(NE):
            for c in range(7):
                r0 = (ex * 7 + c) * 128
                conv_pieces.append((WGd[r0:r0 + 128, :].rearrange("p (c f) -> p c f", c=8),
                                    w_gu_m[ex, :, c * 512:(c + 1) * 512].rearrange("(c p) f -> p c f", p=128), [], convb))
                conv_pieces.append((WUd[r0:r0 + 128, :].rearrange("p (c f) -> p c f", c=8),
                                    w_gu_m[ex, :, FF + c * 512:FF + (c + 1) * 512].rearrange("(c p) f -> p c f", p=128),
                                    [], convb))
                conv_pieces.append((WDd[r0:r0 + 128, :].rearrange("p (j d) -> p j d", j=4),
                                    w_dn_m[ex, c * 512:(c + 1) * 512, :].rearrange("(j p) d -> p j d", p=128), [], convb))
        conv_pos = [0]

        def conv_step(n):
            for _ in range(n):
                if conv_pos[0] >= len(conv_pieces):
                    return
                o, i_, rd, sb = conv_pieces[conv_pos[0]]
                conv_pos[0] += 1
                S.dma("pool", o, i_, reads=rd, sembuf=sb)
                sb.w = (sb.dsem["sem"], sb.dsem["count"])

        def rstd_from_ss(ss, rstd, scale):
            S.op("act", lambda e: e.activation(out=rstd.t[:], in_=ss.t[:], func=AF.Sqrt, scale=scale,
                                               bias=epsb.t[:]), reads=[ss.b, epsb.b], writes=[rstd.b])
            S.op("dve", lambda e: e.reciprocal(out=rstd.t[:], in_=rstd.t[:]), reads=[rstd.b], writes=[rstd.b])

        def phase_ada(l):
            es = ExitStack()
            with es:
                cv = alloc(es, "cv", [128, 16], F32, dma=True)
                sc = alloc(es, "sc", [128, 16], F32)
                scb = alloc(es, "scb", [128, 16], BF16)
                wch = allocn(es, 2, "wch", [128, 8, 512], BF16, dma="pool")
                row = alloc(es, "row", [1, 2, 6 * D], F32)
                bad = alloc(es, "bad", [1, 6 * D], F32, dma=True)
                ng = alloc(es, "ng", [1, 4, D], F32, dma=True)
                ps = allocn(es, 2, "adaps", [128, 512], F32, psum=True)
                stb = S.buf(dma=True)
                S.dma("sp", cv.t[:], cvec[:, :], writes=[cv.b], sembuf=cv.b)
                S.dma("sp", bad.t[:], b_ada[l:l + 1, :], writes=[bad.b], sembuf=bad.b)
                S.dma("sp", ng.t[:], norm_g[4 * l:4 * l + 4, :].rearrange("(o r) d -> o r d", o=1),
                      writes=[ng.b], sembuf=ng.b)
                S.op("act", lambda e: e.activation(out=sc.t[:], in_=cv.t[:], func=AF.Silu),
                     reads=[cv.b], writes=[sc.b])
                S.op("dve", lambda e: e.tensor_copy(out=scb.t[:], in_=sc.t[:]), reads=[sc.b], writes=[scb.b])
                for j in range(12):
                    w = wch[j % 2]
                    S.dma("pool", w.t[:], w_ada[l, :, j * 512:(j + 1) * 512].rearrange("(c p) f -> p c f", p=128),
                          writes=[w.b], sembuf=w.b)
                    for src in range(2):
                        p = ps[src]
                        for ch in range(8):
                            S.op("pe", lambda e: e.matmul(p.t[0:1, :], lhsT=scb.t[:, src * 8 + ch:src * 8 + ch + 1],
                                                          rhs=w.t[:, ch, :], start=(ch == 0), stop=(ch == 7)),
                                 reads=[scb.b, w.b], writes=[p.b])
                        S.op("dve", lambda e: e.tensor_tensor(out=row.t[0:1, src, j * 512:(j + 1) * 512],
                                                              in0=p.t[0:1, :], in1=bad.t[0:1, j * 512:(j + 1) * 512],
                                                              op=ALU.add),
                             reads=[p.b, bad.b], writes=[row.b])
                for src in range(2):
                    for (k, gi) in ((1, 0), (4, 2)):
                        S.op("dve", lambda e: e.scalar_tensor_tensor(
                            out=row.t[0:1, src, k * D:(k + 1) * D], in0=row.t[0:1, src, k * D:(k + 1) * D],
                            scalar=1.0, in1=ng.t[0:1, gi, :], op0=ALU.add, op1=ALU.mult),
                            reads=[row.b, ng.b], writes=[row.b])
                    for (k, gi) in ((2, 1), (5, 3)):
                        S.op("dve", lambda e: e.tensor_tensor(
                            out=row.t[0:1, src, k * D:(k + 1) * D], in0=row.t[0:1, src, k * D:(k + 1) * D],
                            in1=ng.t[0:1, gi, :], op=ALU.mult), reads=[row.b, ng.b], writes=[row.b])
                S.dma("sp", mods.rearrange("(o r) d -> o (r d)", o=1), row.t[0:1, :, :].rearrange("o s d -> o (s d)"),
                      reads=[row.b], sembuf=stb)
                S.end_phase()

        def load_bc(es, name, rowidx, n=D, src=None):
            t = alloc(es, name, [128, n], F32, dma=True)
            srcap = mods if src is None else src
            S.dma("sp", t.t[:], _bc_rows(srcap, rowidx, n), writes=[t.b], sembuf=t.b)
            return t

        def norm_tile(xt, gs, sh, ss, rstd, junk, tmp, hb, hb32=None):
            S.op("act", lambda e: e.activation(out=junk.t[:], in_=xt.t[:], func=AF.Square, accum_out=ss.t[:]),
                 reads=[xt.b], writes=[junk.b, ss.b])
            rstd_from_ss(ss, rstd, 1.0 / D)
            S.op("dve", lambda e: e.scalar_tensor_tensor(out=tmp.t[:], in0=xt.t[:], scalar=rstd.t[:, 0:1],
                                                         in1=gs.t[:], op0=ALU.mult, op1=ALU.mult),
                 reads=[xt.b, rstd.b, gs.b], writes=[tmp.b])
            if hb32 is not None:
                S.op("pool", lambda e: e.tensor_tensor(out=hb32.t[:], in0=tmp.t[:], in1=sh.t[:], op=ALU.add),
                     reads=[tmp.b, sh.b], writes=[hb32.b])
                S.op("pool", lambda e: e.tensor_copy(out=hb.t[:], in_=hb32.t[:]), reads=[hb32.b], writes=[hb.b])
            else:
                S.op("pool", lambda e: e.tensor_tensor(out=hb.t[:], in0=tmp.t[:], in1=sh.t[:], op=ALU.add),
                     reads=[tmp.b, sh.b], writes=[hb.b])

        def phase_a(l, last):
            es = ExitStack()
            with es:
                win = alloc(es, "win", [128, 8, INW], BF16)
                win_b = [S.buf(dma="pool") for _ in range(6)]
                for j in ([1, 2, 3, 4, 5, 0] if last else [3, 4, 5, 0, 1, 2]):
                    S.dma("pool", win.t[:, :, j * 512:(j + 1) * 512],
                          w_in[l, :, j * 512:(j + 1) * 512].rearrange("(c p) f -> p c f", p=128),
                          writes=[win_b[j]], sembuf=win_b[j])
                gs_x = load_bc(es, "gs_x", 1)
                sh_x = load_bc(es, "sh_x", 0)
                gs_c = load_bc(es, "gs_c", 6 + 1)
                sh_c = load_bc(es, "sh_c", 6 + 0)
                cw = alloc(es, "cw", [128, 12], F32, dma=True)
                S.dma("sp", cw.t[:], convw[l], writes=[cw.b], sembuf=cw.b)
                goc = alloc(es, "goc", [128, 4], F32, dma=True)
                S.dma("sp", goc.t[:], goutc[l], writes=[goc.b], sembuf=goc.b)

                xts = allocn(es, 4, "xt", [128, D], F32, dma=True)
                junk = alloc(es, "junk", [128, D], BF16)
                tmp = allocn(es, 2, "tmp", [128, D], F32)
                hbs = allocn(es, 4, "hb", [128, D], BF16)
                sss = allocn(es, 2, "ss", [128, 1], F32)
                rstds = allocn(es, 2, "rstd", [128, 1], F32)
                hT = allocn(es, 2, "hT", [128, 8, 512], BF16)
                tps = allocn(es, 2, "tp", [128, 8, 128], BF16, psum=True)
                mps = allocn(es, 4, "mp", [128, 512], F32, psum=True)
                sps = alloc(es, "sps", [128, 512], F32, psum=True)
                qks = allocn(es, 2, "qks", [128, 8, 512], BF16, dma=True)
                vst = allocn(es, 2, "vst", [128, 512], BF16, dma=True)
                BT = allocn(es, 2, "BT", [128, 4, 512], BF16)
                vT = allocn(es, 2, "vT", [128, 4, 514], F32)
                csb = allocn(es, 2, "csb", [128, 512], F32)
                cacc = allocn(es, 4, "cacc", [128, 512], F32)
                co = alloc(es, "co", [128, 4, 512], F32)
                sq = allocn(es, 4, "sq", [128, 512], BF16)
                rbc = alloc(es, "rbc", [128, 512], F32)
                cn = allocn(es, 2, "cn", [128, 4, 512], BF16, dma=True)
                cnt = dict(mp=0)

                csrc = ctx_in if l == 0 else ctxs
                xsrc = x_in if l == 0 else y_out
                G = [dict(src=csrc, dst=dict(Q=QcTd, K=KcTd, V=Vcd), tok0=0, T=CTX, gs=gs_c, sh=sh_c,
                          want_q=not last, want_conv=not last, lat=False, cdst=cncTd)]
                for g in range(8):
                    G.append(dict(src=xsrc, dst=dict(Q=QTd, K=KTd, V=Vd), tok0=g * 512, T=512, gs=gs_x, sh=sh_x,
                                  want_q=True, want_conv=True, lat=True, cdst=cnTd))
                n0 = 0
                for k, gd in enumerate(G):
                    gd["k"] = k
                    gd["n0"] = n0
                    n0 += gd["T"] // 128

                def load_part(gd):
                    for i in range(gd["T"] // 128):
                        n = gd["n0"] + i
                        xt = xts[n % 4]
                        S.dma("sp", xt.t[:], gd["src"][gd["tok0"] + i * 128:gd["tok0"] + (i + 1) * 128, :],
                              writes=[xt.b], sembuf=xt.b)

                def norm_part(gd):
                    for i in range(gd["T"] // 128):
                        n = gd["n0"] + i
                        xt = xts[n % 4]
                        k2 = n % 2
                        norm_tile(xt, gd["gs"], gd["sh"], sss[k2], rstds[k2], junk, tmp[k2], hbs[n % 4])

                def trans_part(gd):
                    h = hT[gd["k"] % 2]
                    for i in range(gd["T"] // 128):
                        n = gd["n0"] + i
                        tp = tps[n % 2]
                        hb_ = hbs[n % 4]
                        for c in range(8):
                            S.op("pe", lambda e: e.transpose(out=tp.t[:, c, :], in_=hb_.t[:, c * 128:(c + 1) * 128],
                                                             identity=ident.t[:]),
                                 reads=[hb_.b, ident.b], writes=[tp.b])
                        S.op("act", lambda e: e.activation(out=h.t[:, :, i * 128:(i + 1) * 128], in_=tp.t[:],
                                                           func=AF.Copy), reads=[tp.b], writes=[h.b])

                def fmm(gd, ft):
                    T = gd["T"]
                    h = hT[gd["k"] % 2]
                    p = mps[cnt["mp"] % 4]
                    cnt["mp"] += 1
                    for c in range(8):
                        S.op("pe", lambda e: e.matmul(p.t[:, 0:T], lhsT=win.t[:, c, ft * 128:(ft + 1) * 128],
                                                      rhs=h.t[:, c, 0:T], start=(c == 0), stop=(c == 7)),
                             reads=[win_b[ft // 4], h.b], writes=[p.b])
                    return p

                def mm_bcu(gd):
                    T = gd["T"]
                    bt = BT[gd["k"] % 2]
                    vt = vT[gd["k"] % 2]
                    for c in range(4):
                        p = fmm(gd, 12 + c)
                        S.op("act", lambda e: e.activation(out=bt.t[:, c, 0:T], in_=p.t[:, 0:T], func=AF.Copy),
                             reads=[p.b], writes=[bt.b])
                        p = fmm(gd, 16 + c)
                        cs = csb[c % 2]
                        S.op("act", lambda e: e.activation(out=cs.t[:, 0:T], in_=p.t[:, 0:T], func=AF.Copy),
                             reads=[p.b], writes=[cs.b])
                        p = fmm(gd, 20 + c)
                        S.op("dve", lambda e: e.tensor_tensor(out=vt.t[:, c, 1:T + 1], in0=p.t[:, 0:T], in1=cs.t[:, 0:T],
                                                              op=ALU.mult), reads=[p.b, cs.b], writes=[vt.b])

                def mm_qk(gd):
                    T, tok0, dst = gd["T"], gd["tok0"], gd["dst"]
                    h = hT[gd["k"] % 2]
                    st = qks[gd["k"] % 2]
                    for ft in range(8):
                        if ft < 4 and not gd["want_q"]:
                            continue
                        p = fmm(gd, ft)
                        if ft < 4:
                            S.op("act", lambda e: e.activation(out=st.t[:, ft, 0:T], in_=p.t[:, 0:T], func=AF.Copy,
                                                               scale=0.125), reads=[p.b], writes=[st.b])
                        else:
                            S.op("dve", lambda e: e.tensor_copy(out=st.t[:, ft, 0:T], in_=p.t[:, 0:T]),
                                 reads=[p.b], writes=[st.b])
                    if gd["want_q"]:
                        S.dma("sp", dst["Q"][:, :, tok0:tok0 + T].rearrange("c p t -> p c t"), st.t[:, 0:4, 0:T],
                              reads=[st.b], sembuf=st.b)
                    S.dma("sp", dst["K"][:, :, tok0:tok0 + T].rearrange("c p t -> p c t"), st.t[:, 4:8, 0:T],
                          reads=[st.b], sembuf=st.b)

                def mm_v(gd):
                    T, tok0, dst = gd["T"], gd["tok0"], gd["dst"]
                    h = hT[gd["k"] % 2]
                    for i in range(T // 128):
                        p = mps[cnt["mp"] % 4]
                        cnt["mp"] += 1
                        for c in range(8):
                            S.op("pe", lambda e: e.matmul(p.t[:, :], lhsT=h.t[:, c, i * 128:(i + 1) * 128],
                                                          rhs=win.t[:, c, 1024:1536], start=(c == 0), stop=(c == 7)),
                                 reads=[win_b[2], h.b], writes=[p.b])
                        v = vst[i % 2]
                        S.op("act", lambda e: e.activation(out=v.t[:], in_=p.t[:], func=AF.Copy),
                             reads=[p.b], writes=[v.b])
                        S.dma("sp", dst["V"][tok0 + i * 128:tok0 + (i + 1) * 128, :], v.t[:], reads=[v.b], sembuf=v.b)

                def conv_ew(gd):
                    T = gd["T"]
                    bt = BT[gd["k"] % 2]
                    vt = vT[gd["k"] % 2]
                    for c in range(4):
                        a = cacc[c]
                        S.op("act", lambda e: e.activation(out=a.t[:, 0:T], in_=vt.t[:, c, 1:T + 1], func=AF.Copy,
                                                           scale=cw.t[:, c * 3 + 1:c * 3 + 2]),
                             reads=[vt.b, cw.b], writes=[a.b])
                    for c in range(4):
                        a = cacc[c]
                        S.op("dve", lambda e: e.scalar_tensor_tensor(out=a.t[:, 0:T], in0=vt.t[:, c, 0:T],
                                                                     scalar=cw.t[:, c * 3:c * 3 + 1], in1=a.t[:, 0:T],
                                                                     op0=ALU.mult, op1=ALU.add),
                             reads=[vt.b, cw.b, a.b], writes=[a.b])
                        S.op("dve", lambda e: e.scalar_tensor_tensor(out=a.t[:, 0:T], in0=vt.t[:, c, 2:T + 2],
                                                                     scalar=cw.t[:, c * 3 + 2:c * 3 + 3], in1=a.t[:, 0:T],
                                                                     op0=ALU.mult, op1=ALU.add),
                             reads=[vt.b, cw.b, a.b], writes=[a.b])
                    for c in range(4):
                        a = cacc[c]
                        S.op("pool", lambda e: e.tensor_tensor(out=co.t[:, c, 0:T], in0=a.t[:, 0:T], in1=bt.t[:, c, 0:T],
                                                               op=ALU.mult), reads=[a.b, bt.b], writes=[co.b])

                def conv_sq(gd):
                    T = gd["T"]
                    for c in range(4):
                        s = sq[c]
                        S.op("act", lambda e: e.activation(out=s.t[:, 0:T], in_=co.t[:, c, 0:T], func=AF.Square),
                             reads=[co.b], writes=[s.b])

                def conv_fin(gd):
                    T, tok0 = gd["T"], gd["tok0"]
                    for c in range(4):
                        s = sq[c]
                        S.op("pe", lambda e: e.matmul(sps.t[:, 0:T], lhsT=ones.t[:], rhs=s.t[:, 0:T],
                                                      start=(c == 0), stop=(c == 3)),
                             reads=[ones.b, s.b], writes=[sps.b])
                    S.op("act", lambda e: e.activation(out=rbc.t[:, 0:T], in_=sps.t[:, 0:T], func=AF.Sqrt,
                                                       scale=1.0 / 512, bias=epsb.t[:]),
                         reads=[sps.b, epsb.b], writes=[rbc.b])
                    S.op("dve", lambda e: e.reciprocal(out=rbc.t[:, 0:T], in_=rbc.t[:, 0:T]), reads=[rbc.b], writes=[rbc.b])
                    o = cn[gd["k"] % 2]
                    for c in range(4):
                        S.op("dve", lambda e: e.scalar_tensor_tensor(out=o.t[:, c, 0:T], in0=co.t[:, c, 0:T],
                                                                     scalar=goc.t[:, c:c + 1], in1=rbc.t[:, 0:T],
                                                                     op0=ALU.mult, op1=ALU.mult),
                             reads=[co.b, goc.b, rbc.b], writes=[o.b])
                    S.dma("sp", gd["cdst"][:, :, tok0:tok0 + T].rearrange("c p t -> p c t"), o.t[:, :, 0:T],
                          reads=[o.b], sembuf=o.b)

                load_part(G[0])
                norm_part(G[0])
                trans_part(G[0])
                if len(G) > 1:
                    load_part(G[1])
                prev_lat = None
                for k, gd in enumerate(G):
                    nxt = G[k + 1] if k + 1 < len(G) else None
                    cv = None
                    if nxt is not None:
                        norm_part(nxt)
                        if k + 2 < len(G):
                            load_part(G[k + 2])
                    if gd["want_conv"]:
                        mm_bcu(gd)
                    if gd["want_conv"]:
                        vt = vT[k % 2]
                        if not gd["lat"]:
                            S.op("pool", lambda e: e.memset(vt.t[:, :, 0:1], 0.0), writes=[vt.b])
                            S.op("pool", lambda e: e.memset(vt.t[:, :, CTX + 1:CTX + 2], 0.0), writes=[vt.b])
                            cv = gd
                        elif prev_lat is None:
                            S.op("pool", lambda e: e.memset(vt.t[:, :, 0:1], 0.0), writes=[vt.b])
                        else:
                            pv = vT[prev_lat["k"] % 2]
                            S.op("pool", lambda e: e.tensor_copy(out=vt.t[:, :, 0:1], in_=pv.t[:, :, 512:513]),
                                 reads=[pv.b], writes=[vt.b])
                            S.op("pool", lambda e: e.tensor_copy(out=pv.t[:, :, 513:514], in_=vt.t[:, :, 1:2]),
                                 reads=[vt.b], writes=[pv.b])
                            cv = prev_lat
                        if cv is not None:
                            conv_ew(cv)
                    mm_qk(gd)
                    if cv is not None:
                        conv_sq(cv)
                    if nxt is not None:
                        trans_part(nxt)
                    mm_v(gd)
                    if cv is not None:
                        conv_fin(cv)
                    if gd["lat"]:
                        prev_lat = gd
                        conv_step(3)
                pv = vT[prev_lat["k"] % 2]
                S.op("pool", lambda e: e.memset(pv.t[:, :, 513:514], 0.0), writes=[pv.b])
                conv_ew(prev_lat)
                conv_sq(prev_lat)
                conv_fin(prev_lat)
                S.end_phase()

        def phase_b(l, last):
            es = ExitStack()
            with es:
                KT = alloc(es, "KT", [128, 4, S_LEN], BF16, dma=True)
                V = alloc(es, "V", [128, 32, 512], BF16, dma=True)
                KcT = alloc(es, "KcT", [128, 4, CTX], BF16, dma=True)
                Vc = alloc(es, "Vc", [128, 2, 512], BF16, dma=True)
                nbm = alloc(es, "nbm", [128, 8, 576], F32, dma=True)

                def init_loads_first():
                    S.dma("sp", KcT.t[:], KcTd.rearrange("c p t -> p c t"), writes=[KcT.b], sembuf=KcT.b)
                    S.dma("sp", Vc.t[:], Vcd.rearrange("(t p) f -> p t f", p=128), writes=[Vc.b], sembuf=Vc.b)

                def init_loads_rest():
                    S.dma("sp", KT.t[:, 0, :], KTd[0], writes=[KT.b], sembuf=KT.b)
                    S.dma("sp", V.t[:, 0:8, :], Vd[0:1024, :].rearrange("(t p) f -> p t f", p=128),
                          writes=[V.b], sembuf=V.b)
                    for c in range(1, 4):
                        S.dma("sp", KT.t[:, c, :], KTd[c], writes=[KT.b], sembuf=KT.b)
                    S.dma("sp", nbm.t[:], nab_mid[l], writes=[nbm.b], sembuf=nbm.b)
                    for q in range(1, 4):
                        S.dma("sp", V.t[:, q * 8:(q + 1) * 8, :],
                              Vd[q * 1024:(q + 1) * 1024, :].rearrange("(t p) f -> p t f", p=128),
                              writes=[V.b], sembuf=V.b)
                wo = alloc(es, "wo", [128, 8, D], BF16, dma="pool")
                for j in range(2):
                    S.dma("pool", wo.t[:, :, j * 512:(j + 1) * 512],
                          w_out[l, :, j * 512:(j + 1) * 512].rearrange("(c p) f -> p c f", p=128),
                          writes=[wo.b], sembuf=wo.b)
                ggt_x = load_bc(es, "ggt_x", 2)
                ggt_c = load_bc(es, "ggt_c", 6 + 2) if not last else None
                goa = load_bc(es, "goa", l, n=512, src=gout)

                NB = 3
                QA = allocn(es, 2, "QA", [128, 4, 512], BF16, dma=True)
                QB = allocn(es, 2, "QB", [128, 4, 512], BF16, dma=True)
                for qz in QA:
                    S.op("pool", lambda e: e.memset(qz.t[64:128, :, :], 0.0), writes=[qz.b])
                for qz in QB:
                    S.op("pool", lambda e: e.memset(qz.t[0:64, :, :], 0.0), writes=[qz.b])
                mT = allocn(es, 2, "mT", [128, 8, 512], BF16, dma=True)
                nbe = allocn(es, 4, "nbe", [128, 512], F32, dma=True)
                psA = allocn(es, 2, "psA", [128, 512], F32, psum=True)
                psB = allocn(es, 2, "psB", [128, 512], F32, psum=True)
                psT = allocn(es, 2, "psT", [128, 8, 128], BF16, psum=True)
                psO = alloc(es, "psO", [128, 512], F32, psum=True)
                psY = alloc(es, "psY", [128, 512], F32, psum=True)
                Ssb = allocn(es, NB, "Ssb", [128, 832], F32)
                Sctx_b = [S.buf() for _ in range(NB)]
                Psb = allocn(es, NB, "Psb", [128, 832], BF16)
                PT = allocn(es, NB, "PT", [128, 7, 128], BF16)
                nmx = allocn(es, NB, "nmx", [128, 1], F32)
                rsum = allocn(es, 3, "rsum", [128, 8], F32)
                rinv = allocn(es, 2, "rinv", [128, 8], F32)
                Osb = allocn(es, 2, "Osb", [128, 512], F32)
                On = allocn(es, 2, "On", [128, 512], BF16)
                junk = alloc(es, "junkb", [128, 512], BF16)
                ss = allocn(es, 2, "ssb", [128, 2], F32)
                ss1 = allocn(es, 2, "ss1b", [128, 1], F32)
                rstd = allocn(es, 2, "rstdb", [128, 1], F32)
                xr = allocn(es, 2, "xr", [128, D], F32, dma=True)
                ysb = allocn(es, 2, "ysb", [128, D], F32)

                def rstd_lnexp(ssv, rs_, scale):
                    S.op("act", lambda e: e.activation(out=rs_.t[:], in_=ssv.t[:], func=AF.Ln, scale=scale,
                                                       bias=epsb.t[:]), reads=[ssv.b, epsb.b], writes=[rs_.b])
                    S.op("act", lambda e: e.activation(out=rs_.t[:], in_=rs_.t[:], func=AF.Exp, scale=-0.5),
                         reads=[rs_.b], writes=[rs_.b])

                def stage_a(u):
                    i = u["i"]
                    a, b = psA[i % 2], psB[i % 2]
                    q_ap, pb, chunk = u["q"], u["pb"], u["chunk"]
                    if u["loc"] is not None:
                        tok0, nloc, tile0 = u["loc"]
                        S.op("pe", lambda e: e.matmul(a.t[:, 0:512], lhsT=q_ap,
                                                      rhs=KT.t[:, chunk, tok0:tok0 + 512], start=True, stop=True),
                             reads=[u["qb"], KT.b], writes=[a.b])
                        if nloc > 512:
                            S.op("pe", lambda e: e.matmul(b.t[:, 0:64], lhsT=q_ap,
                                                          rhs=KT.t[:, chunk, tok0 + 512:tok0 + 576],
                                                          start=True, stop=True),
                                 reads=[u["qb"], KT.b], writes=[b.b])
                    S.op("pe", lambda e: e.matmul(b.t[:, 64:320], lhsT=q_ap, rhs=KcT.t[:, chunk, :],
                                                  start=True, stop=True), reads=[u["qb"], KcT.b], writes=[b.b])

                def stage_b1(u):
                    i, h = u["i"], u["h"]
                    a, b = psA[i % 2], psB[i % 2]
                    s, mx = Ssb[i % NB], nmx[i % NB]
                    sc_b = Sctx_b[i % NB]
                    nloc = u["loc"][1] if u["loc"] is not None else 0
                    ntot = nloc + CTX
                    S.op("act", lambda e: e.activation(out=s.t[:, nloc:ntot], in_=b.t[:, 64:320], func=AF.Copy),
                         reads=[b.b], writes=[sc_b])
                    if u["loc"] is not None:
                        if u["edge"] is None:
                            bt, bap = nbm.b, nbm.t[:, h, :]
                        else:
                            nb = nbe[u["nbi"] % 4]
                            bt, bap = nb.b, nb.t[:, :]
                        S.op("dve", lambda e: e.tensor_tensor(out=s.t[:, 0:512], in0=a.t[:, 0:512], in1=bap[:, 0:512],
                                                              op=ALU.add), reads=[a.b, bt], writes=[s.b])
                        if nloc > 512:
                            S.op("dve", lambda e: e.tensor_tensor(out=s.t[:, 512:576], in0=b.t[:, 0:64],
                                                                  in1=bap[:, 512:576], op=ALU.add),
                                 reads=[b.b, bt], writes=[s.b])
                    S.op("dve", lambda e: e.reduce_max(out=mx.t[:], in_=s.t[:, 0:ntot], axis=AX.X, negate=True),
                         reads=[s.b, sc_b], writes=[mx.b])

                def stage_b2(u):
                    i, h = u["i"], u["h"]
                    s, pp, mx = Ssb[i % NB], Psb[i % NB], nmx[i % NB]
                    sc_b = Sctx_b[i % NB]
                    rs = rsum[u["tile"] % 3]
                    nloc = u["loc"][1] if u["loc"] is not None else 0
                    ntot = nloc + CTX
                    S.op("act", lambda e: e.activation(out=pp.t[:, 0:ntot], in_=s.t[:, 0:ntot], func=AF.Exp,
                                                       bias=mx.t[:, 0:1], accum_out=rs.t[:, h:h + 1]),
                         reads=[s.b, sc_b, mx.b], writes=[pp.b, rs.b])

                def chunks_of(u):
                    nloc = 0
                    chunks = []
                    if u["loc"] is not None:
                        tok0, nloc, tile0 = u["loc"]
                        for k in range(4):
                            chunks.append((k * 128, 128, V, tile0 + k))
                        if nloc > 512:
                            chunks.append((512, 64, V, tile0 + 4))
                    chunks.append((nloc, 128, Vc, 0))
                    chunks.append((nloc + 128, 128, Vc, 1))
                    return chunks

                def stage_c1(u):
                    i = u["i"]
                    pp = Psb[i % NB]
                    pst = psT[i % 2]
                    for j, (off, sz, vt, ti) in enumerate(chunks_of(u)):
                        S.op("pe", lambda e: e.transpose(out=pst.t[0:sz, j, :], in_=pp.t[:, off:off + sz],
                                                         identity=ident.t[:]),
                             reads=[pp.b, ident.b], writes=[pst.b])

                def stage_c1b(u):
                    i = u["i"]
                    pt = PT[i % NB]
                    pst = psT[i % 2]
                    nch = len(chunks_of(u))
                    if i % 2 == 0:
                        S.op("act", lambda e: e.activation(out=pt.t[:, 0:nch, :], in_=pst.t[:, 0:nch, :], func=AF.Copy),
                             reads=[pst.b], writes=[pt.b])
                    else:
                        S.op("dve", lambda e: e.tensor_copy(out=pt.t[:, 0:nch, :], in_=pst.t[:, 0:nch, :]),
                             reads=[pst.b], writes=[pt.b])

                def stage_c2(u):
                    i, h = u["i"], u["h"]
                    pt = PT[i % NB]
                    chunks = chunks_of(u)
                    nch = len(chunks)
                    for j, (off, sz, vt, ti) in enumerate(chunks):
                        S.op("pe", lambda e: e.matmul(psO.t[:, h * 64:(h + 1) * 64], lhsT=pt.t[0:sz, j, :],
                                                      rhs=vt.t[0:sz, ti, h * 64:(h + 1) * 64],
                                                      start=(j == 0), stop=(j == nch - 1)),
                             reads=[pt.b, vt.b], writes=[psO.b])

                def T1(ti, m, col0, src_rows, dst_rows, ggt, L):
                    k2 = ti % 2
                    rs = rsum[ti % 3]
                    S.dma("sp", xr[k2].t[:], src_rows, writes=[xr[k2].b], sembuf=xr[k2].b)
                    S.op("dve", lambda e: e.reciprocal(out=rinv[k2].t[:], in_=rs.t[:]),
                         reads=[rs.b], writes=[rinv[k2].b])
                    S.op("dve", lambda e: e.tensor_tensor(
                        out=Osb[k2].t[:].rearrange("p (h d) -> p h d", h=8),
                        in0=psO.t[:].rearrange("p (h d) -> p h d", h=8),
                        in1=rinv[k2].t[:].unsqueeze(2).to_broadcast([128, 8, 64]), op=ALU.mult),
                        reads=[psO.b, rinv[k2].b], writes=[Osb[k2].b])

                def T2(ti, m, col0, src_rows, dst_rows, ggt, L):
                    k2 = ti % 2
                    S.op("act", lambda e: e.activation(out=junk.t[:], in_=Osb[k2].t[:], func=AF.Square,
                                                       accum_out=ss1[k2].t[:]),
                         reads=[Osb[k2].b], writes=[junk.b, ss1[k2].b])
                    rstd_lnexp(ss1[k2], rstd[k2], 1.0 / 512)

                def T3(ti, m, col0, src_rows, dst_rows, ggt, L):
                    k2 = ti % 2
                    S.op("dve", lambda e: e.scalar_tensor_tensor(out=On[k2].t[:], in0=Osb[k2].t[:],
                                                                 scalar=rstd[k2].t[:, 0:1], in1=goa.t[:],
                                                                 op0=ALU.mult, op1=ALU.mult),
                         reads=[Osb[k2].b, rstd[k2].b, goa.b], writes=[On[k2].b])

                def T4(ti, m, col0, src_rows, dst_rows, ggt, L):
                    k2 = ti % 2
                    pst = psT[(L + 7) % 2]
                    for c in range(4):
                        S.op("pe", lambda e: e.transpose(out=pst.t[:, c, :], in_=On[k2].t[:, c * 128:(c + 1) * 128],
                                                         identity=ident.t[:]),
                             reads=[On[k2].b, ident.b], writes=[pst.b])

                def T5(ti, m, col0, src_rows, dst_rows, ggt, L):
                    pst = psT[(L + 7) % 2]
                    S.op("act", lambda e: e.activation(out=m.t[:, 0:4, col0:col0 + 128], in_=pst.t[:, 0:4, :], func=AF.Copy),
                         reads=[pst.b], writes=[m.b])

                def T6(hf, ti, m, col0, src_rows, dst_rows, ggt, L):
                    for k in range(8):
                        S.op("pe", lambda e: e.matmul(psY.t[:], lhsT=m.t[:, k, col0:col0 + 128],
                                                      rhs=wo.t[:, k, hf * 512:(hf + 1) * 512],
                                                      start=(k == 0), stop=(k == 7)),
                             reads=[m.b, wo.b], writes=[psY.b])

                def T7(hf, ti, m, col0, src_rows, dst_rows, ggt, L):
                    k2 = ti % 2
                    S.op("act", lambda e: e.activation(out=ysb[k2].t[:, hf * 512:(hf + 1) * 512], in_=psY.t[:],
                                                       func=AF.Copy), reads=[psY.b], writes=[ysb[k2].b])
                    S.op("act", lambda e: e.activation(out=junk.t[:], in_=ysb[k2].t[:, hf * 512:(hf + 1) * 512],
                                                       func=AF.Square, accum_out=ss[k2].t[:, hf:hf + 1]),
                         reads=[ysb[k2].b], writes=[junk.b, ss[k2].b])

                def T10(ti, m, col0, src_rows, dst_rows, ggt, L):
                    k2 = ti % 2
                    S.op("dve", lambda e: e.tensor_tensor(out=ss1[k2].t[:], in0=ss[k2].t[:, 0:1], in1=ss[k2].t[:, 1:2],
                                                          op=ALU.add), reads=[ss[k2].b], writes=[ss1[k2].b])

                def T11(ti, m, col0, src_rows, dst_rows, ggt, L):
                    k2 = ti % 2
                    rstd_lnexp(ss1[k2], rstd[k2], 1.0 / D)

                def T12(ti, m, col0, src_rows, dst_rows, ggt, L):
                    k2 = ti % 2
                    S.op("dve", lambda e: e.scalar_tensor_tensor(
                        out=ysb[k2].t[:], in0=ysb[k2].t[:], scalar=rstd[k2].t[:, 0:1],
                        in1=ggt.t[:], op0=ALU.mult, op1=ALU.mult),
                        reads=[ysb[k2].b, rstd[k2].b, ggt.b], writes=[ysb[k2].b])
                    S.op("pool", lambda e: e.tensor_tensor(out=xr[k2].t[:], in0=ysb[k2].t[:], in1=xr[k2].t[:], op=ALU.add),
                         reads=[ysb[k2].b, xr[k2].b], writes=[xr[k2].b])
                    S.dma("sp", dst_rows, xr[k2].t[:], reads=[xr[k2].b], sembuf=xr[k2].b)

                units = []
                tails = {}
                groups = []
                tile_no = 0
                nbi = 0
                if not last:
                    csrc = ctx_in if l == 0 else ctxs
                    qa, qb_, m = QA[0], QB[0], mT[0]
                    groups.append((len(units),
                                   [(qa, lambda qa=qa: qa.t[0:64, :, 0:CTX], QcTd[:, 0:64, :].rearrange("c p t -> p c t")),
                                    (qb_, lambda qb_=qb_: qb_.t[64:128, :, 0:CTX], QcTd[:, 64:128, :].rearrange("c p t -> p c t"))],
                                   (m, lambda m=m: m.t[:, 4:8, 0:CTX], cncTd.rearrange("c p t -> p c t"))))
                    for ti in range(2):
                        for h in range(8):
                            pb = (h % 2) * 64
                            q = qa if h % 2 == 0 else qb_
                            units.append(dict(q=q.t[:, h // 2, ti * 128:(ti + 1) * 128], qb=q.b, pb=pb, chunk=h // 2,
                                              h=h, loc=None, edge=None, tile=tile_no))
                        tails[len(units) - 1] = (tile_no, m, ti * 128, csrc[ti * 128:(ti + 1) * 128, :],
                                                 ctxs[ti * 128:(ti + 1) * 128, :], ggt_c)
                        tile_no += 1
                xsrc = x_in if l == 0 else y_out
                for g in range(8):
                    qa, qb_, m = QA[(g + 1) % 2], QB[(g + 1) % 2], mT[(g + 1) % 2]
                    groups.append((len(units),
                                   [(qa, lambda qa=qa: qa.t[0:64, :, :],
                                     QTd[:, 0:64, g * 512:(g + 1) * 512].rearrange("c p t -> p c t")),
                                    (qb_, lambda qb_=qb_: qb_.t[64:128, :, :],
                                     QTd[:, 64:128, g * 512:(g + 1) * 512].rearrange("c p t -> p c t"))],
                                   (m, lambda m=m: m.t[:, 4:8, :], cnTd[:, :, g * 512:(g + 1) * 512].rearrange("c p t -> p c t"))))
                    for pi in range(4):
                        p = g * 4 + pi
                        if 2 <= p <= 29:
                            bs, nloc, edge = 2 * p - 4, 576, None
                        elif p < 2:
                            bs, nloc, edge = 0, 512, p
                        else:
                            bs, nloc, edge = 56, 512, p - 28
                        for h in range(8):
                            pb = (h % 2) * 64
                            q = qa if h % 2 == 0 else qb_
                            u = dict(q=q.t[:, h // 2, pi * 128:(pi + 1) * 128], qb=q.b, pb=pb, chunk=h // 2, h=h,
                                     loc=(bs * 64, nloc, bs // 2), edge=edge, tile=tile_no)
                            if edge is not None:
                                u["nbi"] = nbi
                                nbi += 1
                            units.append(u)
                        tails[len(units) - 1] = (tile_no, m, pi * 128, xsrc[p * 128:(p + 1) * 128, :],
                                                 y_out[p * 128:(p + 1) * 128, :], ggt_x)
                        tile_no += 1
                for i, u in enumerate(units):
                    u["i"] = i
                NU = len(units)
                pre = {}

                def at(step, fn):
                    pre.setdefault(max(step, 0), []).append(fn)

                def mk_load(spec):
                    tt, dst_fn, src_ap = spec
                    return lambda: S.dma("sp", dst_fn(), src_ap, writes=[tt.b], sembuf=tt.b)

                for gi_, (first, qspecs, mspec) in enumerate(groups):
                    if gi_ == 0:
                        for qs in qspecs:
                            at(0, mk_load(qs))
                        at(0, mk_load(mspec))
                    else:
                        pf = groups[gi_ - 1][0]
                        for qs in qspecs:
                            at(pf, mk_load(qs))
                        at(pf + 12, mk_load(mspec))
                for u in units:
                    if u["edge"] is not None:
                        def ld(u=u):
                            nb = nbe[u["nbi"] % 4]
                            S.dma("sp", nb.t[:], nab_edge[l, u["edge"], u["h"]], writes=[nb.b], sembuf=nb.b)
                        at(u["i"] - 2, ld)
                post = {}
                pre2 = {}
                for L, targs in tails.items():
                    A_ = lambda d, s, f, *x: d.setdefault(s, []).append(lambda f=f, x=x, targs=targs, L=L: f(*x, *targs, L))
                    A_(post, L + 4, T1)
                    A_(post, L + 5, T2)
                    A_(post, L + 6, T3)
                    A_(post, L + 7, T4)
                    A_(pre2, L + 8, T5)
                    A_(post, L + 8, T6, 0)
                    A_(pre2, L + 9, T7, 0)
                    A_(post, L + 9, T6, 1)
                    A_(pre2, L + 10, T7, 1)
                    A_(post, L + 10, T10)
                    A_(post, L + 11, T11)
                    A_(post, L + 12, T12)
                    post.setdefault(L + 12, []).append(lambda: conv_step(2))
                init_loads_first()
                for st in range(NU + 13):
                    for fn in pre.get(st, []):
                        fn()
                    if st == 0:
                        init_loads_rest()
                    for fn in pre2.get(st, []):
                        fn()
                    if 0 <= st - 4 < NU:
                        stage_c1b(units[st - 4])
                    if st < NU:
                        stage_a(units[st])
                    if 0 <= st - 1 < NU:
                        stage_b1(units[st - 1])
                    if 0 <= st - 2 < NU:
                        stage_b2(units[st - 2])
                    if 0 <= st - 3 < NU:
                        stage_c1(units[st - 3])
                    if 0 <= st - 4 < NU:
                        stage_c2(units[st - 4])
                    for fn in post.get(st, []):
                        fn()
                S.end_phase()

        def phase_f(l, last):
            es = ExitStack()
            with es:
                gsT = alloc(es, "fgs", [128, D], F32, dma=True)
                shT = alloc(es, "fsh", [128, D], F32, dma=True)
                ggt_x = load_bc(es, "fggt_x", 5)
                ggt_c = load_bc(es, "fggt_c", 6 + 5) if not last else None

                def load_rows(base):
                    S.dma("sp", gsT.t[:], _bc_rows(mods, base + 4, D), writes=[gsT.b], sembuf=gsT.b)
                    S.dma("sp", shT.t[:], _bc_rows(mods, base + 3, D), writes=[shT.b], sembuf=shT.b)

                tiles = []
                if not last:
                    for t in range(CTX // 128):
                        tiles.append(dict(rows=ctxs[t * 128:(t + 1) * 128, :], ctx=True))
                for p in range(S_LEN // 128):
                    tiles.append(dict(rows=y_out[p * 128:(p + 1) * 128, :], ctx=False))
                ngr = 4
                base_n, extra = len(tiles) // ngr, len(tiles) % ngr
                groups = []
                pos = 0
                for k in range(ngr):
                    n = base_n + (1 if k < extra else 0)
                    groups.append(dict(k=k, tiles=tiles[pos:pos + n]))
                    pos += n
                NTM = max(len(g["tiles"]) for g in groups)
                TM = NTM * 128
                for g in groups:
                    nt = len(g["tiles"])
                    g["nt"] = nt
                    g["splits"] = [(0, 512), (512, 512)] if nt == 8 else [(i * 384, 384) for i in range(nt * 128 // 384)]
                    assert sum(w for _, w in g["splits"]) == nt * 128
                    bl = []
                    for i, td in enumerate(g["tiles"]):
                        if bl and len(bl[-1]) < 4 and g["tiles"][bl[-1][0]]["ctx"] == td["ctx"]:
                            bl[-1].append(i)
                        else:
                            bl.append([i])
                    g["batches"] = bl

                xts = allocn(es, 4, "fxt", [128, D], F32, dma=True)
                junk = alloc(es, "fjunk", [128, D], BF16)
                tmp = allocn(es, 2, "ftmp", [128, D], F32)
                hb = allocn(es, 4, "fhb", [128, D], BF16)
                ss4 = allocn(es, 2, "fss4", [128, 4], F32)
                rs4 = allocn(es, 2, "frs4", [128, 4], F32)
                sse = alloc(es, "fsse", [128, NTM], F32)
                rse = alloc(es, "frse", [128, NTM], F32)
                hT = allocn(es, 2, "fhT", [128, 8, TM], BF16)
                tps = allocn(es, 2, "ftp", [128, 8, 128], BF16, psum=True)
                psG = allocn(es, 2, "psG", [128, 512], F32, psum=True)
                psU = allocn(es, 2, "psU", [128, 512], F32, psum=True)
                psD = allocn(es, 2, "psD", [128, 512], F32, psum=True)
                wg = allocn(es, 2, "wg", [128, 8, 512], BF16, dma="pool")
                wu = allocn(es, 2, "wu", [128, 8, 512], BF16, dma="pool")
                wd = allocn(es, 2, "wd", [128, 4, D], BF16, dma="pool")
                sg = allocn(es, 2, "sg", [128, 512], F32)
                aT = allocn(es, 2, "aT", [128, 4, TM], BF16)
                acc = alloc(es, "facc", [128, NTM, D], F32)
                cnt = dict(x=0, h=0, b=0, w=0, gu=0, d=0, a=0, t=0)
                cur_kind = [None]

                def load_batch(g, bi):
                    xl = []
                    for i in g["batches"][bi]:
                        xt = xts[cnt["x"] % 4]
                        cnt["x"] += 1
                        xl.append(xt)
                        S.dma("sp", xt.t[:], g["tiles"][i]["rows"], writes=[xt.b], sembuf=xt.b)
                    g.setdefault("xl", {})[bi] = xl

                def norm_batch(g, bi):
                    idx = g["batches"][bi]
                    kind = g["tiles"][idx[0]]["ctx"]
                    if cur_kind[0] != kind:
                        load_rows(6 if kind else 0)
                        cur_kind[0] = kind
                    kb = cnt["b"] % 2
                    cnt["b"] += 1
                    ssb, rsb = ss4[kb], rs4[kb]
                    xl = g["xl"][bi]
                    for j, i in enumerate(idx):
                        xt = xl[j]
                        S.op("act", lambda e: e.activation(out=junk.t[:], in_=xt.t[:], func=AF.Square,
                                                           accum_out=ssb.t[:, j:j + 1]),
                             reads=[xt.b], writes=[junk.b, ssb.b])
                    nbt = len(idx)
                    S.op("act", lambda e: e.activation(out=rsb.t[:, 0:nbt], in_=ssb.t[:, 0:nbt], func=AF.Sqrt,
                                                       scale=1.0 / D, bias=epsb.t[:]),
                         reads=[ssb.b, epsb.b], writes=[rsb.b])
                    S.op("dve", lambda e: e.reciprocal(out=rsb.t[:, 0:nbt], in_=rsb.t[:, 0:nbt]),
                         reads=[rsb.b], writes=[rsb.b])
                    hl = []
                    for j, i in enumerate(idx):
                        xt = xl[j]
                        tm = tmp[j % 2]
                        h_ = hb[cnt["h"] % 4]
                        cnt["h"] += 1
                        hl.append((i, h_))
                        S.op("dve", lambda e: e.scalar_tensor_tensor(out=tm.t[:], in0=xt.t[:], scalar=rsb.t[:, j:j + 1],
                                                                     in1=gsT.t[:], op0=ALU.mult, op1=ALU.mult),
                             reads=[xt.b, rsb.b, gsT.b], writes=[tm.b])
                        S.op("pool", lambda e: e.tensor_tensor(out=h_.t[:], in0=tm.t[:], in1=shT.t[:], op=ALU.add),
                             reads=[tm.b, shT.b], writes=[h_.b])
                    g.setdefault("hl", {})[bi] = hl

                def trans_batch(g, bi):
                    h = hT[g["k"] % 2]
                    for (i, h_) in g["hl"][bi]:
                        tp = tps[cnt["t"] % 2]
                        cnt["t"] += 1
                        for c in range(8):
                            S.op("pe", lambda e: e.transpose(out=tp.t[:, c, :], in_=h_.t[:, c * 128:(c + 1) * 128],
                                                             identity=ident.t[:]),
                                 reads=[h_.b, ident.b], writes=[tp.b])
                        S.op("act", lambda e: e.activation(out=h.t[:, :, i * 128:(i + 1) * 128], in_=tp.t[:],
                                                           func=AF.Copy), reads=[tp.b], writes=[h.b])

                def epilogue(g):
                    nt = g["nt"]
                    for i in range(nt):
                        S.op("act", lambda e: e.activation(out=junk.t[:], in_=acc.t[:, i, :], func=AF.Square,
                                                           accum_out=sse.t[:, i:i + 1]),
                             reads=[acc.b], writes=[junk.b, sse.b])
                    S.op("act", lambda e: e.activation(out=rse.t[:, 0:nt], in_=sse.t[:, 0:nt], func=AF.Sqrt,
                                                       scale=1.0 / D, bias=epsb.t[:]),
                         reads=[sse.b, epsb.b], writes=[rse.b])
                    S.op("dve", lambda e: e.reciprocal(out=rse.t[:, 0:nt], in_=rse.t[:, 0:nt]),
                         reads=[rse.b], writes=[rse.b])

                    def issue(i):
                        xt = xts[cnt["x"] % 4]
                        cnt["x"] += 1
                        S.dma("sp", xt.t[:], g["tiles"][i]["rows"], writes=[xt.b], sembuf=xt.b)
                        return xt
                    q = [issue(i) for i in range(min(3, nt))]
                    for i in range(nt):
                        xt = q.pop(0)
                        tm = tmp[i % 2]
                        ggt = ggt_c if g["tiles"][i]["ctx"] else ggt_x
                        S.op("dve", lambda e: e.scalar_tensor_tensor(out=tm.t[:], in0=acc.t[:, i, :],
                                                                     scalar=rse.t[:, i:i + 1], in1=ggt.t[:],
                                                                     op0=ALU.mult, op1=ALU.mult),
                             reads=[acc.b, rse.b, ggt.b], writes=[tm.b])
                        S.op("pool", lambda e: e.tensor_tensor(out=xt.t[:], in0=tm.t[:], in1=xt.t[:], op=ALU.add),
                             reads=[tm.b, xt.b], writes=[xt.b])
                        S.dma("sp", g["tiles"][i]["rows"], xt.t[:], reads=[xt.b], sembuf=xt.b)
                        if i + 3 < nt:
                            q.append(issue(i + 3))

                wgu, wdn = w_gu_d[l // 2], w_dn_d[l // 2]

                def wload(c):
                    kw = cnt["w"] % 2
                    cnt["w"] += 1
                    S.dma("pool", wg[kw].t[:], wgu[:, c * 512:(c + 1) * 512].rearrange("(c p) f -> p c f", p=128),
                          writes=[wg[kw].b], sembuf=wg[kw].b)
                    S.dma("pool", wu[kw].t[:],
                          wgu[:, FF + c * 512:FF + (c + 1) * 512].rearrange("(c p) f -> p c f", p=128),
                          writes=[wu[kw].b], sembuf=wu[kw].b)
                    S.dma("pool", wd[kw].t[:], wdn[c * 512:(c + 1) * 512, :].rearrange("(j p) d -> p j d", p=128),
                          writes=[wd[kw].b], sembuf=wd[kw].b)
                    return kw

                def gu_block(g, kw, a, j, c0, cwd):
                    h = hT[g["k"] % 2]
                    kk = cnt["gu"] % 2
                    cnt["gu"] += 1
                    pg, pu, sgt = psG[kk], psU[kk], sg[kk]
                    for ch in range(8):
                        S.op("pe", lambda e: e.matmul(pg.t[:, 0:cwd], lhsT=wg[kw].t[:, ch, j * 128:(j + 1) * 128],
                                                      rhs=h.t[:, ch, c0:c0 + cwd],
                                                      start=(ch == 0), stop=(ch == 7)),
                             reads=[wg[kw].b, h.b], writes=[pg.b])
                    for ch in range(8):
                        S.op("pe", lambda e: e.matmul(pu.t[:, 0:cwd], lhsT=wu[kw].t[:, ch, j * 128:(j + 1) * 128],
                                                      rhs=h.t[:, ch, c0:c0 + cwd],
                                                      start=(ch == 0), stop=(ch == 7)),
                             reads=[wu[kw].b, h.b], writes=[pu.b])
                    S.op("act", lambda e: e.activation(out=sgt.t[:, 0:cwd], in_=pg.t[:, 0:cwd], func=AF.Silu),
                         reads=[pg.b], writes=[sgt.b])
                    S.op("dve", lambda e: e.tensor_tensor(out=a.t[:, j, c0:c0 + cwd],
                                                          in0=pu.t[:, 0:cwd], in1=sgt.t[:, 0:cwd], op=ALU.mult),
                         reads=[pu.b, sgt.b], writes=[a.b])

                def down(g, c, kw, a):
                    for i in range(g["nt"]):
                        for dh in range(2):
                            pd = psD[cnt["d"] % 2]
                            cnt["d"] += 1
                            for j in range(4):
                                S.op("pe", lambda e: e.matmul(pd.t[:], lhsT=a.t[:, j, i * 128:(i + 1) * 128],
                                                              rhs=wd[kw].t[:, j, dh * 512:(dh + 1) * 512],
                                                              start=(j == 0), stop=(j == 3)),
                                     reads=[a.b, wd[kw].b], writes=[pd.b])
                            av = acc.t[:, i, dh * 512:(dh + 1) * 512]
                            if c == 0:
                                S.op("dve", lambda e: e.tensor_copy(out=av, in_=pd.t[:]), reads=[pd.b], writes=[acc.b])
                            else:
                                S.op("dve", lambda e: e.tensor_tensor(out=av, in0=pd.t[:], in1=av, op=ALU.add),
                                     reads=[pd.b, acc.b], writes=[acc.b])

                g0 = groups[0]
                for bi in range(len(g0["batches"])):
                    load_batch(g0, bi)
                    norm_batch(g0, bi)
                    trans_batch(g0, bi)
                pend_d = None
                NCH = 7 * len(groups)
                kw_of = {0: wload(0), 1: wload(1)}
                for k, g in enumerate(groups):
                    nxt = groups[k + 1] if k + 1 < len(groups) else None
                    nbn = len(nxt["batches"]) if nxt is not None else 0
                    for c in range(7):
                        t = 7 * k + c
                        kw = kw_of[t]
                        a = aT[cnt["a"] % 2]
                        cnt["a"] += 1
                        blocks = [(j, c0, cwd) for j in range(4) for (c0, cwd) in g["splits"]]
                        gu_block(g, kw, a, *blocks[0])
                        if pend_d is not None:
                            down(*pend_d)
                            if t + 1 < NCH:
                                kw_of[t + 1] = wload((t + 1) % 7)
                        conv_step(1)
                        if c == 0 and k > 0:
                            epilogue(groups[k - 1])
                        if 0 <= c - 2 < nbn:
                            trans_batch(nxt, c - 2)
                        for blk in blocks[1:]:
                            gu_block(g, kw, a, *blk)
                        if 0 <= c - 1 < nbn:
                            norm_batch(nxt, c - 1)
                        if c < nbn:
                            load_batch(nxt, c)
                        pend_d = (g, c, kw, a)
                down(*pend_d)
                epilogue(groups[-1])
                S.end_phase()


        def phase_moe(l):
            es0 = ExitStack()
            with es0:
                slot = alloc(es0, "slot", [128, 32, 2], U32)
                gates = alloc(es0, "gates", [128, 32, 2], F32)
                idxw = alloc(es0, "idxw", [128, NSEG * 7], U32)
                es = ExitStack()
                with es:
                    gs_x = load_bc(es, "mgs_x", 4)
                    sh_x = load_bc(es, "msh_x", 3)
                    hb_all = alloc(es, "hb_all", [128, 32, D], BF16)
                    xts = allocn(es, 4, "mxt", [128, D], F32, dma=True)
                    junk = alloc(es, "mjunk", [128, D], BF16)
                    tmp = allocn(es, 2, "mtmp", [128, D], F32)
                    hb32 = allocn(es, 3, "mhb32", [128, D], F32)
                    sss = allocn(es, 2, "mss", [128, 1], F32)
                    rstds = allocn(es, 2, "mrstd", [128, 1], F32)
                    tp32 = allocn(es, 2, "mtp32", [128, 4, 128], F32, psum=True)
                    pl = allocn(es, 2, "mpl", [128, 512], F32, psum=True)
                    hT32 = allocn(es, 2, "mhT32", [128, 8, 128], F32)
                    wr = alloc(es, "mwr", [128, 8, NE], F32, dma=True)
                    S.dma("sp", wr.t[:], w_rt, writes=[wr.b], sembuf=wr.b)
                    M1 = alloc(es, "M1", [128, 32, NE], F32)
                    M2 = alloc(es, "M2", [128, 32, NE], F32)
                    M12 = allocn(es, 2, "M12", [128, NE], F32)
                    rt = alloc(es, "rt", [128, 32, 2 * NE], F32)
                    cum = alloc(es, "cum", [128, 32, NE], F32)
                    rank = alloc(es, "rank", [128, 32, NE], F32)
                    lg = allocn(es, 2, "lg", [128, NE], F32)
                    l2 = allocn(es, 2, "l2", [128, NE], F32)
                    m1 = allocn(es, 2, "m1", [128, 1], F32)
                    m2 = allocn(es, 2, "m2", [128, 1], F32)
                    dd = alloc(es, "dd", [128, 32], F32)
                    scb = S.buf(dma="pool")
                    V_ = lambda fn, r, w: S.op("dve", fn, reads=r, writes=w)
                    G_ = lambda fn, r, w: S.op("pool", fn, reads=r, writes=w)
                    NT = 32

                    def r_load(i):
                        xt = xts[i % 4]
                        S.dma("sp", xt.t[:], y_out[i * 128:(i + 1) * 128, :], writes=[xt.b], sembuf=xt.b)

                    def r_norm(i):
                        k2 = i % 2
                        xt = xts[i % 4]
                        h32 = hb32[i % 3]
                        S.op("act", lambda e: e.activation(out=junk.t[:], in_=xt.t[:], func=AF.Square,
                                                           accum_out=sss[k2].t[:]),
                             reads=[xt.b], writes=[junk.b, sss[k2].b])
                        rstd_from_ss(sss[k2], rstds[k2], 1.0 / D)
                        V_(lambda e: e.scalar_tensor_tensor(out=tmp[k2].t[:], in0=xt.t[:], scalar=rstds[k2].t[:, 0:1],
                                                            in1=gs_x.t[:], op0=ALU.mult, op1=ALU.mult),
                           [xt.b, rstds[k2].b, gs_x.b], [tmp[k2].b])
                        G_(lambda e: e.tensor_tensor(out=h32.t[:], in0=tmp[k2].t[:], in1=sh_x.t[:], op=ALU.add),
                           [tmp[k2].b, sh_x.b], [h32.b])
                        S.op("act", lambda e: e.activation(out=hb_all.t[:, i, :], in_=h32.t[:], func=AF.Copy),
                             reads=[h32.b], writes=[hb_all.b])

                    def r_trans(i):
                        k2 = i % 2
                        h32 = hb32[i % 3]
                        for half in range(2):
                            tpp = tp32[half]
                            for c in range(4):
                                cc = half * 4 + c
                                S.op("pe", lambda e: e.transpose(out=tpp.t[:, c, :], in_=h32.t[:, cc * 128:(cc + 1) * 128],
                                                                 identity=ident32.t[:]),
                                     reads=[h32.b, ident32.b], writes=[tpp.b])
                            S.op("act", lambda e: e.activation(out=hT32[k2].t[:, half * 4:half * 4 + 4, :], in_=tpp.t[:],
                                                               func=AF.Copy), reads=[tpp.b], writes=[hT32[k2].b])

                    def r_route(i):
                        k2 = i % 2
                        p = pl[k2]
                        for c in range(8):
                            S.op("pe", lambda e: e.matmul(p.t[:, 0:NE], lhsT=hT32[k2].t[:, c, :], rhs=wr.t[:, c, :],
                                                          start=(c == 0), stop=(c == 7)),
                                 reads=[hT32[k2].b, wr.b], writes=[p.b])
                        lgk, l2k, m1k, m2k = lg[k2], l2[k2], m1[k2], m2[k2]
                        V_(lambda e: e.tensor_copy(out=lgk.t[:], in_=p.t[:, 0:NE]), [p.b], [lgk.b])
                        V_(lambda e: e.reduce_max(out=m1k.t[:], in_=lgk.t[:], axis=AX.X), [lgk.b], [m1k.b])
                        V_(lambda e: e.tensor_scalar(out=M1.t[:, i, :], in0=lgk.t[:], scalar1=m1k.t[:, 0:1], scalar2=None,
                                                     op0=ALU.is_ge), [lgk.b, m1k.b], [M1.b])
                        V_(lambda e: e.scalar_tensor_tensor(out=l2k.t[:], in0=M1.t[:, i, :], scalar=-1e30, in1=lgk.t[:],
                                                            op0=ALU.mult, op1=ALU.add), [M1.b, lgk.b], [l2k.b])
                        V_(lambda e: e.reduce_max(out=m2k.t[:], in_=l2k.t[:], axis=AX.X), [l2k.b], [m2k.b])
                        V_(lambda e: e.tensor_scalar(out=M2.t[:, i, :], in0=l2k.t[:], scalar1=m2k.t[:, 0:1], scalar2=None,
                                                     op0=ALU.is_ge), [l2k.b, m2k.b], [M2.b])
                        V_(lambda e: e.tensor_tensor(out=dd.t[:, i:i + 1], in0=m1k.t[:], in1=m2k.t[:], op=ALU.subtract),
                           [m1k.b, m2k.b], [dd.b])
                        mk = M12[k2]
                        V_(lambda e: e.tensor_tensor(out=mk.t[:], in0=M1.t[:, i, :], in1=M2.t[:, i, :], op=ALU.add),
                           [M1.b, M2.b], [mk.b])
                        S.op("pe", lambda e: e.matmul(p.t[:, 64:64 + NE], lhsT=tri32.t[:], rhs=mk.t[:], start=True, stop=True),
                             reads=[tri32.b, mk.b], writes=[p.b])
                        S.op("pe", lambda e: e.matmul(p.t[:, 64 + NE:64 + 2 * NE], lhsT=ones32.t[:], rhs=mk.t[:],
                                                      start=True, stop=True),
                             reads=[ones32.b, mk.b], writes=[p.b])
                        V_(lambda e: e.tensor_copy(out=rt.t[:, i, :], in_=p.t[:, 64:64 + 2 * NE]), [p.b], [rt.b])

                    for i in range(min(3, NT)):
                        r_load(i)
                    for st in range(NT + 2):
                        if st + 3 < NT:
                            r_load(st + 3)
                        if st < NT:
                            r_norm(st)
                        if 0 <= st - 1 < NT:
                            r_trans(st - 1)
                        if 0 <= st - 2 < NT:
                            r_route(st - 2)
                    S.op("act", lambda e: e.activation(out=gates.t[:, :, 0], in_=dd.t[:, :], func=AF.Sigmoid),
                         reads=[dd.b], writes=[gates.b])
                    V_(lambda e: e.tensor_scalar(out=gates.t[:, :, 1], in0=gates.t[:, :, 0], scalar1=-1.0, scalar2=1.0,
                                                 op0=ALU.mult, op1=ALU.add), [gates.b], [gates.b])
                    G_(lambda e: e.memset(cum.t[:, 0, :], 0.0), [], [cum.b])
                    for i in range(1, NT):
                        V_(lambda e: e.tensor_tensor(out=cum.t[:, i, :], in0=cum.t[:, i - 1, :], in1=rt.t[:, i - 1, NE:2 * NE],
                                                     op=ALU.add), [cum.b, rt.b], [cum.b])
                    V_(lambda e: e.tensor_tensor(out=rank.t[:], in0=rt.t[:, :, 0:NE], in1=cum.t[:], op=ALU.add),
                       [rt.b, cum.b], [rank.b])
                    nb = alloc(es, "nb", [128, NE], F32)
                    pad = alloc(es, "pad", [128, NE], F32)
                    pend = alloc(es, "pend", [128, NE], F32)
                    off = alloc(es, "off", [128, NE], F32)
                    V_(lambda e: e.tensor_tensor(out=nb.t[:], in0=cum.t[:, NT - 1, :], in1=rt.t[:, NT - 1, NE:2 * NE],
                                                 op=ALU.add), [cum.b, rt.b], [nb.b])
                    cmp0 = alloc(es, "cmp0", [128, NE], F32)
                    V_(lambda e: e.tensor_single_scalar(out=pad.t[:], in_=nb.t[:], scalar=0.0, op=ALU.is_gt), [nb.b], [pad.b])
                    for kq in range(1, 8):
                        V_(lambda e: e.tensor_single_scalar(out=cmp0.t[:], in_=nb.t[:], scalar=float(512 * kq), op=ALU.is_gt),
                           [nb.b], [cmp0.b])
                        V_(lambda e: e.tensor_tensor(out=pad.t[:], in0=pad.t[:], in1=cmp0.t[:], op=ALU.add),
                           [pad.b, cmp0.b], [pad.b])
                    V_(lambda e: e.tensor_single_scalar(out=pad.t[:], in_=pad.t[:], scalar=512.0, op=ALU.mult), [pad.b], [pad.b])
                    V_(lambda e: e.tensor_copy(out=pend.t[:, 0:1], in_=pad.t[:, 0:1]), [pad.b], [pend.b])
                    for ex in range(1, NE):
                        V_(lambda e: e.tensor_tensor(out=pend.t[:, ex:ex + 1], in0=pend.t[:, ex - 1:ex], in1=pad.t[:, ex:ex + 1],
                                                     op=ALU.add), [pend.b, pad.b], [pend.b])
                    V_(lambda e: e.tensor_tensor(out=off.t[:], in0=pend.t[:], in1=pad.t[:], op=ALU.subtract),
                       [pend.b, pad.b], [off.b])
                    V_(lambda e: e.tensor_tensor(out=rank.t[:], in0=rank.t[:],
                                                 in1=off.t[:].unsqueeze(1).to_broadcast([128, 32, NE]), op=ALU.add),
                       [rank.b, off.b], [rank.b])
                    prod = alloc(es, "prod", [128, 32, NE], F32)
                    slotf = alloc(es, "slotf", [128, 32, 2], F32)
                    for k, Mk in enumerate((M1, M2)):
                        V_(lambda e: e.tensor_tensor(out=prod.t[:], in0=rank.t[:], in1=Mk.t[:], op=ALU.mult),
                           [rank.b, Mk.b], [prod.b])
                        V_(lambda e: e.reduce_sum(out=slotf.t[:, :, k], in_=prod.t[:], axis=AX.X), [prod.b], [slotf.b])
                    V_(lambda e: e.tensor_copy(out=slot.t[:], in_=slotf.t[:]), [slotf.b], [slot.b])
                    eseg = alloc(es, "eseg", [128, NSEG], F32)
                    cmpt = alloc(es, "cmpt", [128, NE], F32)
                    pci = alloc(es, "pci", [128, 7], I32)
                    pcf = alloc(es, "pcf", [128, 7], F32)
                    idxf = alloc(es, "idxf", [128, NSEG, 7], F32)
                    for sg_ in range(NSEG):
                        V_(lambda e: e.tensor_single_scalar(out=cmpt.t[:], in_=pend.t[:], scalar=float(512 * sg_), op=ALU.is_le),
                           [pend.b], [cmpt.b])
                        V_(lambda e: e.reduce_sum(out=eseg.t[:, sg_:sg_ + 1], in_=cmpt.t[:], axis=AX.X), [cmpt.b], [eseg.b])
                    V_(lambda e: e.tensor_scalar(out=eseg.t[:], in0=eseg.t[:], scalar1=float(NE - 1), scalar2=896.0,
                                                 op0=ALU.min, op1=ALU.mult), [eseg.b], [eseg.b])
                    G_(lambda e: e.iota(pci.t[:], pattern=[[128, 7]], base=0, channel_multiplier=1), [], [pci.b])
                    V_(lambda e: e.tensor_copy(out=pcf.t[:], in_=pci.t[:]), [pci.b], [pcf.b])
                    for sg_ in range(NSEG):
                        V_(lambda e: e.tensor_scalar(out=idxf.t[:, sg_, :], in0=pcf.t[:], scalar1=eseg.t[:, sg_:sg_ + 1],
                                                     scalar2=None, op0=ALU.add), [pcf.b, eseg.b], [idxf.b])
                    V_(lambda e: e.tensor_copy(out=idxw.t[:], in_=idxf.t[:].rearrange("p s c -> p (s c)")), [idxf.b], [idxw.b])
                    for i in range(32):
                        for k in range(2):
                            S.dma_fn("pool", lambda e: e.indirect_dma_start(
                                out=hs, out_offset=bass.IndirectOffsetOnAxis(ap=slot.t[:, i, k:k + 1], axis=0),
                                in_=hb_all.t[:, i, :], in_offset=None),
                                reads=[slot.b, hb_all.b, hsz], sembuf=scb)
                    if dbg is not None and dbg.get("moe_dump"):
                        S.dma("sp", dbg_out[:, 0:64], slot.t[:].rearrange("p i k -> p (i k)").bitcast(F32), reads=[slot.b], sembuf=xts[0].b)
                        S.dma("sp", dbg_out[:, 64:128], gates.t[:].rearrange("p i k -> p (i k)"), reads=[gates.b], sembuf=xts[0].b)
                        S.dma("sp", dbg_out[:, 128:128 + NSEG * 7], idxw.t[:].bitcast(F32), reads=[idxw.b], sembuf=xts[0].b)
                    S.end_phase()
                conv_step(10 ** 6)
                es = ExitStack()
                with es:
                    hsl = allocn(es, 4, "hsl", [128, D], BF16, dma=True)
                    hT = allocn(es, 2, "shT", [128, 8, 512], BF16)
                    tp = allocn(es, 2, "stp", [128, 8, 128], BF16, psum=True)
                    psG = allocn(es, 2, "spsG", [128, 512], F32, psum=True)
                    psU = allocn(es, 2, "spsU", [128, 512], F32, psum=True)
                    psD = allocn(es, 2, "spsD", [128, 512], F32, psum=True)
                    wg = allocn(es, 2, "swg", [128, 8, 512], BF16, dma="pool")
                    wu = allocn(es, 2, "swu", [128, 8, 512], BF16, dma="pool")
                    wd = allocn(es, 2, "swd", [128, 4, D], BF16, dma="pool")
                    sg = allocn(es, 2, "ssg", [128, 512], F32)
                    aT = allocn(es, 2, "saT", [128, 4, 512], BF16)
                    acc = allocn(es, 2, "sacc", [128, 4, D], F32, dma=True)
                    cnt = dict(x=0, w=0, gu=0, d=0, a=0)

                    def prep_load(seg):
                        for i in range(4):
                            hl = hsl[i]
                            S.dma("sp", hl.t[:], hs[seg * 512 + i * 128:seg * 512 + (i + 1) * 128, :], writes=[hl.b], sembuf=hl.b)

                    def prep_trans(seg):
                        h = hT[seg % 2]
                        for i in range(4):
                            hl = hsl[i]
                            t = tp[i % 2]
                            for c in range(8):
                                S.op("pe", lambda e: e.transpose(out=t.t[:, c, :], in_=hl.t[:, c * 128:(c + 1) * 128],
                                                                 identity=ident.t[:]),
                                     reads=[hl.b, ident.b], writes=[t.b])
                            S.op("act", lambda e: e.activation(out=h.t[:, :, i * 128:(i + 1) * 128], in_=t.t[:], func=AF.Copy),
                                 reads=[t.b], writes=[h.b])

                    def wload(seg, c):
                        k = cnt["w"] % 2
                        cnt["w"] += 1
                        ia = idxw.t[:, seg * 7 + c:seg * 7 + c + 1]
                        for (dst, src) in ((wg[k], WGd), (wu[k], WUd), (wd[k], WDd)):
                            S.dma_fn("pool", lambda e: e.indirect_dma_start(
                                out=dst.t[:].rearrange("p a b -> p (a b)"), out_offset=None, in_=src,
                                in_offset=bass.IndirectOffsetOnAxis(ap=ia, axis=0)),
                                reads=[idxw.b, convb], writes=[dst.b], sembuf=dst.b)
                        return k

                    def gu(seg, k, a, j):
                        h = hT[seg % 2]
                        kk = cnt["gu"] % 2
                        cnt["gu"] += 1
                        pg, pu, sgt = psG[kk], psU[kk], sg[kk]
                        for ch in range(8):
                            S.op("pe", lambda e: e.matmul(pg.t[:], lhsT=wg[k].t[:, ch, j * 128:(j + 1) * 128],
                                                          rhs=h.t[:, ch, :], start=(ch == 0), stop=(ch == 7)),
                                 reads=[wg[k].b, h.b], writes=[pg.b])
                        for ch in range(8):
                            S.op("pe", lambda e: e.matmul(pu.t[:], lhsT=wu[k].t[:, ch, j * 128:(j + 1) * 128],
                                                          rhs=h.t[:, ch, :], start=(ch == 0), stop=(ch == 7)),
                                 reads=[wu[k].b, h.b], writes=[pu.b])
                        S.op("act", lambda e: e.activation(out=sgt.t[:], in_=pg.t[:], func=AF.Silu),
                             reads=[pg.b], writes=[sgt.b])
                        S.op("dve", lambda e: e.tensor_tensor(out=a.t[:, j, :], in0=pu.t[:], in1=sgt.t[:], op=ALU.mult),
                             reads=[pu.b, sgt.b], writes=[a.b])

                    def down(seg, c, k, a):
                        ac = acc[seg % 2]
                        for i in range(4):
                            for dh in range(2):
                                pd = psD[cnt["d"] % 2]
                                cnt["d"] += 1
                                for j in range(4):
                                    S.op("pe", lambda e: e.matmul(pd.t[:], lhsT=a.t[:, j, i * 128:(i + 1) * 128],
                                                                  rhs=wd[k].t[:, j, dh * 512:(dh + 1) * 512],
                                                                  start=(j == 0), stop=(j == 3)),
                                         reads=[a.b, wd[k].b], writes=[pd.b])
                                av = ac.t[:, i, dh * 512:(dh + 1) * 512]
                                if c == 0:
                                    S.op("dve", lambda e: e.tensor_copy(out=av, in_=pd.t[:]), reads=[pd.b], writes=[ac.b])
                                else:
                                    S.op("dve", lambda e: e.tensor_tensor(out=av, in0=pd.t[:], in1=av, op=ALU.add),
                                         reads=[pd.b, ac.b], writes=[ac.b])
                        if c == 6:
                            S.dma("sp", ys[seg * 512:(seg + 1) * 512, :].rearrange("(i p) d -> p i d", p=128), ac.t[:],
                                  reads=[ac.b], sembuf=ac.b)

                    prep_load(0)
                    prep_trans(0)
                    pend_d = None
                    for seg in range(NSEG):
                        for c in range(7):
                            k = wload(seg, c)
                            a = aT[cnt["a"] % 2]
                            cnt["a"] += 1
                            gu(seg, k, a, 0)
                            if pend_d is not None:
                                down(*pend_d)
                            for j in range(1, 4):
                                gu(seg, k, a, j)
                            pend_d = (seg, c, k, a)
                            if seg + 1 < NSEG:
                                if c == 1:
                                    prep_load(seg + 1)
                                if c == 4:
                                    prep_trans(seg + 1)
                    down(*pend_d)
                    S.end_phase()
                es = ExitStack()
                with es:
                    ggt_x = load_bc(es, "cggt_x", 5)
                    NBUF = 4
                    r1 = allocn(es, NBUF, "r1", [128, D], F32, dma="pool")
                    r2 = allocn(es, NBUF, "r2", [128, D], F32, dma="pool")
                    xts = allocn(es, NBUF, "cxt", [128, D], F32, dma=True)
                    a1 = allocn(es, 2, "a1", [128, D], F32)
                    junk = alloc(es, "cjunk", [128, D], BF16)
                    sss = allocn(es, 2, "css", [128, 1], F32)
                    rstds = allocn(es, 2, "crstd", [128, 1], F32)
                    tmp = allocn(es, 2, "ctmp", [128, D], F32)

                    def c_issue(i):
                        kb = i % NBUF
                        for (r, k) in ((r1[kb], 0), (r2[kb], 1)):
                            S.dma_fn("pool", lambda e: e.indirect_dma_start(
                                out=r.t[:], out_offset=None, in_=ys,
                                in_offset=bass.IndirectOffsetOnAxis(ap=slot.t[:, i, k:k + 1], axis=0)),
                                reads=[slot.b], writes=[r.b], sembuf=r.b)
                        xt = xts[kb]
                        S.dma("sp", xt.t[:], y_out[i * 128:(i + 1) * 128, :], writes=[xt.b], sembuf=xt.b)

                    for i in range(NBUF - 1):
                        c_issue(i)
                    for i in range(32):
                        if i + NBUF - 1 < 32:
                            c_issue(i + NBUF - 1)
                        k2 = i % 2
                        kb = i % NBUF
                        xt = xts[kb]
                        S.op("act", lambda e: e.activation(out=a1[k2].t[:], in_=r1[kb].t[:], func=AF.Copy,
                                                           scale=gates.t[:, i, 0:1]),
                             reads=[r1[kb].b, gates.b], writes=[a1[k2].b])
                        S.op("dve", lambda e: e.scalar_tensor_tensor(out=a1[k2].t[:], in0=r2[kb].t[:], scalar=gates.t[:, i, 1:2],
                                                                     in1=a1[k2].t[:], op0=ALU.mult, op1=ALU.add),
                             reads=[r2[kb].b, gates.b, a1[k2].b], writes=[a1[k2].b])
                        S.op("act", lambda e: e.activation(out=junk.t[:], in_=a1[k2].t[:], func=AF.Square,
                                                           accum_out=sss[k2].t[:]),
                             reads=[a1[k2].b], writes=[junk.b, sss[k2].b])
                        rstd_from_ss(sss[k2], rstds[k2], 1.0 / D)
                        S.op("dve", lambda e: e.scalar_tensor_tensor(out=tmp[k2].t[:], in0=a1[k2].t[:],
                                                                     scalar=rstds[k2].t[:, 0:1], in1=ggt_x.t[:],
                                                                     op0=ALU.mult, op1=ALU.mult),
                             reads=[a1[k2].b, rstds[k2].b, ggt_x.b], writes=[tmp[k2].b])
                        S.op("pool", lambda e: e.tensor_tensor(out=xt.t[:], in0=tmp[k2].t[:], in1=xt.t[:], op=ALU.add),
                             reads=[tmp[k2].b, xt.b], writes=[xt.b])
                        S.dma("sp", y_out[i * 128:(i + 1) * 128, :], xt.t[:], reads=[xt.b], sembuf=xt.b)
                    S.end_phase()

        stop_after = dbg.get("stop") if dbg else None
        done = False
        for l in range(DEPTH):
            last = (l == DEPTH - 1)
            for name, fn in (("ada", lambda: phase_ada(l)), ("a", lambda: phase_a(l, last)),
                             ("b", lambda: phase_b(l, last)),
                             ("f", (lambda: phase_moe(l)) if (l % 2 == 1 and last) else (lambda: phase_f(l, last)))):
                fn()
                if stop_after == (l, name):
                    done = True
                    break
            if done:
                break
        if dbg is not None:
            es = ExitStack()
            with es:
                srcd = dict(mods=mods, QTd=QTd, KTd=KTd, Vd=Vd, cnTd=cnTd, QcTd=QcTd, KcTd=KcTd, Vcd=Vcd,
                            cncTd=cncTd, ctxs=ctxs)[dbg["src"]]
                b = S.buf(dma=True)
                S.dma("sp", dbg_out, srcd, sembuf=b)
                S.end_phase()
    return nc


def _host_inputs(inputs):
    f = lambda a: np.ascontiguousarray(np.asarray(a, dtype=np.float32))
    x = f(inputs["x"]); c = f(inputs["c"]); ctx = f(inputs["ctx"]); c_ctx = f(inputs["c_ctx"])
    rpb = f(inputs["rpb"])
    conv_w = f(inputs["conv_w"])
    gout = f(inputs["out_norm_g"])
    q = np.arange(128)
    qr_off = q // 64
    qc = q % 64
    cs = np.clip(qc - 8, 0, 48)

    def table(p, bs, nrows):
        qr = 2 * p + qr_off
        rs = np.clip(qr - 4, 0, 56)
        kk = np.arange(nrows * 64)
        kr = bs + kk // 64
        kc = kk % 64
        valid = ((kr[None, :] >= rs[:, None]) & (kr[None, :] < rs[:, None] + 8) &
                 (kc[None, :] >= cs[:, None]) & (kc[None, :] < cs[:, None] + 16))
        dy = np.clip(kr[None, :] - qr[:, None] + 7, 0, 14)
        dx = np.clip(kc[None, :] - qc[:, None] + 15, 0, 30)
        g = rpb[:, :, dy, dx]
        return np.where(valid[None, None], g, np.float32(NEG)).astype(np.float32)

    mid = table(10, 16, 9)
    nab_mid = np.ascontiguousarray(mid.transpose(0, 2, 1, 3))
    edges = [table(0, 0, 8), table(1, 0, 8), table(30, 56, 8), table(31, 56, 8)]
    nab_edge = np.ascontiguousarray(np.stack(edges, axis=1))
    convw = np.ascontiguousarray(conv_w.reshape(DEPTH, 3, 4, 128).transpose(0, 3, 2, 1).reshape(DEPTH, 128, 12))
    goutc = np.ascontiguousarray(gout[:, 512:].reshape(DEPTH, 4, 128).transpose(0, 2, 1))
    w_rt = np.ascontiguousarray(f(inputs["w_router"])[0].reshape(8, 128, NE).transpose(1, 0, 2))
    shared = {
        "w_ada": f(inputs["w_ada"]), "b_ada": f(inputs["b_ada"]),
        "norm_g": f(inputs["norm_g"]).reshape(DEPTH * 4, D),
        "w_in": f(inputs["w_in"]), "nab_mid": nab_mid, "nab_edge": nab_edge, "convw": convw,
        "gout": gout, "goutc": goutc, "w_out": f(inputs["w_out"]),
        "w_gu_dense": f(inputs["w_gu_dense"]), "w_down_dense": f(inputs["w_down_dense"]),
        "w_router": w_rt, "w_gu_moe": f(inputs["w_gu_moe"])[0], "w_down_moe": f(inputs["w_down_moe"])[0],
    }
    maps = []
    for b in range(x.shape[0]):
        cv = np.concatenate([c[b].reshape(8, 128).T, c_ctx.reshape(8, 128).T], axis=1)
        m = dict(shared)
        m["x"] = x[b]
        m["ctx"] = ctx[b]
        m["cvec"] = np.ascontiguousarray(cv)
        maps.append(m)
    return maps


def kernel(**inputs):
    maps = _host_inputs(inputs)
    nc = build_nc()
    res = run_bass_kernel_spmd(nc, maps, core_ids=list(range(len(maps))))
    return np.stack([np.asarray(r["y"], dtype=np.float32) for r in res.results], axis=0)
```

```python
import numpy as np
from contextlib import ExitStack
import concourse.bass as bass
import concourse.mybir as mybir
from concourse.bass_utils import run_bass_kernel_spmd

F32 = mybir.dt.float32
BF16 = mybir.dt.bfloat16
U32 = mybir.dt.uint32
I32 = mybir.dt.int32
NSEG = 23
PIPE_LAGS = (1, 2)
NSLOT = NSEG * 512
AF = mybir.ActivationFunctionType
ALU = mybir.AluOpType
AX = mybir.AxisListType

D = 1024
S_LEN = 4096
CTX = 256
DEPTH = 2
FF = 3584
NE = 8
INW = 3072
NEG = -30000.0


class Buf:
    __slots__ = ("w", "r", "dsem", "psum")

    def __init__(self):
        self.w = None
        self.r = []
        self.dsem = None
        self.psum = False


class Sched:
    def __init__(self, nc, es):
        self.nc = nc
        self.es = es
        self.engs = {}
        for name, e in (("pe", nc.tensor), ("act", nc.scalar), ("dve", nc.vector),
                        ("pool", nc.gpsimd), ("sp", nc.sync)):
            sem = es.enter_context(nc.semaphore("prog_" + name))
            self.engs[name] = dict(e=e, sem=sem, count=0, seen={})
        self.free_dsems = {"sp": [], "pool": []}
        self.all_dsems = []
        self.phase_dsems = []
        self.persist = []

    def buf(self, dma=False, persist=False):
        b = Buf()
        if dma:
            kind = "pool" if dma == "pool" else "sp"
            if self.free_dsems[kind] and not persist:
                d = self.free_dsems[kind].pop()
            else:
                sem = self.es.enter_context(self.nc.semaphore("dsem%d" % (len(self.all_dsems) + len(self.persist))))
                d = dict(sem=sem, count=0, kind=kind)
                (self.persist if persist else self.all_dsems).append(d)
            b.dsem = d
            if not persist:
                self.phase_dsems.append(d)
        return b

    def _wait(self, eng, deps):
        best = {}
        for sem, val in deps:
            k = id(sem)
            if k not in best or best[k][1] < val:
                best[k] = (sem, val)
        for k, (sem, val) in best.items():
            if eng["seen"].get(k, 0) < val:
                eng["e"].wait_ge(sem, val)
                eng["seen"][k] = val

    @staticmethod
    def _deps(reads, writes):
        deps = []
        for b in reads:
            if b.w is not None:
                deps.append(b.w)
            if b.psum:
                deps.extend(b.r)
        for b in writes:
            if b.w is not None:
                deps.append(b.w)
            deps.extend(b.r)
        return deps

    @staticmethod
    def _mark(tok, reads, writes):
        for b in reads:
            b.r.append(tok)
            if len(b.r) > 24:
                best = {}
                for s, v in b.r:
                    if id(s) not in best or best[id(s)][1] < v:
                        best[id(s)] = (s, v)
                b.r = list(best.values())
        for b in writes:
            b.w = tok
            b.r = []

    def op(self, engname, fn, reads=(), writes=()):
        eng = self.engs[engname]
        deps = self._deps(reads, writes)
        if engname == "pe":
            deps = [d for d in deps if d[0] is not eng["sem"]]
        self._wait(eng, deps)
        ins = fn(eng["e"])
        eng["count"] += 1
        ins.then_inc(eng["sem"], 1)
        eng["seen"].pop(id(eng["sem"]), None)
        self._mark((eng["sem"], eng["count"]), reads, writes)
        return ins

    def dma(self, engname, out, in_, reads=(), writes=(), sembuf=None):
        eng = self.engs[engname]
        self._wait(eng, self._deps(reads, writes))
        ins = eng["e"].dma_start(out=out, in_=in_)
        d = sembuf.dsem
        assert d["kind"] == ("pool" if engname == "pool" else "sp"), (d["kind"], engname)
        d["count"] += 16
        ins.then_inc(d["sem"], 16)
        self._mark((d["sem"], d["count"]), reads, writes)
        return ins

    def dma_fn(self, engname, fn, reads=(), writes=(), sembuf=None):
        eng = self.engs[engname]
        self._wait(eng, self._deps(reads, writes))
        ins = fn(eng["e"])
        d = sembuf.dsem
        assert d["kind"] == ("pool" if engname == "pool" else "sp"), (d["kind"], engname)
        d["count"] += 16
        ins.then_inc(d["sem"], 16)
        self._mark((d["sem"], d["count"]), reads, writes)
        return ins

    def barrier(self):
        deps = [(e["sem"], e["count"]) for e in self.engs.values() if e["count"] > 0]
        deps += [(d["sem"], d["count"]) for d in self.all_dsems if d["count"] > 0]
        for eng in self.engs.values():
            self._wait(eng, deps)

    def end_phase(self):
        self.barrier()
        for d in self.phase_dsems:
            self.free_dsems[d["kind"]].append(d)
        self.phase_dsems = []


class TT:
    def __init__(self, t, b):
        self.t = t
        self.b = b


def _bc_rows(handle_ap, row, n, parts=128):
    return handle_ap[row:row + 1, 0:n].partition_broadcast(parts)


def build_nc(dbg=None):
    nc = bass.Bass("TRN2", target_bir_lowering=False)
    di = lambda name, shape, dt=F32: nc.dram_tensor(name, list(shape), dt, kind="ExternalInput").ap()
    x_in = di("x", [S_LEN, D])
    ctx_in = di("ctx", [CTX, D])
    cvec = di("cvec", [128, 16])
    w_ada = di("w_ada", [DEPTH, D, 6 * D])
    b_ada = di("b_ada", [DEPTH, 6 * D])
    norm_g = di("norm_g", [DEPTH * 4, D])
    w_in = di("w_in", [DEPTH, D, INW])
    nab_mid = di("nab_mid", [DEPTH, 128, 8, 576])
    nab_edge = di("nab_edge", [DEPTH, 4, 8, 128, 512])
    convw = di("convw", [DEPTH, 128, 12])
    gout = di("gout", [DEPTH, D])
    goutc = di("goutc", [DEPTH, 128, 4])
    w_out = di("w_out", [DEPTH, D, D])
    w_gu_d = di("w_gu_dense", [1, D, 2 * FF])
    w_dn_d = di("w_down_dense", [1, FF, D])
    w_rt = di("w_router", [128, 8, NE])
    w_gu_m = di("w_gu_moe", [NE, D, 2 * FF])
    w_dn_m = di("w_down_moe", [NE, FF, D])
    y_out = nc.dram_tensor("y", [S_LEN, D], F32, kind="ExternalOutput").ap()

    ds = lambda name, shape, dt: nc.dram_tensor(name, list(shape), dt).ap()
    QTd = ds("QTd", [4, 128, S_LEN], BF16)
    KTd = ds("KTd", [4, 128, S_LEN], BF16)
    Vd = ds("Vd", [S_LEN, 512], BF16)
    cnTd = ds("cnTd", [4, 128, S_LEN], BF16)
    QcTd = ds("QcTd", [4, 128, CTX], BF16)
    KcTd = ds("KcTd", [4, 128, CTX], BF16)
    Vcd = ds("Vcd", [CTX, 512], BF16)
    cncTd = ds("cncTd", [4, 128, CTX], BF16)
    mods = ds("mods", [12, D], F32)
    WGd = ds("WGd", [NE * 7 * 128, 8 * 512], BF16)
    WUd = ds("WUd", [NE * 7 * 128, 8 * 512], BF16)
    WDd = ds("WDd", [NE * 7 * 128, 4 * D], BF16)
    hs = ds("hs", [NSLOT, D], BF16)
    ys = ds("ys", [NSLOT, D], F32)
    ctxs = ds("ctxs", [CTX, D], F32)
    dbg_out = None
    if dbg is not None:
        dbg_out = nc.dram_tensor("dbg", list(dbg["shape"]), dbg.get("dt", F32), kind="ExternalOutput").ap()

    ges = ExitStack()
    with ges:
        S = Sched(nc, ges)

        uid = [0]

        def alloc(es, name, shape, dt, dma=False, psum=False):
            uid[0] += 1
            name = "%s_%d" % (name, uid[0])
            if psum:
                t = es.enter_context(nc.psum_tensor(name, list(shape), dt))
            else:
                t = es.enter_context(nc.sbuf_tensor(name, list(shape), dt))
            tt = TT(t, S.buf(dma=dma))
            tt.b.psum = bool(psum)
            return tt

        def allocn(es, n, name, shape, dt, dma=False, psum=False):
            return [alloc(es, "%s%d" % (name, i), shape, dt, dma=dma, psum=psum) for i in range(n)]

        ident = alloc(ges, "ident", [128, 128], BF16)
        ident32 = alloc(ges, "ident32", [128, 128], F32)
        ones = alloc(ges, "ones", [128, 128], BF16)
        epsb = alloc(ges, "epsb", [128, 1], F32)
        for idt in (ident, ident32):
            S.op("pool", lambda e: e.memset(idt.t[:], 0.0), writes=[idt.b])
            S.op("pool", lambda e: e.affine_select(out=idt.t[:], in_=idt.t[:], pattern=[[-1, 128]],
                                                   compare_op=ALU.not_equal, fill=1.0, base=0,
                                                   channel_multiplier=1), reads=[idt.b], writes=[idt.b])
        S.op("pool", lambda e: e.memset(ones.t[:], 1.0), writes=[ones.b])
        S.op("pool", lambda e: e.memset(epsb.t[:], 1e-6), writes=[epsb.b])

        ones32 = alloc(ges, "ones32", [128, 128], F32)
        tri32 = alloc(ges, "tri32", [128, 128], F32)
        S.op("pool", lambda e: e.memset(ones32.t[:], 1.0), writes=[ones32.b])
        S.op("pool", lambda e: e.memset(tri32.t[:], 1.0), writes=[tri32.b])
        S.op("pool", lambda e: e.affine_select(out=tri32.t[:], in_=tri32.t[:], pattern=[[1, 128]],
                                               compare_op=ALU.is_gt, fill=0.0, base=0,
                                               channel_multiplier=-1), reads=[tri32.b], writes=[tri32.b])
        zt = alloc(ges, "zt", [128, D], BF16)
        S.op("pool", lambda e: e.memset(zt.t[:], 0.0), writes=[zt.b])
        hsz = S.buf(dma="pool", persist=True)
        convb = S.buf(dma="pool", persist=True)
        conv_pieces = []
        for r in range(NSLOT // 512):
            conv_pieces.append((hs[r * 512:(r + 1) * 512, :].rearrange("(r p) d -> p r d", p=128),
                                zt.t[:].unsqueeze(1).to_broadcast([128, 4, D]), [zt.b], hsz))
        for ex in range(NE):
            for c in range(7):
                r0 = (ex * 7 + c) * 128
                conv_pieces.append((WGd[r0:r0 + 128, :].rearrange("p (c f) -> p c f", c=8),
                                    w_gu_m[ex, :, c * 512:(c + 1) * 512].rearrange("(c p) f -> p c f", p=128), [], convb))
                conv_pieces.append((WUd[r0:r0 + 128, :].rearrange("p (c f) -> p c f", c=8),
                                    w_gu_m[ex, :, FF + c * 512:FF + (c + 1) * 512].rearrange("(c p) f -> p c f", p=128),
                                    [], convb))
                conv_pieces.append((WDd[r0:r0 + 128, :].rearrange("p (j d) -> p j d", j=4),
                                    w_dn_m[ex, c * 512:(c + 1) * 512, :].rearrange("(j p) d -> p j d", p=128), [], convb))
        conv_pos = [0]

        def conv_step(n):
            for _ in range(n):
                if conv_pos[0] >= len(conv_pieces):
                    return
                o, i_, rd, sb = conv_pieces[conv_pos[0]]
                conv_pos[0] += 1
                S.dma("pool", o, i_, reads=rd, sembuf=sb)
                sb.w = (sb.dsem["sem"], sb.dsem["count"])

        def rstd_from_ss(ss, rstd, scale):
            S.op("act", lambda e: e.activation(out=rstd.t[:], in_=ss.t[:], func=AF.Sqrt, scale=scale,
                                               bias=epsb.t[:]), reads=[ss.b, epsb.b], writes=[rstd.b])
            S.op("dve", lambda e: e.reciprocal(out=rstd.t[:], in_=rstd.t[:]), reads=[rstd.b], writes=[rstd.b])

        def phase_ada(l):
            es = ExitStack()
            with es:
                cv = alloc(es, "cv", [128, 16], F32, dma=True)
                sc = alloc(es, "sc", [128, 16], F32)
                scb = alloc(es, "scb", [128, 16], BF16)
                wch = allocn(es, 2, "wch", [128, 8, 512], BF16, dma="pool")
                row = alloc(es, "row", [1, 2, 6 * D], F32)
                bad = alloc(es, "bad", [1, 6 * D], F32, dma=True)
                ng = alloc(es, "ng", [1, 4, D], F32, dma=True)
                ps = allocn(es, 2, "adaps", [128, 512], F32, psum=True)
                stb = S.buf(dma=True)
                S.dma("sp", cv.t[:], cvec[:, :], writes=[cv.b], sembuf=cv.b)
                S.dma("sp", bad.t[:], b_ada[l:l + 1, :], writes=[bad.b], sembuf=bad.b)
                S.dma("sp", ng.t[:], norm_g[4 * l:4 * l + 4, :].rearrange("(o r) d -> o r d", o=1),
                      writes=[ng.b], sembuf=ng.b)
                S.op("act", lambda e: e.activation(out=sc.t[:], in_=cv.t[:], func=AF.Silu),
                     reads=[cv.b], writes=[sc.b])
                S.op("dve", lambda e: e.tensor_copy(out=scb.t[:], in_=sc.t[:]), reads=[sc.b], writes=[scb.b])
                for j in range(12):
                    w = wch[j % 2]
                    S.dma("pool", w.t[:], w_ada[l, :, j * 512:(j + 1) * 512].rearrange("(c p) f -> p c f", p=128),
                          writes=[w.b], sembuf=w.b)
                    for src in range(2):
                        p = ps[src]
                        for ch in range(8):
                            S.op("pe", lambda e: e.matmul(p.t[0:1, :], lhsT=scb.t[:, src * 8 + ch:src * 8 + ch + 1],
                                                          rhs=w.t[:, ch, :], start=(ch == 0), stop=(ch == 7)),
                                 reads=[scb.b, w.b], writes=[p.b])
                        S.op("dve", lambda e: e.tensor_tensor(out=row.t[0:1, src, j * 512:(j + 1) * 512],
                                                              in0=p.t[0:1, :], in1=bad.t[0:1, j * 512:(j + 1) * 512],
                                                              op=ALU.add),
                             reads=[p.b, bad.b], writes=[row.b])
                for src in range(2):
                    for (k, gi) in ((1, 0), (4, 2)):
                        S.op("dve", lambda e: e.scalar_tensor_tensor(
                            out=row.t[0:1, src, k * D:(k + 1) * D], in0=row.t[0:1, src, k * D:(k + 1) * D],
                            scalar=1.0, in1=ng.t[0:1, gi, :], op0=ALU.add, op1=ALU.mult),
                            reads=[row.b, ng.b], writes=[row.b])
                    for (k, gi) in ((2, 1), (5, 3)):
                        S.op("dve", lambda e: e.tensor_tensor(
                            out=row.t[0:1, src, k * D:(k + 1) * D], in0=row.t[0:1, src, k * D:(k + 1) * D],
                            in1=ng.t[0:1, gi, :], op=ALU.mult), reads=[row.b, ng.b], writes=[row.b])
                S.dma("sp", mods.rearrange("(o r) d -> o (r d)", o=1), row.t[0:1, :, :].rearrange("o s d -> o (s d)"),
                      reads=[row.b], sembuf=stb)
                S.end_phase()

        def load_bc(es, name, rowidx, n=D, src=None):
            t = alloc(es, name, [128, n], F32, dma=True)
            srcap = mods if src is None else src
            S.dma("sp", t.t[:], _bc_rows(srcap, rowidx, n), writes=[t.b], sembuf=t.b)
            return t

        def norm_tile(xt, gs, sh, ss, rstd, junk, tmp, hb, hb32=None):
            S.op("act", lambda e: e.activation(out=junk.t[:], in_=xt.t[:], func=AF.Square, accum_out=ss.t[:]),
                 reads=[xt.b], writes=[junk.b, ss.b])
            rstd_from_ss(ss, rstd, 1.0 / D)
            S.op("dve", lambda e: e.scalar_tensor_tensor(out=tmp.t[:], in0=xt.t[:], scalar=rstd.t[:, 0:1],
                                                         in1=gs.t[:], op0=ALU.mult, op1=ALU.mult),
                 reads=[xt.b, rstd.b, gs.b], writes=[tmp.b])
            if hb32 is not None:
                S.op("pool", lambda e: e.tensor_tensor(out=hb32.t[:], in0=tmp.t[:], in1=sh.t[:], op=ALU.add),
                     reads=[tmp.b, sh.b], writes=[hb32.b])
                S.op("pool", lambda e: e.tensor_copy(out=hb.t[:], in_=hb32.t[:]), reads=[hb32.b], writes=[hb.b])
            else:
                S.op("pool", lambda e: e.tensor_tensor(out=hb.t[:], in0=tmp.t[:], in1=sh.t[:], op=ALU.add),
                     reads=[tmp.b, sh.b], writes=[hb.b])

        def phase_a(l, last):
            es = ExitStack()
            with es:
                win = alloc(es, "win", [128, 8, INW], BF16)
                win_b = [S.buf(dma="pool") for _ in range(6)]
                for j in ([1, 2, 3, 4, 5, 0] if last else [3, 4, 5, 0, 1, 2]):
                    S.dma("pool", win.t[:, :, j * 512:(j + 1) * 512],
                          w_in[l, :, j * 512:(j + 1) * 512].rearrange("(c p) f -> p c f", p=128),
                          writes=[win_b[j]], sembuf=win_b[j])
                gs_x = load_bc(es, "gs_x", 1)
                sh_x = load_bc(es, "sh_x", 0)
                gs_c = load_bc(es, "gs_c", 6 + 1)
                sh_c = load_bc(es, "sh_c", 6 + 0)
                cw = alloc(es, "cw", [128, 12], F32, dma=True)
                S.dma("sp", cw.t[:], convw[l], writes=[cw.b], sembuf=cw.b)
                goc = alloc(es, "goc", [128, 4], F32, dma=True)
                S.dma("sp", goc.t[:], goutc[l], writes=[goc.b], sembuf=goc.b)

                xts = allocn(es, 4, "xt", [128, D], F32, dma=True)
                junk = alloc(es, "junk", [128, D], BF16)
                tmp = allocn(es, 2, "tmp", [128, D], F32)
                hbs = allocn(es, 4, "hb", [128, D], BF16)
                sss = allocn(es, 2, "ss", [128, 1], F32)
                rstds = allocn(es, 2, "rstd", [128, 1], F32)
                hT = allocn(es, 2, "hT", [128, 8, 512], BF16)
                tps = allocn(es, 2, "tp", [128, 8, 128], BF16, psum=True)
                mps = allocn(es, 4, "mp", [128, 512], F32, psum=True)
                sps = alloc(es, "sps", [128, 512], F32, psum=True)
                qks = allocn(es, 2, "qks", [128, 8, 512], BF16, dma=True)
                vst = allocn(es, 2, "vst", [128, 512], BF16, dma=True)
                BT = allocn(es, 2, "BT", [128, 4, 512], BF16)
                vT = allocn(es, 2, "vT", [128, 4, 514], F32)
                csb = allocn(es, 2, "csb", [128, 512], F32)
                cacc = allocn(es, 4, "cacc", [128, 512], F32)
                co = alloc(es, "co", [128, 4, 512], F32)
                sq = allocn(es, 4, "sq", [128, 512], BF16)
                rbc = alloc(es, "rbc", [128, 512], F32)
                cn = allocn(es, 2, "cn", [128, 4, 512], BF16, dma=True)
                cnt = dict(mp=0)

                csrc = ctx_in if l == 0 else ctxs
                xsrc = x_in if l == 0 else y_out
                G = [dict(src=csrc, dst=dict(Q=QcTd, K=KcTd, V=Vcd), tok0=0, T=CTX, gs=gs_c, sh=sh_c,
                          want_q=not last, want_conv=not last, lat=False, cdst=cncTd)]
                for g in range(8):
                    G.append(dict(src=xsrc, dst=dict(Q=QTd, K=KTd, V=Vd), tok0=g * 512, T=512, gs=gs_x, sh=sh_x,
                                  want_q=True, want_conv=True, lat=True, cdst=cnTd))
                n0 = 0
                for k, gd in enumerate(G):
                    gd["k"] = k
                    gd["n0"] = n0
                    n0 += gd["T"] // 128

                def load_part(gd):
                    for i in range(gd["T"] // 128):
                        n = gd["n0"] + i
                        xt = xts[n % 4]
                        S.dma("sp", xt.t[:], gd["src"][gd["tok0"] + i * 128:gd["tok0"] + (i + 1) * 128, :],
                              writes=[xt.b], sembuf=xt.b)

                def norm_part(gd):
                    for i in range(gd["T"] // 128):
                        n = gd["n0"] + i
                        xt = xts[n % 4]
                        k2 = n % 2
                        norm_tile(xt, gd["gs"], gd["sh"], sss[k2], rstds[k2], junk, tmp[k2], hbs[n % 4])

                def trans_part(gd):
                    h = hT[gd["k"] % 2]
                    for i in range(gd["T"] // 128):
                        n = gd["n0"] + i
                        tp = tps[n % 2]
                        hb_ = hbs[n % 4]
                        for c in range(8):
                            S.op("pe", lambda e: e.transpose(out=tp.t[:, c, :], in_=hb_.t[:, c * 128:(c + 1) * 128],
                                                             identity=ident.t[:]),
                                 reads=[hb_.b, ident.b], writes=[tp.b])
                        S.op("act", lambda e: e.activation(out=h.t[:, :, i * 128:(i + 1) * 128], in_=tp.t[:],
                                                           func=AF.Copy), reads=[tp.b], writes=[h.b])

                def fmm(gd, ft):
                    T = gd["T"]
                    h = hT[gd["k"] % 2]
                    p = mps[cnt["mp"] % 4]
                    cnt["mp"] += 1
                    for c in range(8):
                        S.op("pe", lambda e: e.matmul(p.t[:, 0:T], lhsT=win.t[:, c, ft * 128:(ft + 1) * 128],
                                                      rhs=h.t[:, c, 0:T], start=(c == 0), stop=(c == 7)),
                             reads=[win_b[ft // 4], h.b], writes=[p.b])
                    return p

                def mm_bcu(gd):
                    T = gd["T"]
                    bt = BT[gd["k"] % 2]
                    vt = vT[gd["k"] % 2]
                    for c in range(4):
                        p = fmm(gd, 12 + c)
                        S.op("act", lambda e: e.activation(out=bt.t[:, c, 0:T], in_=p.t[:, 0:T], func=AF.Copy),
                             reads=[p.b], writes=[bt.b])
                        p = fmm(gd, 16 + c)
                        cs = csb[c % 2]
                        S.op("act", lambda e: e.activation(out=cs.t[:, 0:T], in_=p.t[:, 0:T], func=AF.Copy),
                             reads=[p.b], writes=[cs.b])
                        p = fmm(gd, 20 + c)
                        S.op("dve", lambda e: e.tensor_tensor(out=vt.t[:, c, 1:T + 1], in0=p.t[:, 0:T], in1=cs.t[:, 0:T],
                                                              op=ALU.mult), reads=[p.b, cs.b], writes=[vt.b])

                def mm_qk(gd):
                    T, tok0, dst = gd["T"], gd["tok0"], gd["dst"]
                    h = hT[gd["k"] % 2]
                    st = qks[gd["k"] % 2]
                    for ft in range(8):
                        if ft < 4 and not gd["want_q"]:
                            continue
                        p = fmm(gd, ft)
                        if ft < 4:
                            S.op("act", lambda e: e.activation(out=st.t[:, ft, 0:T], in_=p.t[:, 0:T], func=AF.Copy,
                                                               scale=0.125), reads=[p.b], writes=[st.b])
                        else:
                            S.op("dve", lambda e: e.tensor_copy(out=st.t[:, ft, 0:T], in_=p.t[:, 0:T]),
                                 reads=[p.b], writes=[st.b])
                    if gd["want_q"]:
                        S.dma("sp", dst["Q"][:, :, tok0:tok0 + T].rearrange("c p t -> p c t"), st.t[:, 0:4, 0:T],
                              reads=[st.b], sembuf=st.b)
                    S.dma("sp", dst["K"][:, :, tok0:tok0 + T].rearrange("c p t -> p c t"), st.t[:, 4:8, 0:T],
                          reads=[st.b], sembuf=st.b)

                def mm_v(gd):
                    T, tok0, dst = gd["T"], gd["tok0"], gd["dst"]
                    h = hT[gd["k"] % 2]
                    for i in range(T // 128):
                        p = mps[cnt["mp"] % 4]
                        cnt["mp"] += 1
                        for c in range(8):
                            S.op("pe", lambda e: e.matmul(p.t[:, :], lhsT=h.t[:, c, i * 128:(i + 1) * 128],
                                                          rhs=win.t[:, c, 1024:1536], start=(c == 0), stop=(c == 7)),
                                 reads=[win_b[2], h.b], writes=[p.b])
                        v = vst[i % 2]
                        S.op("act", lambda e: e.activation(out=v.t[:], in_=p.t[:], func=AF.Copy),
                             reads=[p.b], writes=[v.b])
                        S.dma("sp", dst["V"][tok0 + i * 128:tok0 + (i + 1) * 128, :], v.t[:], reads=[v.b], sembuf=v.b)

                def conv_ew(gd):
                    T = gd["T"]
                    bt = BT[gd["k"] % 2]
                    vt = vT[gd["k"] % 2]
                    for c in range(4):
                        a = cacc[c]
                        S.op("act", lambda e: e.activation(out=a.t[:, 0:T], in_=vt.t[:, c, 1:T + 1], func=AF.Copy,
                                                           scale=cw.t[:, c * 3 + 1:c * 3 + 2]),
                             reads=[vt.b, cw.b], writes=[a.b])
                    for c in range(4):
                        a = cacc[c]
                        S.op("dve", lambda e: e.scalar_tensor_tensor(out=a.t[:, 0:T], in0=vt.t[:, c, 0:T],
                                                                     scalar=cw.t[:, c * 3:c * 3 + 1], in1=a.t[:, 0:T],
                                                                     op0=ALU.mult, op1=ALU.add),
                             reads=[vt.b, cw.b, a.b], writes=[a.b])
                        S.op("dve", lambda e: e.scalar_tensor_tensor(out=a.t[:, 0:T], in0=vt.t[:, c, 2:T + 2],
                                                                     scalar=cw.t[:, c * 3 + 2:c * 3 + 3], in1=a.t[:, 0:T],
                                                                     op0=ALU.mult, op1=ALU.add),
                             reads=[vt.b, cw.b, a.b], writes=[a.b])
                    for c in range(4):
                        a = cacc[c]
                        S.op("pool", lambda e: e.tensor_tensor(out=co.t[:, c, 0:T], in0=a.t[:, 0:T], in1=bt.t[:, c, 0:T],
                                                               op=ALU.mult), reads=[a.b, bt.b], writes=[co.b])

                def conv_sq(gd):
                    T = gd["T"]
                    for c in range(4):
                        s = sq[c]
                        S.op("act", lambda e: e.activation(out=s.t[:, 0:T], in_=co.t[:, c, 0:T], func=AF.Square),
                             reads=[co.b], writes=[s.b])

                def conv_fin(gd):
                    T, tok0 = gd["T"], gd["tok0"]
                    for c in range(4):
                        s = sq[c]
                        S.op("pe", lambda e: e.matmul(sps.t[:, 0:T], lhsT=ones.t[:], rhs=s.t[:, 0:T],
                                                      start=(c == 0), stop=(c == 3)),
                             reads=[ones.b, s.b], writes=[sps.b])
                    S.op("act", lambda e: e.activation(out=rbc.t[:, 0:T], in_=sps.t[:, 0:T], func=AF.Sqrt,
                                                       scale=1.0 / 512, bias=epsb.t[:]),
                         reads=[sps.b, epsb.b], writes=[rbc.b])
                    S.op("dve", lambda e: e.reciprocal(out=rbc.t[:, 0:T], in_=rbc.t[:, 0:T]), reads=[rbc.b], writes=[rbc.b])
                    o = cn[gd["k"] % 2]
                    for c in range(4):
                        S.op("dve", lambda e: e.scalar_tensor_tensor(out=o.t[:, c, 0:T], in0=co.t[:, c, 0:T],
                                                                     scalar=goc.t[:, c:c + 1], in1=rbc.t[:, 0:T],
                                                                     op0=ALU.mult, op1=ALU.mult),
                             reads=[co.b, goc.b, rbc.b], writes=[o.b])
                    S.dma("sp", gd["cdst"][:, :, tok0:tok0 + T].rearrange("c p t -> p c t"), o.t[:, :, 0:T],
                          reads=[o.b], sembuf=o.b)

                load_part(G[0])
                norm_part(G[0])
                trans_part(G[0])
                if len(G) > 1:
                    load_part(G[1])
                prev_lat = None
                for k, gd in enumerate(G):
                    nxt = G[k + 1] if k + 1 < len(G) else None
                    cv = None
                    if nxt is not None:
                        norm_part(nxt)
                        if k + 2 < len(G):
                            load_part(G[k + 2])
                    if gd["want_conv"]:
                        mm_bcu(gd)
                    if gd["want_conv"]:
                        vt = vT[k % 2]
                        if not gd["lat"]:
                            S.op("pool", lambda e: e.memset(vt.t[:, :, 0:1], 0.0), writes=[vt.b])
                            S.op("pool", lambda e: e.memset(vt.t[:, :, CTX + 1:CTX + 2], 0.0), writes=[vt.b])
                            cv = gd
                        elif prev_lat is None:
                            S.op("pool", lambda e: e.memset(vt.t[:, :, 0:1], 0.0), writes=[vt.b])
                        else:
                            pv = vT[prev_lat["k"] % 2]
                            S.op("pool", lambda e: e.tensor_copy(out=vt.t[:, :, 0:1], in_=pv.t[:, :, 512:513]),
                                 reads=[pv.b], writes=[vt.b])
                            S.op("pool", lambda e: e.tensor_copy(out=pv.t[:, :, 513:514], in_=vt.t[:, :, 1:2]),
                                 reads=[vt.b], writes=[pv.b])
                            cv = prev_lat
                        if cv is not None:
                            conv_ew(cv)
                    mm_qk(gd)
                    if cv is not None:
                        conv_sq(cv)
                    if nxt is not None:
                        trans_part(nxt)
                    mm_v(gd)
                    if cv is not None:
                        conv_fin(cv)
                    if gd["lat"]:
                        prev_lat = gd
                        conv_step(3)
                pv = vT[prev_lat["k"] % 2]
                S.op("pool", lambda e: e.memset(pv.t[:, :, 513:514], 0.0), writes=[pv.b])
                conv_ew(prev_lat)
                conv_sq(prev_lat)
                conv_fin(prev_lat)
                S.end_phase()

        def phase_b(l, last):
            es = ExitStack()
            with es:
                KT = alloc(es, "KT", [128, 4, S_LEN], BF16, dma=True)
                V = alloc(es, "V", [128, 32, 512], BF16, dma=True)
                KcT = alloc(es, "KcT", [128, 4, CTX], BF16, dma=True)
                Vc = alloc(es, "Vc", [128, 2, 512], BF16, dma=True)
                nbm = alloc(es, "nbm", [128, 8, 576], F32, dma=True)

                def init_loads_first():
                    S.dma("sp", KcT.t[:], KcTd.rearrange("c p t -> p c t"), writes=[KcT.b], sembuf=KcT.b)
                    S.dma("sp", Vc.t[:], Vcd.rearrange("(t p) f -> p t f", p=128), writes=[Vc.b], sembuf=Vc.b)

                KT_b = [S.buf(dma=True) for _ in range(8)]
                V_b = [S.buf(dma=True) for _ in range(4)]

                def ld_kt(bk):
                    S.dma("sp", KT.t[:, :, bk * 512:(bk + 1) * 512],
                          KTd[:, :, bk * 512:(bk + 1) * 512].rearrange("c p t -> p c t"),
                          writes=[KT_b[bk]], sembuf=KT_b[bk])

                def ld_v(q):
                    S.dma("sp", V.t[:, q * 8:(q + 1) * 8, :],
                          Vd[q * 1024:(q + 1) * 1024, :].rearrange("(t p) f -> p t f", p=128),
                          writes=[V_b[q]], sembuf=V_b[q])

                def init_loads_rest():
                    ld_kt(0)
                    ld_kt(1)
                    ld_v(0)
                    S.dma("sp", nbm.t[:], nab_mid[l], writes=[nbm.b], sembuf=nbm.b)
                    ld_kt(2)
                    ld_v(1)
                    ld_kt(3)
                    ld_kt(4)
                    ld_v(2)
                    ld_kt(5)
                    ld_kt(6)
                    ld_v(3)
                    ld_kt(7)
                wo = alloc(es, "wo", [128, 8, D], BF16, dma="pool")

                def load_wo():
                    for j in range(2):
                        S.dma("pool", wo.t[:, :, j * 512:(j + 1) * 512],
                              w_out[l, :, j * 512:(j + 1) * 512].rearrange("(c p) f -> p c f", p=128),
                              writes=[wo.b], sembuf=wo.b)
                ggt_x = load_bc(es, "ggt_x", 2)
                ggt_c = load_bc(es, "ggt_c", 6 + 2) if not last else None
                goa = load_bc(es, "goa", l, n=512, src=gout)

                NB = 3
                QA = allocn(es, 2, "QA", [128, 4, 512], BF16, dma=True)
                QB = allocn(es, 2, "QB", [128, 4, 512], BF16, dma=True)
                for qz in QA:
                    S.op("pool", lambda e: e.memset(qz.t[64:128, :, :], 0.0), writes=[qz.b])
                for qz in QB:
                    S.op("pool", lambda e: e.memset(qz.t[0:64, :, :], 0.0), writes=[qz.b])
                mT = allocn(es, 2, "mT", [128, 8, 512], BF16, dma=True)
                nbe = allocn(es, 4, "nbe", [128, 512], F32, dma=True)
                psA = allocn(es, 2, "psA", [128, 512], F32, psum=True)
                psB = allocn(es, 2, "psB", [128, 512], F32, psum=True)
                psT = allocn(es, 2, "psT", [128, 8, 128], BF16, psum=True)
                psO = alloc(es, "psO", [128, 512], F32, psum=True)
                psY = alloc(es, "psY", [128, 512], F32, psum=True)
                Ssb = allocn(es, NB, "Ssb", [128, 832], F32)
                Sctx_b = [S.buf() for _ in range(NB)]
                Psb = allocn(es, NB, "Psb", [128, 832], BF16)
                PT = allocn(es, NB, "PT", [128, 7, 128], BF16)
                nmx = allocn(es, NB, "nmx", [128, 1], F32)
                rsum = allocn(es, 3, "rsum", [128, 8], F32)
                rinv = allocn(es, 2, "rinv", [128, 8], F32)
                Osb = allocn(es, 2, "Osb", [128, 512], F32)
                On = allocn(es, 2, "On", [128, 512], BF16)
                junk = alloc(es, "junkb", [128, 512], BF16)
                ss = allocn(es, 2, "ssb", [128, 2], F32)
                ss1 = allocn(es, 2, "ss1b", [128, 1], F32)
                rstd = allocn(es, 2, "rstdb", [128, 1], F32)
                xr = allocn(es, 2, "xr", [128, D], F32, dma=True)
                ysb = allocn(es, 2, "ysb", [128, D], F32)

                def rstd_lnexp(ssv, rs_, scale):
                    S.op("act", lambda e: e.activation(out=rs_.t[:], in_=ssv.t[:], func=AF.Ln, scale=scale,
                                                       bias=epsb.t[:]), reads=[ssv.b, epsb.b], writes=[rs_.b])
                    S.op("act", lambda e: e.activation(out=rs_.t[:], in_=rs_.t[:], func=AF.Exp, scale=-0.5),
                         reads=[rs_.b], writes=[rs_.b])

                def stage_a(u):
                    i = u["i"]
                    a, b = psA[i % 2], psB[i % 2]
                    q_ap, pb, chunk = u["q"], u["pb"], u["chunk"]
                    if u["loc"] is not None:
                        tok0, nloc, tile0 = u["loc"]
                        kbs = [KT_b[bk] for bk in range(tok0 // 512, (tok0 + nloc - 1) // 512 + 1)]
                        S.op("pe", lambda e: e.matmul(a.t[:, 0:512], lhsT=q_ap,
                                                      rhs=KT.t[:, chunk, tok0:tok0 + 512], start=True, stop=True),
                             reads=[u["qb"]] + kbs, writes=[a.b])
                        if nloc > 512:
                            S.op("pe", lambda e: e.matmul(b.t[:, 0:64], lhsT=q_ap,
                                                          rhs=KT.t[:, chunk, tok0 + 512:tok0 + 576],
                                                          start=True, stop=True),
                                 reads=[u["qb"]] + kbs, writes=[b.b])
                    S.op("pe", lambda e: e.matmul(b.t[:, 64:320], lhsT=q_ap, rhs=KcT.t[:, chunk, :],
                                                  start=True, stop=True), reads=[u["qb"], KcT.b], writes=[b.b])

                def stage_b1(u):
                    i, h = u["i"], u["h"]
                    a, b = psA[i % 2], psB[i % 2]
                    s, mx = Ssb[i % NB], nmx[i % NB]
                    sc_b = Sctx_b[i % NB]
                    nloc = u["loc"][1] if u["loc"] is not None else 0
                    ntot = nloc + CTX
                    S.op("act", lambda e: e.activation(out=s.t[:, nloc:ntot], in_=b.t[:, 64:320], func=AF.Copy),
                         reads=[b.b], writes=[sc_b])
                    if u["loc"] is not None:
                        if u["edge"] is None:
                            bt, bap = nbm.b, nbm.t[:, h, :]
                        else:
                            nb = nbe[u["nbi"] % 4]
                            bt, bap = nb.b, nb.t[:, :]
                        S.op("dve", lambda e: e.tensor_tensor(out=s.t[:, 0:512], in0=a.t[:, 0:512], in1=bap[:, 0:512],
                                                              op=ALU.add), reads=[a.b, bt], writes=[s.b])
                        if nloc > 512:
                            S.op("dve", lambda e: e.tensor_tensor(out=s.t[:, 512:576], in0=b.t[:, 0:64],
                                                                  in1=bap[:, 512:576], op=ALU.add),
                                 reads=[b.b, bt], writes=[s.b])
                    S.op("dve", lambda e: e.reduce_max(out=mx.t[:], in_=s.t[:, 0:ntot], axis=AX.X, negate=True),
                         reads=[s.b, sc_b], writes=[mx.b])

                def stage_b2(u):
                    i, h = u["i"], u["h"]
                    s, pp, mx = Ssb[i % NB], Psb[i % NB], nmx[i % NB]
                    sc_b = Sctx_b[i % NB]
                    rs = rsum[u["tile"] % 3]
                    nloc = u["loc"][1] if u["loc"] is not None else 0
                    ntot = nloc + CTX
                    S.op("act", lambda e: e.activation(out=pp.t[:, 0:ntot], in_=s.t[:, 0:ntot], func=AF.Exp,
                                                       bias=mx.t[:, 0:1], accum_out=rs.t[:, h:h + 1]),
                         reads=[s.b, sc_b, mx.b], writes=[pp.b, rs.b])

                def chunks_of(u):
                    nloc = 0
                    chunks = []
                    if u["loc"] is not None:
                        tok0, nloc, tile0 = u["loc"]
                        for k in range(4):
                            chunks.append((k * 128, 128, V, tile0 + k))
                        if nloc > 512:
                            chunks.append((512, 64, V, tile0 + 4))
                    chunks.append((nloc, 128, Vc, 0))
                    chunks.append((nloc + 128, 128, Vc, 1))
                    return chunks

                def stage_c1(u):
                    i = u["i"]
                    pp = Psb[i % NB]
                    pst = psT[i % 2]
                    for j, (off, sz, vt, ti) in enumerate(chunks_of(u)):
                        S.op("pe", lambda e: e.transpose(out=pst.t[0:sz, j, :], in_=pp.t[:, off:off + sz],
                                                         identity=ident.t[:]),
                             reads=[pp.b, ident.b], writes=[pst.b])

                def stage_c1b(u):
                    i = u["i"]
                    pt = PT[i % NB]
                    pst = psT[i % 2]
                    nch = len(chunks_of(u))
                    if i % 2 == 0:
                        S.op("act", lambda e: e.activation(out=pt.t[:, 0:nch, :], in_=pst.t[:, 0:nch, :], func=AF.Copy),
                             reads=[pst.b], writes=[pt.b])
                    else:
                        S.op("dve", lambda e: e.tensor_copy(out=pt.t[:, 0:nch, :], in_=pst.t[:, 0:nch, :]),
                             reads=[pst.b], writes=[pt.b])

                def stage_c2(u):
                    i, h = u["i"], u["h"]
                    pt = PT[i % NB]
                    chunks = chunks_of(u)
                    nch = len(chunks)
                    for j, (off, sz, vt, ti) in enumerate(chunks):
                        S.op("pe", lambda e: e.matmul(psO.t[:, h * 64:(h + 1) * 64], lhsT=pt.t[0:sz, j, :],
                                                      rhs=vt.t[0:sz, ti, h * 64:(h + 1) * 64],
                                                      start=(j == 0), stop=(j == nch - 1)),
                             reads=[pt.b, (V_b[ti // 8] if vt is V else vt.b)], writes=[psO.b])

                def T1(ti, m, col0, src_rows, dst_rows, ggt, L):
                    k2 = ti % 2
                    rs = rsum[ti % 3]
                    S.dma("sp", xr[k2].t[:], src_rows, writes=[xr[k2].b], sembuf=xr[k2].b)
                    S.op("dve", lambda e: e.reciprocal(out=rinv[k2].t[:], in_=rs.t[:]),
                         reads=[rs.b], writes=[rinv[k2].b])
                    S.op("dve", lambda e: e.tensor_tensor(
                        out=Osb[k2].t[:].rearrange("p (h d) -> p h d", h=8),
                        in0=psO.t[:].rearrange("p (h d) -> p h d", h=8),
                        in1=rinv[k2].t[:].unsqueeze(2).to_broadcast([128, 8, 64]), op=ALU.mult),
                        reads=[psO.b, rinv[k2].b], writes=[Osb[k2].b])

                def T2(ti, m, col0, src_rows, dst_rows, ggt, L):
                    k2 = ti % 2
                    S.op("act", lambda e: e.activation(out=junk.t[:], in_=Osb[k2].t[:], func=AF.Square,
                                                       accum_out=ss1[k2].t[:]),
                         reads=[Osb[k2].b], writes=[junk.b, ss1[k2].b])
                    rstd_lnexp(ss1[k2], rstd[k2], 1.0 / 512)

                def T3(ti, m, col0, src_rows, dst_rows, ggt, L):
                    k2 = ti % 2
                    S.op("dve", lambda e: e.scalar_tensor_tensor(out=On[k2].t[:], in0=Osb[k2].t[:],
                                                                 scalar=rstd[k2].t[:, 0:1], in1=goa.t[:],
                                                                 op0=ALU.mult, op1=ALU.mult),
                         reads=[Osb[k2].b, rstd[k2].b, goa.b], writes=[On[k2].b])

                def T4(ti, m, col0, src_rows, dst_rows, ggt, L):
                    k2 = ti % 2
                    pst = psT[(L + 7) % 2]
                    for c in range(4):
                        S.op("pe", lambda e: e.transpose(out=pst.t[:, c, :], in_=On[k2].t[:, c * 128:(c + 1) * 128],
                                                         identity=ident.t[:]),
                             reads=[On[k2].b, ident.b], writes=[pst.b])

                def T5(ti, m, col0, src_rows, dst_rows, ggt, L):
                    pst = psT[(L + 7) % 2]
                    S.op("act", lambda e: e.activation(out=m.t[:, 0:4, col0:col0 + 128], in_=pst.t[:, 0:4, :], func=AF.Copy),
                         reads=[pst.b], writes=[m.b])

                def T6(hf, ti, m, col0, src_rows, dst_rows, ggt, L):
                    for k in range(8):
                        S.op("pe", lambda e: e.matmul(psY.t[:], lhsT=m.t[:, k, col0:col0 + 128],
                                                      rhs=wo.t[:, k, hf * 512:(hf + 1) * 512],
                                                      start=(k == 0), stop=(k == 7)),
                             reads=[m.b, wo.b], writes=[psY.b])

                def T7(hf, ti, m, col0, src_rows, dst_rows, ggt, L):
                    k2 = ti % 2
                    S.op("act", lambda e: e.activation(out=ysb[k2].t[:, hf * 512:(hf + 1) * 512], in_=psY.t[:],
                                                       func=AF.Copy), reads=[psY.b], writes=[ysb[k2].b])
                    S.op("act", lambda e: e.activation(out=junk.t[:], in_=ysb[k2].t[:, hf * 512:(hf + 1) * 512],
                                                       func=AF.Square, accum_out=ss[k2].t[:, hf:hf + 1]),
                         reads=[ysb[k2].b], writes=[junk.b, ss[k2].b])

                def T10(ti, m, col0, src_rows, dst_rows, ggt, L):
                    k2 = ti % 2
                    S.op("dve", lambda e: e.tensor_tensor(out=ss1[k2].t[:], in0=ss[k2].t[:, 0:1], in1=ss[k2].t[:, 1:2],
                                                          op=ALU.add), reads=[ss[k2].b], writes=[ss1[k2].b])

                def T11(ti, m, col0, src_rows, dst_rows, ggt, L):
                    k2 = ti % 2
                    rstd_lnexp(ss1[k2], rstd[k2], 1.0 / D)

                def T12(ti, m, col0, src_rows, dst_rows, ggt, L):
                    k2 = ti % 2
                    S.op("dve", lambda e: e.scalar_tensor_tensor(
                        out=ysb[k2].t[:], in0=ysb[k2].t[:], scalar=rstd[k2].t[:, 0:1],
                        in1=ggt.t[:], op0=ALU.mult, op1=ALU.mult),
                        reads=[ysb[k2].b, rstd[k2].b, ggt.b], writes=[ysb[k2].b])
                    S.op("pool", lambda e: e.tensor_tensor(out=xr[k2].t[:], in0=ysb[k2].t[:], in1=xr[k2].t[:], op=ALU.add),
                         reads=[ysb[k2].b, xr[k2].b], writes=[xr[k2].b])
                    S.dma("sp", dst_rows, xr[k2].t[:], reads=[xr[k2].b], sembuf=xr[k2].b)

                units = []
                tails = {}
                groups = []
                tile_no = 0
                nbi = 0
                if not last:
                    csrc = ctx_in if l == 0 else ctxs
                    qa, qb_, m = QA[0], QB[0], mT[0]
                    groups.append((len(units),
                                   [(qa, lambda qa=qa: qa.t[0:64, :, 0:CTX], QcTd[:, 0:64, :].rearrange("c p t -> p c t")),
                                    (qb_, lambda qb_=qb_: qb_.t[64:128, :, 0:CTX], QcTd[:, 64:128, :].rearrange("c p t -> p c t"))],
                                   (m, lambda m=m: m.t[:, 4:8, 0:CTX], cncTd.rearrange("c p t -> p c t"))))
                    for ti in range(2):
                        for h in range(8):
                            pb = (h % 2) * 64
                            q = qa if h % 2 == 0 else qb_
                            units.append(dict(q=q.t[:, h // 2, ti * 128:(ti + 1) * 128], qb=q.b, pb=pb, chunk=h // 2,
                                              h=h, loc=None, edge=None, tile=tile_no))
                        tails[len(units) - 1] = (tile_no, m, ti * 128, csrc[ti * 128:(ti + 1) * 128, :],
                                                 ctxs[ti * 128:(ti + 1) * 128, :], ggt_c)
                        tile_no += 1
                xsrc = x_in if l == 0 else y_out
                for g in range(8):
                    qa, qb_, m = QA[(g + 1) % 2], QB[(g + 1) % 2], mT[(g + 1) % 2]
                    groups.append((len(units),
                                   [(qa, lambda qa=qa: qa.t[0:64, :, :],
                                     QTd[:, 0:64, g * 512:(g + 1) * 512].rearrange("c p t -> p c t")),
                                    (qb_, lambda qb_=qb_: qb_.t[64:128, :, :],
                                     QTd[:, 64:128, g * 512:(g + 1) * 512].rearrange("c p t -> p c t"))],
                                   (m, lambda m=m: m.t[:, 4:8, :], cnTd[:, :, g * 512:(g + 1) * 512].rearrange("c p t -> p c t"))))
                    for pi in range(4):
                        p = g * 4 + pi
                        if 2 <= p <= 29:
                            bs, nloc, edge = 2 * p - 4, 576, None
                        elif p < 2:
                            bs, nloc, edge = 0, 512, p
                        else:
                            bs, nloc, edge = 56, 512, p - 28
                        for h in range(8):
                            pb = (h % 2) * 64
                            q = qa if h % 2 == 0 else qb_
                            u = dict(q=q.t[:, h // 2, pi * 128:(pi + 1) * 128], qb=q.b, pb=pb, chunk=h // 2, h=h,
                                     loc=(bs * 64, nloc, bs // 2), edge=edge, tile=tile_no)
                            if edge is not None:
                                u["nbi"] = nbi
                                nbi += 1
                            units.append(u)
                        tails[len(units) - 1] = (tile_no, m, pi * 128, xsrc[p * 128:(p + 1) * 128, :],
                                                 y_out[p * 128:(p + 1) * 128, :], ggt_x)
                        tile_no += 1
                for i, u in enumerate(units):
                    u["i"] = i
                NU = len(units)
                pre = {}

                def at(step, fn):
                    pre.setdefault(max(step, 0), []).append(fn)

                def mk_load(spec):
                    tt, dst_fn, src_ap = spec
                    return lambda: S.dma("sp", dst_fn(), src_ap, writes=[tt.b], sembuf=tt.b)

                for gi_, (first, qspecs, mspec) in enumerate(groups):
                    if gi_ == 0:
                        for qs in qspecs:
                            at(0, mk_load(qs))
                        at(0, mk_load(mspec))
                    else:
                        pf = groups[gi_ - 1][0]
                        for qs in qspecs:
                            at(pf, mk_load(qs))
                        at(pf + 12, mk_load(mspec))
                for u in units:
                    if u["edge"] is not None:
                        def ld(u=u):
                            nb = nbe[u["nbi"] % 4]
                            S.dma("sp", nb.t[:], nab_edge[l, u["edge"], u["h"]], writes=[nb.b], sembuf=nb.b)
                        at(u["i"] - 2, ld)
                post = {}
                pre2 = {}
                for L, targs in tails.items():
                    A_ = lambda d, s, f, *x: d.setdefault(s, []).append(lambda f=f, x=x, targs=targs, L=L: f(*x, *targs, L))
                    A_(post, L + 4, T1)
                    A_(post, L + 5, T2)
                    A_(post, L + 6, T3)
                    A_(post, L + 7, T4)
                    A_(pre2, L + 8, T5)
                    A_(post, L + 8, T6, 0)
                    A_(pre2, L + 9, T7, 0)
                    A_(post, L + 9, T6, 1)
                    A_(pre2, L + 10, T7, 1)
                    A_(post, L + 10, T10)
                    A_(post, L + 11, T11)
                    A_(post, L + 12, T12)
                    post.setdefault(L + 12, []).append(lambda: conv_step(2))
                init_loads_first()
                for st in range(NU + 13):
                    for fn in pre.get(st, []):
                        fn()
                    if st == 0:
                        init_loads_rest()
                    if st == 2:
                        load_wo()
                    for fn in pre2.get(st, []):
                        fn()
                    if 0 <= st - 4 < NU:
                        stage_c1b(units[st - 4])
                    if st < NU:
                        stage_a(units[st])
                    if 0 <= st - 1 < NU:
                        stage_b1(units[st - 1])
                    if 0 <= st - 2 < NU:
                        stage_b2(units[st - 2])
                    if 0 <= st - 3 < NU:
                        stage_c1(units[st - 3])
                    if 0 <= st - 4 < NU:
                        stage_c2(units[st - 4])
                    for fn in post.get(st, []):
                        fn()
                S.end_phase()

        def phase_f(l, last):
            es = ExitStack()
            with es:
                gsT = alloc(es, "fgs", [128, D], F32, dma=True)
                shT = alloc(es, "fsh", [128, D], F32, dma=True)
                ggt_x = load_bc(es, "fggt_x", 5)
                ggt_c = load_bc(es, "fggt_c", 6 + 5) if not last else None

                def load_rows(base):
                    S.dma("sp", gsT.t[:], _bc_rows(mods, base + 4, D), writes=[gsT.b], sembuf=gsT.b)
                    S.dma("sp", shT.t[:], _bc_rows(mods, base + 3, D), writes=[shT.b], sembuf=shT.b)

                tiles = []
                if not last:
                    for t in range(CTX // 128):
                        tiles.append(dict(rows=ctxs[t * 128:(t + 1) * 128, :], ctx=True))
                for p in range(S_LEN // 128):
                    tiles.append(dict(rows=y_out[p * 128:(p + 1) * 128, :], ctx=False))
                ngr = 4
                base_n, extra = len(tiles) // ngr, len(tiles) % ngr
                groups = []
                pos = 0
                for k in range(ngr):
                    n = base_n + (1 if k < extra else 0)
                    groups.append(dict(k=k, tiles=tiles[pos:pos + n]))
                    pos += n
                NTM = max(len(g["tiles"]) for g in groups)
                TM = NTM * 128
                for g in groups:
                    nt = len(g["tiles"])
                    g["nt"] = nt
                    g["splits"] = [(0, 512), (512, 512)] if nt == 8 else [(i * 384, 384) for i in range(nt * 128 // 384)]
                    assert sum(w for _, w in g["splits"]) == nt * 128
                    bl = []
                    for i, td in enumerate(g["tiles"]):
                        if bl and len(bl[-1]) < 4 and g["tiles"][bl[-1][0]]["ctx"] == td["ctx"]:
                            bl[-1].append(i)
                        else:
                            bl.append([i])
                    g["batches"] = bl

                xts = allocn(es, 4, "fxt", [128, D], F32, dma=True)
                junk = alloc(es, "fjunk", [128, D], BF16)
                tmp = allocn(es, 2, "ftmp", [128, D], F32)
                hb = allocn(es, 4, "fhb", [128, D], BF16)
                ss4 = allocn(es, 2, "fss4", [128, 4], F32)
                rs4 = allocn(es, 2, "frs4", [128, 4], F32)
                sse = alloc(es, "fsse", [128, NTM], F32)
                rse = alloc(es, "frse", [128, NTM], F32)
                hT = allocn(es, 2, "fhT", [128, 8, TM], BF16)
                tps = allocn(es, 2, "ftp", [128, 8, 128], BF16, psum=True)
                psG = allocn(es, 2, "psG", [128, 512], F32, psum=True)
                psU = allocn(es, 2, "psU", [128, 512], F32, psum=True)
                psD = allocn(es, 2, "psD", [128, 512], F32, psum=True)
                wg = allocn(es, 2, "wg", [128, 8, 512], BF16, dma="pool")
                wu = allocn(es, 2, "wu", [128, 8, 512], BF16, dma="pool")
                wd = allocn(es, 2, "wd", [128, 4, D], BF16, dma="pool")
                sg = allocn(es, 2, "sg", [128, 512], F32)
                aT = allocn(es, 2, "aT", [128, 4, TM], BF16)
                acc = alloc(es, "facc", [128, NTM, D], F32)
                cnt = dict(x=0, h=0, b=0, w=0, gu=0, d=0, a=0, t=0)
                cur_kind = [None]

                def load_batch(g, bi):
                    xl = []
                    for i in g["batches"][bi]:
                        xt = xts[cnt["x"] % 4]
                        cnt["x"] += 1
                        xl.append(xt)
                        S.dma("sp", xt.t[:], g["tiles"][i]["rows"], writes=[xt.b], sembuf=xt.b)
                    g.setdefault("xl", {})[bi] = xl

                def norm_batch(g, bi):
                    idx = g["batches"][bi]
                    kind = g["tiles"][idx[0]]["ctx"]
                    if cur_kind[0] != kind:
                        load_rows(6 if kind else 0)
                        cur_kind[0] = kind
                    kb = cnt["b"] % 2
                    cnt["b"] += 1
                    ssb, rsb = ss4[kb], rs4[kb]
                    xl = g["xl"][bi]
                    for j, i in enumerate(idx):
                        xt = xl[j]
                        S.op("act", lambda e: e.activation(out=junk.t[:], in_=xt.t[:], func=AF.Square,
                                                           accum_out=ssb.t[:, j:j + 1]),
                             reads=[xt.b], writes=[junk.b, ssb.b])
                    nbt = len(idx)
                    S.op("act", lambda e: e.activation(out=rsb.t[:, 0:nbt], in_=ssb.t[:, 0:nbt], func=AF.Sqrt,
                                                       scale=1.0 / D, bias=epsb.t[:]),
                         reads=[ssb.b, epsb.b], writes=[rsb.b])
                    S.op("dve", lambda e: e.reciprocal(out=rsb.t[:, 0:nbt], in_=rsb.t[:, 0:nbt]),
                         reads=[rsb.b], writes=[rsb.b])
                    hl = []
                    for j, i in enumerate(idx):
                        xt = xl[j]
                        tm = tmp[j % 2]
                        h_ = hb[cnt["h"] % 4]
                        cnt["h"] += 1
                        hl.append((i, h_))
                        S.op("dve", lambda e: e.scalar_tensor_tensor(out=tm.t[:], in0=xt.t[:], scalar=rsb.t[:, j:j + 1],
                                                                     in1=gsT.t[:], op0=ALU.mult, op1=ALU.mult),
                             reads=[xt.b, rsb.b, gsT.b], writes=[tm.b])
                        S.op("pool", lambda e: e.tensor_tensor(out=h_.t[:], in0=tm.t[:], in1=shT.t[:], op=ALU.add),
                             reads=[tm.b, shT.b], writes=[h_.b])
                    g.setdefault("hl", {})[bi] = hl

                def trans_batch(g, bi):
                    h = hT[g["k"] % 2]
                    for (i, h_) in g["hl"][bi]:
                        tp = tps[cnt["t"] % 2]
                        cnt["t"] += 1
                        for c in range(8):
                            S.op("pe", lambda e: e.transpose(out=tp.t[:, c, :], in_=h_.t[:, c * 128:(c + 1) * 128],
                                                             identity=ident.t[:]),
                                 reads=[h_.b, ident.b], writes=[tp.b])
                        S.op("act", lambda e: e.activation(out=h.t[:, :, i * 128:(i + 1) * 128], in_=tp.t[:],
                                                           func=AF.Copy), reads=[tp.b], writes=[h.b])

                def epilogue(g):
                    nt = g["nt"]
                    for i in range(nt):
                        S.op("act", lambda e: e.activation(out=junk.t[:], in_=acc.t[:, i, :], func=AF.Square,
                                                           accum_out=sse.t[:, i:i + 1]),
                             reads=[acc.b], writes=[junk.b, sse.b])
                    S.op("act", lambda e: e.activation(out=rse.t[:, 0:nt], in_=sse.t[:, 0:nt], func=AF.Sqrt,
                                                       scale=1.0 / D, bias=epsb.t[:]),
                         reads=[sse.b, epsb.b], writes=[rse.b])
                    S.op("dve", lambda e: e.reciprocal(out=rse.t[:, 0:nt], in_=rse.t[:, 0:nt]),
                         reads=[rse.b], writes=[rse.b])

                    def issue(i):
                        xt = xts[cnt["x"] % 4]
                        cnt["x"] += 1
                        S.dma("sp", xt.t[:], g["tiles"][i]["rows"], writes=[xt.b], sembuf=xt.b)
                        return xt
                    q = [issue(i) for i in range(min(3, nt))]
                    for i in range(nt):
                        xt = q.pop(0)
                        tm = tmp[i % 2]
                        ggt = ggt_c if g["tiles"][i]["ctx"] else ggt_x
                        S.op("dve", lambda e: e.scalar_tensor_tensor(out=tm.t[:], in0=acc.t[:, i, :],
                                                                     scalar=rse.t[:, i:i + 1], in1=ggt.t[:],
                                                                     op0=ALU.mult, op1=ALU.mult),
                             reads=[acc.b, rse.b, ggt.b], writes=[tm.b])
                        S.op("pool", lambda e: e.tensor_tensor(out=xt.t[:], in0=tm.t[:], in1=xt.t[:], op=ALU.add),
                             reads=[tm.b, xt.b], writes=[xt.b])
                        S.dma("sp", g["tiles"][i]["rows"], xt.t[:], reads=[xt.b], sembuf=xt.b)
                        if i + 3 < nt:
                            q.append(issue(i + 3))

                wgu, wdn = w_gu_d[l // 2], w_dn_d[l // 2]

                def wload(c):
                    kw = cnt["w"] % 2
                    cnt["w"] += 1
                    S.dma("pool", wg[kw].t[:], wgu[:, c * 512:(c + 1) * 512].rearrange("(c p) f -> p c f", p=128),
                          writes=[wg[kw].b], sembuf=wg[kw].b)
                    S.dma("pool", wu[kw].t[:],
                          wgu[:, FF + c * 512:FF + (c + 1) * 512].rearrange("(c p) f -> p c f", p=128),
                          writes=[wu[kw].b], sembuf=wu[kw].b)
                    S.dma("pool", wd[kw].t[:], wdn[c * 512:(c + 1) * 512, :].rearrange("(j p) d -> p j d", p=128),
                          writes=[wd[kw].b], sembuf=wd[kw].b)
                    return kw

                def gu_block(g, kw, a, j, c0, cwd):
                    h = hT[g["k"] % 2]
                    kk = cnt["gu"] % 2
                    cnt["gu"] += 1
                    pg, pu, sgt = psG[kk], psU[kk], sg[kk]
                    for ch in range(8):
                        S.op("pe", lambda e: e.matmul(pg.t[:, 0:cwd], lhsT=wg[kw].t[:, ch, j * 128:(j + 1) * 128],
                                                      rhs=h.t[:, ch, c0:c0 + cwd],
                                                      start=(ch == 0), stop=(ch == 7)),
                             reads=[wg[kw].b, h.b], writes=[pg.b])
                    for ch in range(8):
                        S.op("pe", lambda e: e.matmul(pu.t[:, 0:cwd], lhsT=wu[kw].t[:, ch, j * 128:(j + 1) * 128],
                                                      rhs=h.t[:, ch, c0:c0 + cwd],
                                                      start=(ch == 0), stop=(ch == 7)),
                             reads=[wu[kw].b, h.b], writes=[pu.b])
                    S.op("act", lambda e: e.activation(out=sgt.t[:, 0:cwd], in_=pg.t[:, 0:cwd], func=AF.Silu),
                         reads=[pg.b], writes=[sgt.b])
                    S.op("dve", lambda e: e.tensor_tensor(out=a.t[:, j, c0:c0 + cwd],
                                                          in0=pu.t[:, 0:cwd], in1=sgt.t[:, 0:cwd], op=ALU.mult),
                         reads=[pu.b, sgt.b], writes=[a.b])

                def down(g, c, kw, a):
                    for i in range(g["nt"]):
                        for dh in range(2):
                            pd = psD[cnt["d"] % 2]
                            cnt["d"] += 1
                            for j in range(4):
                                S.op("pe", lambda e: e.matmul(pd.t[:], lhsT=a.t[:, j, i * 128:(i + 1) * 128],
                                                              rhs=wd[kw].t[:, j, dh * 512:(dh + 1) * 512],
                                                              start=(j == 0), stop=(j == 3)),
                                     reads=[a.b, wd[kw].b], writes=[pd.b])
                            av = acc.t[:, i, dh * 512:(dh + 1) * 512]
                            if c == 0:
                                S.op("dve", lambda e: e.tensor_copy(out=av, in_=pd.t[:]), reads=[pd.b], writes=[acc.b])
                            else:
                                S.op("dve", lambda e: e.tensor_tensor(out=av, in0=pd.t[:], in1=av, op=ALU.add),
                                     reads=[pd.b, acc.b], writes=[acc.b])

                g0 = groups[0]
                for bi in range(len(g0["batches"])):
                    load_batch(g0, bi)
                    norm_batch(g0, bi)
                    trans_batch(g0, bi)
                pend_d = None
                NCH = 7 * len(groups)
                kw_of = {0: wload(0), 1: wload(1)}
                for k, g in enumerate(groups):
                    nxt = groups[k + 1] if k + 1 < len(groups) else None
                    nbn = len(nxt["batches"]) if nxt is not None else 0
                    for c in range(7):
                        t = 7 * k + c
                        kw = kw_of[t]
                        a = aT[cnt["a"] % 2]
                        cnt["a"] += 1
                        blocks = [(j, c0, cwd) for j in range(4) for (c0, cwd) in g["splits"]]
                        gu_block(g, kw, a, *blocks[0])
                        if pend_d is not None:
                            down(*pend_d)
                            if t + 1 < NCH:
                                kw_of[t + 1] = wload((t + 1) % 7)
                        conv_step(1)
                        if c == 0 and k > 0:
                            epilogue(groups[k - 1])
                        if 0 <= c - 2 < nbn:
                            trans_batch(nxt, c - 2)
                        for blk in blocks[1:]:
                            gu_block(g, kw, a, *blk)
                        if 0 <= c - 1 < nbn:
                            norm_batch(nxt, c - 1)
                        if c < nbn:
                            load_batch(nxt, c)
                        pend_d = (g, c, kw, a)
                down(*pend_d)
                epilogue(groups[-1])
                S.end_phase()


        def phase_moe(l):
            es0 = ExitStack()
            with es0:
                slot = alloc(es0, "slot", [128, 32, 2], U32)
                gates = alloc(es0, "gates", [128, 32, 2], F32)
                idxw = alloc(es0, "idxw", [128, NSEG * 7], U32)
                es = ExitStack()
                with es:
                    gs_x = load_bc(es, "mgs_x", 4)
                    sh_x = load_bc(es, "msh_x", 3)
                    hb_all = alloc(es, "hb_all", [128, 32, D], BF16)
                    xts = allocn(es, 4, "mxt", [128, D], F32, dma=True)
                    junk = alloc(es, "mjunk", [128, D], BF16)
                    tmp = allocn(es, 2, "mtmp", [128, D], F32)
                    hb32 = allocn(es, 3, "mhb32", [128, D], F32)
                    sss = allocn(es, 2, "mss", [128, 1], F32)
                    rstds = allocn(es, 2, "mrstd", [128, 1], F32)
                    tp32 = allocn(es, 2, "mtp32", [128, 4, 128], F32, psum=True)
                    pl = allocn(es, 2, "mpl", [128, 512], F32, psum=True)
                    hT32 = allocn(es, 2, "mhT32", [128, 8, 128], F32)
                    wr = alloc(es, "mwr", [128, 8, NE], F32, dma=True)
                    S.dma("sp", wr.t[:], w_rt, writes=[wr.b], sembuf=wr.b)
                    M1 = alloc(es, "M1", [128, 32, NE], F32)
                    M2 = alloc(es, "M2", [128, 32, NE], F32)
                    M12 = allocn(es, 2, "M12", [128, NE], F32)
                    rt = alloc(es, "rt", [128, 32, 2 * NE], F32)
                    cum = alloc(es, "cum", [128, 32, NE], F32)
                    rank = alloc(es, "rank", [128, 32, NE], F32)
                    lg = allocn(es, 2, "lg", [128, NE], F32)
                    l2 = allocn(es, 2, "l2", [128, NE], F32)
                    m1 = allocn(es, 2, "m1", [128, 1], F32)
                    m2 = allocn(es, 2, "m2", [128, 1], F32)
                    dd = alloc(es, "dd", [128, 32], F32)
                    scb = S.buf(dma="pool")
                    V_ = lambda fn, r, w: S.op("dve", fn, reads=r, writes=w)
                    G_ = lambda fn, r, w: S.op("pool", fn, reads=r, writes=w)
                    NT = 32

                    def r_load(i):
                        xt = xts[i % 4]
                        S.dma("sp", xt.t[:], y_out[i * 128:(i + 1) * 128, :], writes=[xt.b], sembuf=xt.b)

                    def r_norm(i):
                        k2 = i % 2
                        xt = xts[i % 4]
                        h32 = hb32[i % 3]
                        S.op("act", lambda e: e.activation(out=junk.t[:], in_=xt.t[:], func=AF.Square,
                                                           accum_out=sss[k2].t[:]),
                             reads=[xt.b], writes=[junk.b, sss[k2].b])
                        rstd_from_ss(sss[k2], rstds[k2], 1.0 / D)
                        V_(lambda e: e.scalar_tensor_tensor(out=tmp[k2].t[:], in0=xt.t[:], scalar=rstds[k2].t[:, 0:1],
                                                            in1=gs_x.t[:], op0=ALU.mult, op1=ALU.mult),
                           [xt.b, rstds[k2].b, gs_x.b], [tmp[k2].b])
                        G_(lambda e: e.tensor_tensor(out=h32.t[:], in0=tmp[k2].t[:], in1=sh_x.t[:], op=ALU.add),
                           [tmp[k2].b, sh_x.b], [h32.b])
                        S.op("act", lambda e: e.activation(out=hb_all.t[:, i, :], in_=h32.t[:], func=AF.Copy),
                             reads=[h32.b], writes=[hb_all.b])

                    def r_trans(i):
                        k2 = i % 2
                        h32 = hb32[i % 3]
                        for half in range(2):
                            tpp = tp32[half]
                            for c in range(4):
                                cc = half * 4 + c
                                S.op("pe", lambda e: e.transpose(out=tpp.t[:, c, :], in_=h32.t[:, cc * 128:(cc + 1) * 128],
                                                                 identity=ident32.t[:]),
                                     reads=[h32.b, ident32.b], writes=[tpp.b])
                            S.op("act", lambda e: e.activation(out=hT32[k2].t[:, half * 4:half * 4 + 4, :], in_=tpp.t[:],
                                                               func=AF.Copy), reads=[tpp.b], writes=[hT32[k2].b])

                    def r_route(i):
                        k2 = i % 2
                        p = pl[k2]
                        for c in range(8):
                            S.op("pe", lambda e: e.matmul(p.t[:, 0:NE], lhsT=hT32[k2].t[:, c, :], rhs=wr.t[:, c, :],
                                                          start=(c == 0), stop=(c == 7)),
                                 reads=[hT32[k2].b, wr.b], writes=[p.b])
                        lgk, l2k, m1k, m2k = lg[k2], l2[k2], m1[k2], m2[k2]
                        V_(lambda e: e.tensor_copy(out=lgk.t[:], in_=p.t[:, 0:NE]), [p.b], [lgk.b])
                        V_(lambda e: e.reduce_max(out=m1k.t[:], in_=lgk.t[:], axis=AX.X), [lgk.b], [m1k.b])
                        V_(lambda e: e.tensor_scalar(out=M1.t[:, i, :], in0=lgk.t[:], scalar1=m1k.t[:, 0:1], scalar2=None,
                                                     op0=ALU.is_ge), [lgk.b, m1k.b], [M1.b])
                        V_(lambda e: e.scalar_tensor_tensor(out=l2k.t[:], in0=M1.t[:, i, :], scalar=-1e30, in1=lgk.t[:],
                                                            op0=ALU.mult, op1=ALU.add), [M1.b, lgk.b], [l2k.b])
                        V_(lambda e: e.reduce_max(out=m2k.t[:], in_=l2k.t[:], axis=AX.X), [l2k.b], [m2k.b])
                        V_(lambda e: e.tensor_scalar(out=M2.t[:, i, :], in0=l2k.t[:], scalar1=m2k.t[:, 0:1], scalar2=None,
                                                     op0=ALU.is_ge), [l2k.b, m2k.b], [M2.b])
                        V_(lambda e: e.tensor_tensor(out=dd.t[:, i:i + 1], in0=m1k.t[:], in1=m2k.t[:], op=ALU.subtract),
                           [m1k.b, m2k.b], [dd.b])
                        mk = M12[k2]
                        V_(lambda e: e.tensor_tensor(out=mk.t[:], in0=M1.t[:, i, :], in1=M2.t[:, i, :], op=ALU.add),
                           [M1.b, M2.b], [mk.b])
                        S.op("pe", lambda e: e.matmul(p.t[:, 64:64 + NE], lhsT=tri32.t[:], rhs=mk.t[:], start=True, stop=True),
                             reads=[tri32.b, mk.b], writes=[p.b])
                        S.op("pe", lambda e: e.matmul(p.t[:, 64 + NE:64 + 2 * NE], lhsT=ones32.t[:], rhs=mk.t[:],
                                                      start=True, stop=True),
                             reads=[ones32.b, mk.b], writes=[p.b])
                        V_(lambda e: e.tensor_copy(out=rt.t[:, i, :], in_=p.t[:, 64:64 + 2 * NE]), [p.b], [rt.b])

                    for i in range(min(3, NT)):
                        r_load(i)
                    for st in range(NT + 2):
                        if st + 3 < NT:
                            r_load(st + 3)
                        if st < NT:
                            r_norm(st)
                        if 0 <= st - 1 < NT:
                            r_trans(st - 1)
                        if 0 <= st - 2 < NT:
                            r_route(st - 2)
                    S.op("act", lambda e: e.activation(out=gates.t[:, :, 0], in_=dd.t[:, :], func=AF.Sigmoid),
                         reads=[dd.b], writes=[gates.b])
                    V_(lambda e: e.tensor_scalar(out=gates.t[:, :, 1], in0=gates.t[:, :, 0], scalar1=-1.0, scalar2=1.0,
                                                 op0=ALU.mult, op1=ALU.add), [gates.b], [gates.b])
                    G_(lambda e: e.memset(cum.t[:, 0, :], 0.0), [], [cum.b])
                    for i in range(1, NT):
                        V_(lambda e: e.tensor_tensor(out=cum.t[:, i, :], in0=cum.t[:, i - 1, :], in1=rt.t[:, i - 1, NE:2 * NE],
                                                     op=ALU.add), [cum.b, rt.b], [cum.b])
                    V_(lambda e: e.tensor_tensor(out=rank.t[:], in0=rt.t[:, :, 0:NE], in1=cum.t[:], op=ALU.add),
                       [rt.b, cum.b], [rank.b])
                    nb = alloc(es, "nb", [128, NE], F32)
                    pad = alloc(es, "pad", [128, NE], F32)
                    pend = alloc(es, "pend", [128, NE], F32)
                    off = alloc(es, "off", [128, NE], F32)
                    V_(lambda e: e.tensor_tensor(out=nb.t[:], in0=cum.t[:, NT - 1, :], in1=rt.t[:, NT - 1, NE:2 * NE],
                                                 op=ALU.add), [cum.b, rt.b], [nb.b])
                    cmp0 = alloc(es, "cmp0", [128, NE], F32)
                    V_(lambda e: e.tensor_single_scalar(out=pad.t[:], in_=nb.t[:], scalar=0.0, op=ALU.is_gt), [nb.b], [pad.b])
                    for kq in range(1, 8):
                        V_(lambda e: e.tensor_single_scalar(out=cmp0.t[:], in_=nb.t[:], scalar=float(512 * kq), op=ALU.is_gt),
                           [nb.b], [cmp0.b])
                        V_(lambda e: e.tensor_tensor(out=pad.t[:], in0=pad.t[:], in1=cmp0.t[:], op=ALU.add),
                           [pad.b, cmp0.b], [pad.b])
                    V_(lambda e: e.tensor_single_scalar(out=pad.t[:], in_=pad.t[:], scalar=512.0, op=ALU.mult), [pad.b], [pad.b])
                    V_(lambda e: e.tensor_copy(out=pend.t[:, 0:1], in_=pad.t[:, 0:1]), [pad.b], [pend.b])
                    for ex in range(1, NE):
                        V_(lambda e: e.tensor_tensor(out=pend.t[:, ex:ex + 1], in0=pend.t[:, ex - 1:ex], in1=pad.t[:, ex:ex + 1],
                                                     op=ALU.add), [pend.b, pad.b], [pend.b])
                    V_(lambda e: e.tensor_tensor(out=off.t[:], in0=pend.t[:], in1=pad.t[:], op=ALU.subtract),
                       [pend.b, pad.b], [off.b])
                    V_(lambda e: e.tensor_tensor(out=rank.t[:], in0=rank.t[:],
                                                 in1=off.t[:].unsqueeze(1).to_broadcast([128, 32, NE]), op=ALU.add),
                       [rank.b, off.b], [rank.b])
                    prod = alloc(es, "prod", [128, 32, NE], F32)
                    slotf = alloc(es, "slotf", [128, 32, 2], F32)
                    for k, Mk in enumerate((M1, M2)):
                        V_(lambda e: e.tensor_tensor(out=prod.t[:], in0=rank.t[:], in1=Mk.t[:], op=ALU.mult),
                           [rank.b, Mk.b], [prod.b])
                        V_(lambda e: e.reduce_sum(out=slotf.t[:, :, k], in_=prod.t[:], axis=AX.X), [prod.b], [slotf.b])
                    V_(lambda e: e.tensor_copy(out=slot.t[:], in_=slotf.t[:]), [slotf.b], [slot.b])
                    eseg = alloc(es, "eseg", [128, NSEG], F32)
                    cmpt = alloc(es, "cmpt", [128, NE], F32)
                    pci = alloc(es, "pci", [128, 7], I32)
                    pcf = alloc(es, "pcf", [128, 7], F32)
                    idxf = alloc(es, "idxf", [128, NSEG, 7], F32)
                    for sg_ in range(NSEG):
                        V_(lambda e: e.tensor_single_scalar(out=cmpt.t[:], in_=pend.t[:], scalar=float(512 * sg_), op=ALU.is_le),
                           [pend.b], [cmpt.b])
                        V_(lambda e: e.reduce_sum(out=eseg.t[:, sg_:sg_ + 1], in_=cmpt.t[:], axis=AX.X), [cmpt.b], [eseg.b])
                    V_(lambda e: e.tensor_scalar(out=eseg.t[:], in0=eseg.t[:], scalar1=float(NE - 1), scalar2=896.0,
                                                 op0=ALU.min, op1=ALU.mult), [eseg.b], [eseg.b])
                    G_(lambda e: e.iota(pci.t[:], pattern=[[128, 7]], base=0, channel_multiplier=1), [], [pci.b])
                    V_(lambda e: e.tensor_copy(out=pcf.t[:], in_=pci.t[:]), [pci.b], [pcf.b])
                    for sg_ in range(NSEG):
                        V_(lambda e: e.tensor_scalar(out=idxf.t[:, sg_, :], in0=pcf.t[:], scalar1=eseg.t[:, sg_:sg_ + 1],
                                                     scalar2=None, op0=ALU.add), [pcf.b, eseg.b], [idxf.b])
                    V_(lambda e: e.tensor_copy(out=idxw.t[:], in_=idxf.t[:].rearrange("p s c -> p (s c)")), [idxf.b], [idxw.b])
                    for i in range(32):
                        for k in range(2):
                            S.dma_fn("pool", lambda e: e.indirect_dma_start(
                                out=hs, out_offset=bass.IndirectOffsetOnAxis(ap=slot.t[:, i, k:k + 1], axis=0),
                                in_=hb_all.t[:, i, :], in_offset=None),
                                reads=[slot.b, hb_all.b, hsz], sembuf=scb)
                    if dbg is not None and dbg.get("moe_dump"):
                        S.dma("sp", dbg_out[:, 0:64], slot.t[:].rearrange("p i k -> p (i k)").bitcast(F32), reads=[slot.b], sembuf=xts[0].b)
                        S.dma("sp", dbg_out[:, 64:128], gates.t[:].rearrange("p i k -> p (i k)"), reads=[gates.b], sembuf=xts[0].b)
                        S.dma("sp", dbg_out[:, 128:128 + NSEG * 7], idxw.t[:].bitcast(F32), reads=[idxw.b], sembuf=xts[0].b)
                    S.end_phase()
                conv_step(10 ** 6)
                es = ExitStack()
                with es:
                    hsl = allocn(es, 4, "hsl", [128, D], BF16, dma=True)
                    hT = allocn(es, 2, "shT", [128, 8, 512], BF16)
                    tp = allocn(es, 2, "stp", [128, 8, 128], BF16, psum=True)
                    psG = allocn(es, 2, "spsG", [128, 512], F32, psum=True)
                    psU = allocn(es, 2, "spsU", [128, 512], F32, psum=True)
                    psD = allocn(es, 2, "spsD", [128, 512], F32, psum=True)
                    wg = allocn(es, 2, "swg", [128, 8, 512], BF16, dma="pool")
                    wu = allocn(es, 2, "swu", [128, 8, 512], BF16, dma="pool")
                    wd = allocn(es, 2, "swd", [128, 4, D], BF16, dma="pool")
                    sg = allocn(es, 2, "ssg", [128, 512], F32)
                    aT = allocn(es, 2, "saT", [128, 4, 512], BF16)
                    acc = allocn(es, 2, "sacc", [128, 4, D], F32, dma=True)
                    cnt = dict(x=0, w=0, gu=0, d=0, a=0)

                    def prep_load(seg):
                        for i in range(4):
                            hl = hsl[i]
                            S.dma("sp", hl.t[:], hs[seg * 512 + i * 128:seg * 512 + (i + 1) * 128, :], writes=[hl.b], sembuf=hl.b)

                    def prep_trans(seg):
                        h = hT[seg % 2]
                        for i in range(4):
                            hl = hsl[i]
                            t = tp[i % 2]
                            for c in range(8):
                                S.op("pe", lambda e: e.transpose(out=t.t[:, c, :], in_=hl.t[:, c * 128:(c + 1) * 128],
                                                                 identity=ident.t[:]),
                                     reads=[hl.b, ident.b], writes=[t.b])
                            S.op("act", lambda e: e.activation(out=h.t[:, :, i * 128:(i + 1) * 128], in_=t.t[:], func=AF.Copy),
                                 reads=[t.b], writes=[h.b])

                    def wload(seg, c):
                        k = cnt["w"] % 2
                        cnt["w"] += 1
                        ia = idxw.t[:, seg * 7 + c:seg * 7 + c + 1]
                        for (dst, src) in ((wg[k], WGd), (wu[k], WUd), (wd[k], WDd)):
                            S.dma_fn("pool", lambda e: e.indirect_dma_start(
                                out=dst.t[:].rearrange("p a b -> p (a b)"), out_offset=None, in_=src,
                                in_offset=bass.IndirectOffsetOnAxis(ap=ia, axis=0)),
                                reads=[idxw.b, convb], writes=[dst.b], sembuf=dst.b)
                        return k

                    def gu(seg, k, a, j):
                        h = hT[seg % 2]
                        kk = cnt["gu"] % 2
                        cnt["gu"] += 1
                        pg, pu, sgt = psG[kk], psU[kk], sg[kk]
                        for ch in range(8):
                            S.op("pe", lambda e: e.matmul(pg.t[:], lhsT=wg[k].t[:, ch, j * 128:(j + 1) * 128],
                                                          rhs=h.t[:, ch, :], start=(ch == 0), stop=(ch == 7)),
                                 reads=[wg[k].b, h.b], writes=[pg.b])
                        for ch in range(8):
                            S.op("pe", lambda e: e.matmul(pu.t[:], lhsT=wu[k].t[:, ch, j * 128:(j + 1) * 128],
                                                          rhs=h.t[:, ch, :], start=(ch == 0), stop=(ch == 7)),
                                 reads=[wu[k].b, h.b], writes=[pu.b])
                        S.op("act", lambda e: e.activation(out=sgt.t[:], in_=pg.t[:], func=AF.Silu),
                             reads=[pg.b], writes=[sgt.b])
                        S.op("dve", lambda e: e.tensor_tensor(out=a.t[:, j, :], in0=pu.t[:], in1=sgt.t[:], op=ALU.mult),
                             reads=[pu.b, sgt.b], writes=[a.b])

                    def down(seg, c, k, a):
                        ac = acc[seg % 2]
                        for i in range(4):
                            for dh in range(2):
                                pd = psD[cnt["d"] % 2]
                                cnt["d"] += 1
                                for j in range(4):
                                    S.op("pe", lambda e: e.matmul(pd.t[:], lhsT=a.t[:, j, i * 128:(i + 1) * 128],
                                                                  rhs=wd[k].t[:, j, dh * 512:(dh + 1) * 512],
                                                                  start=(j == 0), stop=(j == 3)),
                                         reads=[a.b, wd[k].b], writes=[pd.b])
                                av = ac.t[:, i, dh * 512:(dh + 1) * 512]
                                if c == 0:
                                    S.op("dve", lambda e: e.tensor_copy(out=av, in_=pd.t[:]), reads=[pd.b], writes=[ac.b])
                                else:
                                    S.op("dve", lambda e: e.tensor_tensor(out=av, in0=pd.t[:], in1=av, op=ALU.add),
                                         reads=[pd.b, ac.b], writes=[ac.b])
                        if c == 6:
                            S.dma("sp", ys[seg * 512:(seg + 1) * 512, :].rearrange("(i p) d -> p i d", p=128), ac.t[:],
                                  reads=[ac.b], sembuf=ac.b)

                    prep_load(0)
                    prep_trans(0)
                    pend_d = None
                    for seg in range(NSEG):
                        for c in range(7):
                            k = wload(seg, c)
                            a = aT[cnt["a"] % 2]
                            cnt["a"] += 1
                            gu(seg, k, a, 0)
                            if pend_d is not None:
                                down(*pend_d)
                            for j in range(1, 4):
                                gu(seg, k, a, j)
                            pend_d = (seg, c, k, a)
                            if seg + 1 < NSEG:
                                if c == 1:
                                    prep_load(seg + 1)
                                if c == 4:
                                    prep_trans(seg + 1)
                    down(*pend_d)
                    S.end_phase()
                es = ExitStack()
                with es:
                    ggt_x = load_bc(es, "cggt_x", 5)
                    NBUF = 4
                    r1 = allocn(es, NBUF, "r1", [128, D], F32, dma="pool")
                    r2 = allocn(es, NBUF, "r2", [128, D], F32, dma="pool")
                    xts = allocn(es, NBUF, "cxt", [128, D], F32, dma=True)
                    a1 = allocn(es, 2, "a1", [128, D], F32)
                    junk = alloc(es, "cjunk", [128, D], BF16)
                    sss = allocn(es, 2, "css", [128, 1], F32)
                    rstds = allocn(es, 2, "crstd", [128, 1], F32)
                    tmp = allocn(es, 2, "ctmp", [128, D], F32)

                    def c_issue(i):
                        kb = i % NBUF
                        for (r, k) in ((r1[kb], 0), (r2[kb], 1)):
                            S.dma_fn("pool", lambda e: e.indirect_dma_start(
                                out=r.t[:], out_offset=None, in_=ys,
                                in_offset=bass.IndirectOffsetOnAxis(ap=slot.t[:, i, k:k + 1], axis=0)),
                                reads=[slot.b], writes=[r.b], sembuf=r.b)
                        xt = xts[kb]
                        S.dma("sp", xt.t[:], y_out[i * 128:(i + 1) * 128, :], writes=[xt.b], sembuf=xt.b)

                    for i in range(NBUF - 1):
                        c_issue(i)
                    for i in range(32):
                        if i + NBUF - 1 < 32:
                            c_issue(i + NBUF - 1)
                        k2 = i % 2
                        kb = i % NBUF
                        xt = xts[kb]
                        S.op("act", lambda e: e.activation(out=a1[k2].t[:], in_=r1[kb].t[:], func=AF.Copy,
                                                           scale=gates.t[:, i, 0:1]),
                             reads=[r1[kb].b, gates.b], writes=[a1[k2].b])
                        S.op("dve", lambda e: e.scalar_tensor_tensor(out=a1[k2].t[:], in0=r2[kb].t[:], scalar=gates.t[:, i, 1:2],
                                                                     in1=a1[k2].t[:], op0=ALU.mult, op1=ALU.add),
                             reads=[r2[kb].b, gates.b, a1[k2].b], writes=[a1[k2].b])
                        S.op("act", lambda e: e.activation(out=junk.t[:], in_=a1[k2].t[:], func=AF.Square,
                                                           accum_out=sss[k2].t[:]),
                             reads=[a1[k2].b], writes=[junk.b, sss[k2].b])
                        rstd_from_ss(sss[k2], rstds[k2], 1.0 / D)
                        S.op("dve", lambda e: e.scalar_tensor_tensor(out=tmp[k2].t[:], in0=a1[k2].t[:],
                                                                     scalar=rstds[k2].t[:, 0:1], in1=ggt_x.t[:],
                                                                     op0=ALU.mult, op1=ALU.mult),
                             reads=[a1[k2].b, rstds[k2].b, ggt_x.b], writes=[tmp[k2].b])
                        S.op("pool", lambda e: e.tensor_tensor(out=xt.t[:], in0=tmp[k2].t[:], in1=xt.t[:], op=ALU.add),
                             reads=[tmp[k2].b, xt.b], writes=[xt.b])
                        S.dma("sp", y_out[i * 128:(i + 1) * 128, :], xt.t[:], reads=[xt.b], sembuf=xt.b)
                    S.end_phase()

        stop_after = dbg.get("stop") if dbg else None
        done = False
        for l in range(DEPTH):
            last = (l == DEPTH - 1)
            for name, fn in (("ada", lambda: phase_ada(l)), ("a", lambda: phase_a(l, last)),
                             ("b", lambda: phase_b(l, last)),
                             ("f", (lambda: phase_moe(l)) if (l % 2 == 1 and last) else (lambda: phase_f(l, last)))):
                fn()
                if stop_after == (l, name):
                    done = True
                    break
            if done:
                break
        if dbg is not None:
            es = ExitStack()
            with es:
                srcd = dict(mods=mods, QTd=QTd, KTd=KTd, Vd=Vd, cnTd=cnTd, QcTd=QcTd, KcTd=KcTd, Vcd=Vcd,
                            cncTd=cncTd, ctxs=ctxs)[dbg["src"]]
                b = S.buf(dma=True)
                S.dma("sp", dbg_out, srcd, sembuf=b)
                S.end_phase()
    return nc


def _host_inputs(inputs):
    f = lambda a: np.ascontiguousarray(np.asarray(a, dtype=np.float32))
    x = f(inputs["x"]); c = f(inputs["c"]); ctx = f(inputs["ctx"]); c_ctx = f(inputs["c_ctx"])
    rpb = f(inputs["rpb"])
    conv_w = f(inputs["conv_w"])
    gout = f(inputs["out_norm_g"])
    q = np.arange(128)
    qr_off = q // 64
    qc = q % 64
    cs = np.clip(qc - 8, 0, 48)

    def table(p, bs, nrows):
        qr = 2 * p + qr_off
        rs = np.clip(qr - 4, 0, 56)
        kk = np.arange(nrows * 64)
        kr = bs + kk // 64
        kc = kk % 64
        valid = ((kr[None, :] >= rs[:, None]) & (kr[None, :] < rs[:, None] + 8) &
                 (kc[None, :] >= cs[:, None]) & (kc[None, :] < cs[:, None] + 16))
        dy = np.clip(kr[None, :] - qr[:, None] + 7, 0, 14)
        dx = np.clip(kc[None, :] - qc[:, None] + 15, 0, 30)
        g = rpb[:, :, dy, dx]
        return np.where(valid[None, None], g, np.float32(NEG)).astype(np.float32)

    mid = table(10, 16, 9)
    nab_mid = np.ascontiguousarray(mid.transpose(0, 2, 1, 3))
    edges = [table(0, 0, 8), table(1, 0, 8), table(30, 56, 8), table(31, 56, 8)]
    nab_edge = np.ascontiguousarray(np.stack(edges, axis=1))
    convw = np.ascontiguousarray(conv_w.reshape(DEPTH, 3, 4, 128).transpose(0, 3, 2, 1).reshape(DEPTH, 128, 12))
    goutc = np.ascontiguousarray(gout[:, 512:].reshape(DEPTH, 4, 128).transpose(0, 2, 1))
    w_rt = np.ascontiguousarray(f(inputs["w_router"])[0].reshape(8, 128, NE).transpose(1, 0, 2))
    shared = {
        "w_ada": f(inputs["w_ada"]), "b_ada": f(inputs["b_ada"]),
        "norm_g": f(inputs["norm_g"]).reshape(DEPTH * 4, D),
        "w_in": f(inputs["w_in"]), "nab_mid": nab_mid, "nab_edge": nab_edge, "convw": convw,
        "gout": gout, "goutc": goutc, "w_out": f(inputs["w_out"]),
        "w_gu_dense": f(inputs["w_gu_dense"]), "w_down_dense": f(inputs["w_down_dense"]),
        "w_router": w_rt, "w_gu_moe": f(inputs["w_gu_moe"])[0], "w_down_moe": f(inputs["w_down_moe"])[0],
    }
    maps = []
    for b in range(x.shape[0]):
        cv = np.concatenate([c[b].reshape(8, 128).T, c_ctx.reshape(8, 128).T], axis=1)
        m = dict(shared)
        m["x"] = x[b]
        m["ctx"] = ctx[b]
        m["cvec"] = np.ascontiguousarray(cv)
        maps.append(m)
    return maps


def kernel(**inputs):
    maps = _host_inputs(inputs)
    nc = build_nc()
    res = run_bass_kernel_spmd(nc, maps, core_ids=list(range(len(maps))))
    return np.stack([np.asarray(r["y"], dtype=np.float32) for r in res.results], axis=0)
```

```python
import numpy as np
from contextlib import ExitStack
import concourse.bass as bass
import concourse.mybir as mybir
from concourse.bass_utils import run_bass_kernel_spmd

F32 = mybir.dt.float32
BF16 = mybir.dt.bfloat16
U32 = mybir.dt.uint32
I32 = mybir.dt.int32
NSEG = 23
PIPE_LAGS = (1, 2)
NSLOT = NSEG * 512
AF = mybir.ActivationFunctionType
ALU = mybir.AluOpType
AX = mybir.AxisListType

D = 1024
S_LEN = 4096
CTX = 256
DEPTH = 2
FF = 3584
NE = 8
INW = 3072
NEG = -30000.0


class Buf:
    __slots__ = ("w", "r", "dsem", "psum")

    def __init__(self):
        self.w = None
        self.r = []
        self.dsem = None
        self.psum = False


class Sched:
    def __init__(self, nc, es):
        self.nc = nc
        self.es = es
        self.engs = {}
        for name, e in (("pe", nc.tensor), ("act", nc.scalar), ("dve", nc.vector),
                        ("pool", nc.gpsimd), ("sp", nc.sync)):
            sem = es.enter_context(nc.semaphore("prog_" + name))
            self.engs[name] = dict(e=e, sem=sem, count=0, seen={})
        self.free_dsems = {"sp": [], "pool": []}
        self.all_dsems = []
        self.phase_dsems = []
        self.persist = []

    def buf(self, dma=False, persist=False):
        b = Buf()
        if dma:
            kind = "pool" if dma == "pool" else "sp"
            if self.free_dsems[kind] and not persist:
                d = self.free_dsems[kind].pop()
            else:
                sem = self.es.enter_context(self.nc.semaphore("dsem%d" % (len(self.all_dsems) + len(self.persist))))
                d = dict(sem=sem, count=0, kind=kind)
                (self.persist if persist else self.all_dsems).append(d)
            b.dsem = d
            if not persist:
                self.phase_dsems.append(d)
        return b

    def _wait(self, eng, deps):
        best = {}
        for sem, val in deps:
            k = id(sem)
            if k not in best or best[k][1] < val:
                best[k] = (sem, val)
        for k, (sem, val) in best.items():
            if eng["seen"].get(k, 0) < val:
                eng["e"].wait_ge(sem, val)
                eng["seen"][k] = val

    @staticmethod
    def _deps(reads, writes):
        deps = []
        for b in reads:
            if b.w is not None:
                deps.append(b.w)
            if b.psum:
                deps.extend(b.r)
        for b in writes:
            if b.w is not None:
                deps.append(b.w)
            deps.extend(b.r)
        return deps

    @staticmethod
    def _mark(tok, reads, writes):
        for b in reads:
            b.r.append(tok)
            if len(b.r) > 24:
                best = {}
                for s, v in b.r:
                    if id(s) not in best or best[id(s)][1] < v:
                        best[id(s)] = (s, v)
                b.r = list(best.values())
        for b in writes:
            b.w = tok
            b.r = []

    def op(self, engname, fn, reads=(), writes=()):
        eng = self.engs[engname]
        deps = self._deps(reads, writes)
        if engname == "pe":
            deps = [d for d in deps if d[0] is not eng["sem"]]
        self._wait(eng, deps)
        ins = fn(eng["e"])
        eng["count"] += 1
        ins.then_inc(eng["sem"], 1)
        eng["seen"].pop(id(eng["sem"]), None)
        self._mark((eng["sem"], eng["count"]), reads, writes)
        return ins

    def dma(self, engname, out, in_, reads=(), writes=(), sembuf=None):
        eng = self.engs[engname]
        self._wait(eng, self._deps(reads, writes))
        ins = eng["e"].dma_start(out=out, in_=in_)
        d = sembuf.dsem
        assert d["kind"] == ("pool" if engname == "pool" else "sp"), (d["kind"], engname)
        d["count"] += 16
        ins.then_inc(d["sem"], 16)
        self._mark((d["sem"], d["count"]), reads, writes)
        return ins

    def dma_fn(self, engname, fn, reads=(), writes=(), sembuf=None):
        eng = self.engs[engname]
        self._wait(eng, self._deps(reads, writes))
        ins = fn(eng["e"])
        d = sembuf.dsem
        assert d["kind"] == ("pool" if engname == "pool" else "sp"), (d["kind"], engname)
        d["count"] += 16
        ins.then_inc(d["sem"], 16)
        self._mark((d["sem"], d["count"]), reads, writes)
        return ins

    def barrier(self):
        deps = [(e["sem"], e["count"]) for e in self.engs.values() if e["count"] > 0]
        deps += [(d["sem"], d["count"]) for d in self.all_dsems if d["count"] > 0]
        for eng in self.engs.values():
            self._wait(eng, deps)

    def end_phase(self):
        self.barrier()
        for d in self.phase_dsems:
            self.free_dsems[d["kind"]].append(d)
        self.phase_dsems = []


class TT:
    def __init__(self, t, b):
        self.t = t
        self.b = b


def _bc_rows(handle_ap, row, n, parts=128):
    return handle_ap[row:row + 1, 0:n].partition_broadcast(parts)


def build_nc(dbg=None):
    nc = bass.Bass("TRN2", target_bir_lowering=False)
    di = lambda name, shape, dt=F32: nc.dram_tensor(name, list(shape), dt, kind="ExternalInput").ap()
    x_in = di("x", [S_LEN, D])
    ctx_in = di("ctx", [CTX, D])
    cvec = di("cvec", [128, 16])
    w_ada = di("w_ada", [DEPTH, D, 6 * D])
    b_ada = di("b_ada", [DEPTH, 6 * D])
    norm_g = di("norm_g", [DEPTH * 4, D])
    w_in = di("w_in", [DEPTH, D, INW])
    nab_mid = di("nab_mid", [DEPTH, 128, 8, 576])
    nab_edge = di("nab_edge", [DEPTH, 4, 8, 128, 512])
    convw = di("convw", [DEPTH, 128, 12])
    gout = di("gout", [DEPTH, D])
    goutc = di("goutc", [DEPTH, 128, 4])
    w_out = di("w_out", [DEPTH, D, D])
    w_gu_d = di("w_gu_dense", [1, D, 2 * FF])
    w_dn_d = di("w_down_dense", [1, FF, D])
    w_rt = di("w_router", [128, 8, NE])
    w_gu_m = di("w_gu_moe", [NE, D, 2 * FF])
    w_dn_m = di("w_down_moe", [NE, FF, D])
    y_out = nc.dram_tensor("y", [S_LEN, D], F32, kind="ExternalOutput").ap()

    ds = lambda name, shape, dt: nc.dram_tensor(name, list(shape), dt).ap()
    QTd = ds("QTd", [4, 128, S_LEN], BF16)
    KTd = ds("KTd", [4, 128, S_LEN], BF16)
    Vd = ds("Vd", [S_LEN, 512], BF16)
    cnTd = ds("cnTd", [4, 128, S_LEN], BF16)
    QcTd = ds("QcTd", [4, 128, CTX], BF16)
    KcTd = ds("KcTd", [4, 128, CTX], BF16)
    Vcd = ds("Vcd", [CTX, 512], BF16)
    cncTd = ds("cncTd", [4, 128, CTX], BF16)
    mods = ds("mods", [12, D], F32)
    WGd = ds("WGd", [NE * 7 * 128, 8 * 512], BF16)
    WUd = ds("WUd", [NE * 7 * 128, 8 * 512], BF16)
    WDd = ds("WDd", [NE * 7 * 128, 4 * D], BF16)
    hs = ds("hs", [NSLOT, D], BF16)
    ys = ds("ys", [NSLOT, D], F32)
    ctxs = ds("ctxs", [CTX, D], F32)
    dbg_out = None
    if dbg is not None:
        dbg_out = nc.dram_tensor("dbg", list(dbg["shape"]), dbg.get("dt", F32), kind="ExternalOutput").ap()

    ges = ExitStack()
    with ges:
        S = Sched(nc, ges)

        uid = [0]

        def alloc(es, name, shape, dt, dma=False, psum=False):
            uid[0] += 1
            name = "%s_%d" % (name, uid[0])
            if psum:
                t = es.enter_context(nc.psum_tensor(name, list(shape), dt))
            else:
                t = es.enter_context(nc.sbuf_tensor(name, list(shape), dt))
            tt = TT(t, S.buf(dma=dma))
            tt.b.psum = bool(psum)
            return tt

        def allocn(es, n, name, shape, dt, dma=False, psum=False):
            return [alloc(es, "%s%d" % (name, i), shape, dt, dma=dma, psum=psum) for i in range(n)]

        ident = alloc(ges, "ident", [128, 128], BF16)
        ident32 = alloc(ges, "ident32", [128, 128], F32)
        ones = alloc(ges, "ones", [128, 128], BF16)
        epsb = alloc(ges, "epsb", [128, 1], F32)
        for idt in (ident, ident32):
            S.op("pool", lambda e: e.memset(idt.t[:], 0.0), writes=[idt.b])
            S.op("pool", lambda e: e.affine_select(out=idt.t[:], in_=idt.t[:], pattern=[[-1, 128]],
                                                   compare_op=ALU.not_equal, fill=1.0, base=0,
                                                   channel_multiplier=1), reads=[idt.b], writes=[idt.b])
        S.op("pool", lambda e: e.memset(ones.t[:], 1.0), writes=[ones.b])
        S.op("pool", lambda e: e.memset(epsb.t[:], 1e-6), writes=[epsb.b])

        ones32 = alloc(ges, "ones32", [128, 128], F32)
        tri32 = alloc(ges, "tri32", [128, 128], F32)
        S.op("pool", lambda e: e.memset(ones32.t[:], 1.0), writes=[ones32.b])
        S.op("pool", lambda e: e.memset(tri32.t[:], 1.0), writes=[tri32.b])
        S.op("pool", lambda e: e.affine_select(out=tri32.t[:], in_=tri32.t[:], pattern=[[1, 128]],
                                               compare_op=ALU.is_gt, fill=0.0, base=0,
                                               channel_multiplier=-1), reads=[tri32.b], writes=[tri32.b])
        zt = alloc(ges, "zt", [128, D], BF16)
        S.op("pool", lambda e: e.memset(zt.t[:], 0.0), writes=[zt.b])
        hsz = S.buf(dma="pool", persist=True)
        convb = S.buf(dma="pool", persist=True)
        conv_pieces = []
        for r in range(NSLOT // 512):
            conv_pieces.append((hs[r * 512:(r + 1) * 512, :].rearrange("(r p) d -> p r d", p=128),
                                zt.t[:].unsqueeze(1).to_broadcast([128, 4, D]), [zt.b], hsz))
        for ex in range(NE):
            for c in range(7):
                r0 = (ex * 7 + c) * 128
                conv_pieces.append((WGd[r0:r0 + 128, :].rearrange("p (c f) -> p c f", c=8),
                                    w_gu_m[ex, :, c * 512:(c + 1) * 512].rearrange("(c p) f -> p c f", p=128), [], convb))
                conv_pieces.append((WUd[r0:r0 + 128, :].rearrange("p (c f) -> p c f", c=8),
                                    w_gu_m[ex, :, FF + c * 512:FF + (c + 1) * 512].rearrange("(c p) f -> p c f", p=128),
                                    [], convb))
                conv_pieces.append((WDd[r0:r0 + 128, :].rearrange("p (j d) -> p j d", j=4),
                                    w_dn_m[ex, c * 512:(c + 1) * 512, :].rearrange("(j p) d -> p j d", p=128), [], convb))
        conv_pos = [0]

        def conv_step(n):
            for _ in range(n):
                if conv_pos[0] >= len(conv_pieces):
                    return
                o, i_, rd, sb = conv_pieces[conv_pos[0]]
                conv_pos[0] += 1
                S.dma("pool", o, i_, reads=rd, sembuf=sb)
                sb.w = (sb.dsem["sem"], sb.dsem["count"])

        def rstd_from_ss(ss, rstd, scale):
            S.op("act", lambda e: e.activation(out=rstd.t[:], in_=ss.t[:], func=AF.Sqrt, scale=scale,
                                               bias=epsb.t[:]), reads=[ss.b, epsb.b], writes=[rstd.b])
            S.op("dve", lambda e: e.reciprocal(out=rstd.t[:], in_=rstd.t[:]), reads=[rstd.b], writes=[rstd.b])

        def phase_ada(l):
            es = ExitStack()
            with es:
                cv = alloc(es, "cv", [128, 16], F32, dma=True)
                sc = alloc(es, "sc", [128, 16], F32)
                scb = alloc(es, "scb", [128, 16], BF16)
                wch = allocn(es, 2, "wch", [128, 8, 512], BF16, dma="pool")
                row = alloc(es, "row", [1, 2, 6 * D], F32)
                bad = alloc(es, "bad", [1, 6 * D], F32, dma=True)
                ng = alloc(es, "ng", [1, 4, D], F32, dma=True)
                ps = allocn(es, 2, "adaps", [128, 512], F32, psum=True)
                stb = S.buf(dma=True)
                S.dma("sp", cv.t[:], cvec[:, :], writes=[cv.b], sembuf=cv.b)
                S.dma("sp", bad.t[:], b_ada[l:l + 1, :], writes=[bad.b], sembuf=bad.b)
                S.dma("sp", ng.t[:], norm_g[4 * l:4 * l + 4, :].rearrange("(o r) d -> o r d", o=1),
                      writes=[ng.b], sembuf=ng.b)
                S.op("act", lambda e: e.activation(out=sc.t[:], in_=cv.t[:], func=AF.Silu),
                     reads=[cv.b], writes=[sc.b])
                S.op("dve", lambda e: e.tensor_copy(out=scb.t[:], in_=sc.t[:]), reads=[sc.b], writes=[scb.b])
                for j in range(12):
                    w = wch[j % 2]
                    S.dma("pool", w.t[:], w_ada[l, :, j * 512:(j + 1) * 512].rearrange("(c p) f -> p c f", p=128),
                          writes=[w.b], sembuf=w.b)
                    for src in range(2):
                        p = ps[src]
                        for ch in range(8):
                            S.op("pe", lambda e: e.matmul(p.t[0:1, :], lhsT=scb.t[:, src * 8 + ch:src * 8 + ch + 1],
                                                          rhs=w.t[:, ch, :], start=(ch == 0), stop=(ch == 7)),
                                 reads=[scb.b, w.b], writes=[p.b])
                        S.op("dve", lambda e: e.tensor_tensor(out=row.t[0:1, src, j * 512:(j + 1) * 512],
                                                              in0=p.t[0:1, :], in1=bad.t[0:1, j * 512:(j + 1) * 512],
                                                              op=ALU.add),
                             reads=[p.b, bad.b], writes=[row.b])
                for src in range(2):
                    for (k, gi) in ((1, 0), (4, 2)):
                        S.op("dve", lambda e: e.scalar_tensor_tensor(
                            out=row.t[0:1, src, k * D:(k + 1) * D], in0=row.t[0:1, src, k * D:(k + 1) * D],
                            scalar=1.0, in1=ng.t[0:1, gi, :], op0=ALU.add, op1=ALU.mult),
                            reads=[row.b, ng.b], writes=[row.b])
                    for (k, gi) in ((2, 1), (5, 3)):
                        S.op("dve", lambda e: e.tensor_tensor(
                            out=row.t[0:1, src, k * D:(k + 1) * D], in0=row.t[0:1, src, k * D:(k + 1) * D],
                            in1=ng.t[0:1, gi, :], op=ALU.mult), reads=[row.b, ng.b], writes=[row.b])
                S.dma("sp", mods.rearrange("(o r) d -> o (r d)", o=1), row.t[0:1, :, :].rearrange("o s d -> o (s d)"),
                      reads=[row.b], sembuf=stb)
                S.end_phase()

        def load_bc(es, name, rowidx, n=D, src=None):
            t = alloc(es, name, [128, n], F32, dma=True)
            srcap = mods if src is None else src
            S.dma("sp", t.t[:], _bc_rows(srcap, rowidx, n), writes=[t.b], sembuf=t.b)
            return t

        def norm_tile(xt, gs, sh, ss, rstd, junk, tmp, hb, hb32=None):
            S.op("act", lambda e: e.activation(out=junk.t[:], in_=xt.t[:], func=AF.Square, accum_out=ss.t[:]),
                 reads=[xt.b], writes=[junk.b, ss.b])
            rstd_from_ss(ss, rstd, 1.0 / D)
            S.op("dve", lambda e: e.scalar_tensor_tensor(out=tmp.t[:], in0=xt.t[:], scalar=rstd.t[:, 0:1],
                                                         in1=gs.t[:], op0=ALU.mult, op1=ALU.mult),
                 reads=[xt.b, rstd.b, gs.b], writes=[tmp.b])
            if hb32 is not None:
                S.op("pool", lambda e: e.tensor_tensor(out=hb32.t[:], in0=tmp.t[:], in1=sh.t[:], op=ALU.add),
                     reads=[tmp.b, sh.b], writes=[hb32.b])
                S.op("pool", lambda e: e.tensor_copy(out=hb.t[:], in_=hb32.t[:]), reads=[hb32.b], writes=[hb.b])
            else:
                S.op("pool", lambda e: e.tensor_tensor(out=hb.t[:], in0=tmp.t[:], in1=sh.t[:], op=ALU.add),
                     reads=[tmp.b, sh.b], writes=[hb.b])

        def phase_a(l, last):
            es = ExitStack()
            with es:
                win = alloc(es, "win", [128, 8, INW], BF16)
                win_b = [S.buf(dma="pool") for _ in range(6)]
                for j in ([1, 2, 3, 4, 5, 0] if last else [3, 4, 5, 0, 1, 2]):
                    S.dma("pool", win.t[:, :, j * 512:(j + 1) * 512],
                          w_in[l, :, j * 512:(j + 1) * 512].rearrange("(c p) f -> p c f", p=128),
                          writes=[win_b[j]], sembuf=win_b[j])
                gs_x = load_bc(es, "gs_x", 1)
                sh_x = load_bc(es, "sh_x", 0)
                gs_c = load_bc(es, "gs_c", 6 + 1)
                sh_c = load_bc(es, "sh_c", 6 + 0)
                cw = alloc(es, "cw", [128, 12], F32, dma=True)
                S.dma("sp", cw.t[:], convw[l], writes=[cw.b], sembuf=cw.b)
                goc = alloc(es, "goc", [128, 4], F32, dma=True)
                S.dma("sp", goc.t[:], goutc[l], writes=[goc.b], sembuf=goc.b)

                xts = allocn(es, 4, "xt", [128, D], F32, dma=True)
                junk = alloc(es, "junk", [128, D], BF16)
                tmp = allocn(es, 2, "tmp", [128, D], F32)
                hbs = allocn(es, 4, "hb", [128, D], BF16)
                sss = allocn(es, 2, "ss", [128, 1], F32)
                rstds = allocn(es, 2, "rstd", [128, 1], F32)
                hT = allocn(es, 2, "hT", [128, 8, 512], BF16)
                tps = allocn(es, 2, "tp", [128, 8, 128], BF16, psum=True)
                mps = allocn(es, 4, "mp", [128, 512], F32, psum=True)
                sps = alloc(es, "sps", [128, 512], F32, psum=True)
                qks = allocn(es, 2, "qks", [128, 8, 512], BF16, dma=True)
                vst = allocn(es, 2, "vst", [128, 512], BF16, dma=True)
                BT = allocn(es, 2, "BT", [128, 4, 512], BF16)
                vT = allocn(es, 2, "vT", [128, 4, 514], F32)
                csb = allocn(es, 2, "csb", [128, 512], F32)
                cacc = allocn(es, 4, "cacc", [128, 512], F32)
                co = alloc(es, "co", [128, 4, 512], F32)
                sq = allocn(es, 4, "sq", [128, 512], BF16)
                rbc = alloc(es, "rbc", [128, 512], F32)
                cn = allocn(es, 2, "cn", [128, 4, 512], BF16, dma=True)
                cnt = dict(mp=0)

                csrc = ctx_in if l == 0 else ctxs
                xsrc = x_in if l == 0 else y_out
                G = [dict(src=csrc, dst=dict(Q=QcTd, K=KcTd, V=Vcd), tok0=0, T=CTX, gs=gs_c, sh=sh_c,
                          want_q=not last, want_conv=not last, lat=False, cdst=cncTd)]
                for g in range(8):
                    G.append(dict(src=xsrc, dst=dict(Q=QTd, K=KTd, V=Vd), tok0=g * 512, T=512, gs=gs_x, sh=sh_x,
                                  want_q=True, want_conv=True, lat=True, cdst=cnTd))
                n0 = 0
                for k, gd in enumerate(G):
                    gd["k"] = k
                    gd["n0"] = n0
                    n0 += gd["T"] // 128

                def load_part(gd):
                    for i in range(gd["T"] // 128):
                        n = gd["n0"] + i
                        xt = xts[n % 4]
                        S.dma("sp", xt.t[:], gd["src"][gd["tok0"] + i * 128:gd["tok0"] + (i + 1) * 128, :],
                              writes=[xt.b], sembuf=xt.b)

                def norm_part(gd):
                    for i in range(gd["T"] // 128):
                        n = gd["n0"] + i
                        xt = xts[n % 4]
                        k2 = n % 2
                        norm_tile(xt, gd["gs"], gd["sh"], sss[k2], rstds[k2], junk, tmp[k2], hbs[n % 4])

                def trans_part(gd):
                    h = hT[gd["k"] % 2]
                    for i in range(gd["T"] // 128):
                        n = gd["n0"] + i
                        tp = tps[n % 2]
                        hb_ = hbs[n % 4]
                        for c in range(8):
                            S.op("pe", lambda e: e.transpose(out=tp.t[:, c, :], in_=hb_.t[:, c * 128:(c + 1) * 128],
                                                             identity=ident.t[:]),
                                 reads=[hb_.b, ident.b], writes=[tp.b])
                        S.op("act", lambda e: e.activation(out=h.t[:, :, i * 128:(i + 1) * 128], in_=tp.t[:],
                                                           func=AF.Copy), reads=[tp.b], writes=[h.b])

                def fmm(gd, ft):
                    T = gd["T"]
                    h = hT[gd["k"] % 2]
                    p = mps[cnt["mp"] % 4]
                    cnt["mp"] += 1
                    for c in range(8):
                        S.op("pe", lambda e: e.matmul(p.t[:, 0:T], lhsT=win.t[:, c, ft * 128:(ft + 1) * 128],
                                                      rhs=h.t[:, c, 0:T], start=(c == 0), stop=(c == 7)),
                             reads=[win_b[ft // 4], h.b], writes=[p.b])
                    return p

                def mm_bcu(gd):
                    T = gd["T"]
                    bt = BT[gd["k"] % 2]
                    vt = vT[gd["k"] % 2]
                    for c in range(4):
                        p = fmm(gd, 12 + c)
                        S.op("act", lambda e: e.activation(out=bt.t[:, c, 0:T], in_=p.t[:, 0:T], func=AF.Copy),
                             reads=[p.b], writes=[bt.b])
                        p = fmm(gd, 16 + c)
                        cs = csb[c % 2]
                        S.op("act", lambda e: e.activation(out=cs.t[:, 0:T], in_=p.t[:, 0:T], func=AF.Copy),
                             reads=[p.b], writes=[cs.b])
                        p = fmm(gd, 20 + c)
                        S.op("dve", lambda e: e.tensor_tensor(out=vt.t[:, c, 1:T + 1], in0=p.t[:, 0:T], in1=cs.t[:, 0:T],
                                                              op=ALU.mult), reads=[p.b, cs.b], writes=[vt.b])

                def mm_qk(gd):
                    T, tok0, dst = gd["T"], gd["tok0"], gd["dst"]
                    h = hT[gd["k"] % 2]
                    st = qks[gd["k"] % 2]
                    for ft in range(8):
                        if ft < 4 and not gd["want_q"]:
                            continue
                        p = fmm(gd, ft)
                        if ft < 4:
                            S.op("act", lambda e: e.activation(out=st.t[:, ft, 0:T], in_=p.t[:, 0:T], func=AF.Copy,
                                                               scale=0.125), reads=[p.b], writes=[st.b])
                        else:
                            S.op("dve", lambda e: e.tensor_copy(out=st.t[:, ft, 0:T], in_=p.t[:, 0:T]),
                                 reads=[p.b], writes=[st.b])
                    if gd["want_q"]:
                        S.dma("sp", dst["Q"][:, :, tok0:tok0 + T].rearrange("c p t -> p c t"), st.t[:, 0:4, 0:T],
                              reads=[st.b], sembuf=st.b)
                    S.dma("sp", dst["K"][:, :, tok0:tok0 + T].rearrange("c p t -> p c t"), st.t[:, 4:8, 0:T],
                          reads=[st.b], sembuf=st.b)

                def mm_v(gd):
                    T, tok0, dst = gd["T"], gd["tok0"], gd["dst"]
                    h = hT[gd["k"] % 2]
                    for i in range(T // 128):
                        p = mps[cnt["mp"] % 4]
                        cnt["mp"] += 1
                        for c in range(8):
                            S.op("pe", lambda e: e.matmul(p.t[:, :], lhsT=h.t[:, c, i * 128:(i + 1) * 128],
                                                          rhs=win.t[:, c, 1024:1536], start=(c == 0), stop=(c == 7)),
                                 reads=[win_b[2], h.b], writes=[p.b])
                        v = vst[i % 2]
                        S.op("act", lambda e: e.activation(out=v.t[:], in_=p.t[:], func=AF.Copy),
                             reads=[p.b], writes=[v.b])
                        S.dma("sp", dst["V"][tok0 + i * 128:tok0 + (i + 1) * 128, :], v.t[:], reads=[v.b], sembuf=v.b)

                def conv_ew(gd):
                    T = gd["T"]
                    bt = BT[gd["k"] % 2]
                    vt = vT[gd["k"] % 2]
                    for c in range(4):
                        a = cacc[c]
                        S.op("act", lambda e: e.activation(out=a.t[:, 0:T], in_=vt.t[:, c, 1:T + 1], func=AF.Copy,
                                                           scale=cw.t[:, c * 3 + 1:c * 3 + 2]),
                             reads=[vt.b, cw.b], writes=[a.b])
                    for c in range(4):
                        a = cacc[c]
                        S.op("dve", lambda e: e.scalar_tensor_tensor(out=a.t[:, 0:T], in0=vt.t[:, c, 0:T],
                                                                     scalar=cw.t[:, c * 3:c * 3 + 1], in1=a.t[:, 0:T],
                                                                     op0=ALU.mult, op1=ALU.add),
                             reads=[vt.b, cw.b, a.b], writes=[a.b])
                        S.op("dve", lambda e: e.scalar_tensor_tensor(out=a.t[:, 0:T], in0=vt.t[:, c, 2:T + 2],
                                                                     scalar=cw.t[:, c * 3 + 2:c * 3 + 3], in1=a.t[:, 0:T],
                                                                     op0=ALU.mult, op1=ALU.add),
                             reads=[vt.b, cw.b, a.b], writes=[a.b])
                    for c in range(4):
                        a = cacc[c]
                        S.op("pool", lambda e: e.tensor_tensor(out=co.t[:, c, 0:T], in0=a.t[:, 0:T], in1=bt.t[:, c, 0:T],
                                                               op=ALU.mult), reads=[a.b, bt.b], writes=[co.b])

                def conv_sq(gd):
                    T = gd["T"]
                    for c in range(4):
                        s = sq[c]
                        S.op("act", lambda e: e.activation(out=s.t[:, 0:T], in_=co.t[:, c, 0:T], func=AF.Square),
                             reads=[co.b], writes=[s.b])

                def conv_fin(gd):
                    T, tok0 = gd["T"], gd["tok0"]
                    for c in range(4):
                        s = sq[c]
                        S.op("pe", lambda e: e.matmul(sps.t[:, 0:T], lhsT=ones.t[:], rhs=s.t[:, 0:T],
                                                      start=(c == 0), stop=(c == 3)),
                             reads=[ones.b, s.b], writes=[sps.b])
                    S.op("act", lambda e: e.activation(out=rbc.t[:, 0:T], in_=sps.t[:, 0:T], func=AF.Sqrt,
                                                       scale=1.0 / 512, bias=epsb.t[:]),
                         reads=[sps.b, epsb.b], writes=[rbc.b])
                    S.op("dve", lambda e: e.reciprocal(out=rbc.t[:, 0:T], in_=rbc.t[:, 0:T]), reads=[rbc.b], writes=[rbc.b])
                    o = cn[gd["k"] % 2]
                    for c in range(4):
                        S.op("dve", lambda e: e.scalar_tensor_tensor(out=o.t[:, c, 0:T], in0=co.t[:, c, 0:T],
                                                                     scalar=goc.t[:, c:c + 1], in1=rbc.t[:, 0:T],
                                                                     op0=ALU.mult, op1=ALU.mult),
                             reads=[co.b, goc.b, rbc.b], writes=[o.b])
                    S.dma("sp", gd["cdst"][:, :, tok0:tok0 + T].rearrange("c p t -> p c t"), o.t[:, :, 0:T],
                          reads=[o.b], sembuf=o.b)

                load_part(G[0])
                norm_part(G[0])
                trans_part(G[0])
                if len(G) > 1:
                    load_part(G[1])
                prev_lat = None
                for k, gd in enumerate(G):
                    nxt = G[k + 1] if k + 1 < len(G) else None
                    cv = None
                    if nxt is not None:
                        norm_part(nxt)
                        if k + 2 < len(G):
                            load_part(G[k + 2])
                    if gd["want_conv"]:
                        mm_bcu(gd)
                    if gd["want_conv"]:
                        vt = vT[k % 2]
                        if not gd["lat"]:
                            S.op("pool", lambda e: e.memset(vt.t[:, :, 0:1], 0.0), writes=[vt.b])
                            S.op("pool", lambda e: e.memset(vt.t[:, :, CTX + 1:CTX + 2], 0.0), writes=[vt.b])
                            cv = gd
                        elif prev_lat is None:
                            S.op("pool", lambda e: e.memset(vt.t[:, :, 0:1], 0.0), writes=[vt.b])
                        else:
                            pv = vT[prev_lat["k"] % 2]
                            S.op("pool", lambda e: e.tensor_copy(out=vt.t[:, :, 0:1], in_=pv.t[:, :, 512:513]),
                                 reads=[pv.b], writes=[vt.b])
                            S.op("pool", lambda e: e.tensor_copy(out=pv.t[:, :, 513:514], in_=vt.t[:, :, 1:2]),
                                 reads=[vt.b], writes=[pv.b])
                            cv = prev_lat
                        if cv is not None:
                            conv_ew(cv)
                    mm_qk(gd)
                    if cv is not None:
                        conv_sq(cv)
                    if nxt is not None:
                        trans_part(nxt)
                    mm_v(gd)
                    if cv is not None:
                        conv_fin(cv)
                    if gd["lat"]:
                        prev_lat = gd
                        conv_step(3)
                pv = vT[prev_lat["k"] % 2]
                S.op("pool", lambda e: e.memset(pv.t[:, :, 513:514], 0.0), writes=[pv.b])
                conv_ew(prev_lat)
                conv_sq(prev_lat)
                conv_fin(prev_lat)
                S.end_phase()

        def phase_b(l, last):
            es = ExitStack()
            with es:
                KT = alloc(es, "KT", [128, 4, S_LEN], BF16, dma=True)
                V = alloc(es, "V", [128, 32, 512], BF16, dma=True)
                KcT = alloc(es, "KcT", [128, 4, CTX], BF16, dma=True)
                Vc = alloc(es, "Vc", [128, 2, 512], BF16, dma=True)
                nbm = alloc(es, "nbm", [128, 8, 576], F32, dma=True)

                def init_loads_first():
                    S.dma("sp", KcT.t[:], KcTd.rearrange("c p t -> p c t"), writes=[KcT.b], sembuf=KcT.b)
                    S.dma("sp", Vc.t[:], Vcd.rearrange("(t p) f -> p t f", p=128), writes=[Vc.b], sembuf=Vc.b)

                KT_b = [S.buf(dma=True) for _ in range(8)]
                V_b = [S.buf(dma=True) for _ in range(4)]

                def ld_kt(bk):
                    S.dma("sp", KT.t[:, :, bk * 512:(bk + 1) * 512],
                          KTd[:, :, bk * 512:(bk + 1) * 512].rearrange("c p t -> p c t"),
                          writes=[KT_b[bk]], sembuf=KT_b[bk])

                def ld_v(q):
                    S.dma("sp", V.t[:, q * 8:(q + 1) * 8, :],
                          Vd[q * 1024:(q + 1) * 1024, :].rearrange("(t p) f -> p t f", p=128),
                          writes=[V_b[q]], sembuf=V_b[q])

                def init_loads_rest():
                    ld_kt(0)
                    ld_kt(1)
                    ld_v(0)
                    S.dma("sp", nbm.t[:], nab_mid[l], writes=[nbm.b], sembuf=nbm.b)
                    ld_kt(2)
                    ld_v(1)
                    ld_kt(3)
                    ld_kt(4)
                    ld_v(2)
                    ld_kt(5)
                    ld_kt(6)
                    ld_v(3)
                    ld_kt(7)
                wo = alloc(es, "wo", [128, 8, D], BF16, dma="pool")

                def load_wo():
                    for j in range(2):
                        S.dma("pool", wo.t[:, :, j * 512:(j + 1) * 512],
                              w_out[l, :, j * 512:(j + 1) * 512].rearrange("(c p) f -> p c f", p=128),
                              writes=[wo.b], sembuf=wo.b)
                ggt_x = load_bc(es, "ggt_x", 2)
                ggt_c = load_bc(es, "ggt_c", 6 + 2) if not last else None
                goa = load_bc(es, "goa", l, n=512, src=gout)

                NB = 3
                QA = allocn(es, 2, "QA", [128, 4, 512], BF16, dma=True)
                QB = allocn(es, 2, "QB", [128, 4, 512], BF16, dma=True)
                for qz in QA:
                    S.op("pool", lambda e: e.memset(qz.t[64:128, :, :], 0.0), writes=[qz.b])
                for qz in QB:
                    S.op("pool", lambda e: e.memset(qz.t[0:64, :, :], 0.0), writes=[qz.b])
                mT = allocn(es, 2, "mT", [128, 8, 512], BF16, dma=True)
                nbe = allocn(es, 4, "nbe", [128, 512], F32, dma=True)
                psA = allocn(es, 2, "psA", [128, 512], F32, psum=True)
                psB = allocn(es, 2, "psB", [128, 512], F32, psum=True)
                psT = allocn(es, 2, "psT", [128, 8, 128], BF16, psum=True)
                psO = alloc(es, "psO", [128, 512], F32, psum=True)
                psY = alloc(es, "psY", [128, 512], F32, psum=True)
                Ssb = allocn(es, NB, "Ssb", [128, 832], F32)
                Sctx_b = [S.buf() for _ in range(NB)]
                Psb = allocn(es, NB, "Psb", [128, 832], BF16)
                PT = allocn(es, NB, "PT", [128, 7, 128], BF16)
                nmx = allocn(es, NB, "nmx", [128, 1], F32)
                rsum = allocn(es, 3, "rsum", [128, 8], F32)
                rinv = allocn(es, 2, "rinv", [128, 8], F32)
                Osb = allocn(es, 2, "Osb", [128, 512], F32)
                On = allocn(es, 2, "On", [128, 512], BF16)
                junk = alloc(es, "junkb", [128, 512], BF16)
                ss = allocn(es, 2, "ssb", [128, 2], F32)
                ss1 = allocn(es, 2, "ss1b", [128, 1], F32)
                rstd = allocn(es, 2, "rstdb", [128, 1], F32)
                xr = allocn(es, 2, "xr", [128, D], F32, dma=True)
                ysb = allocn(es, 2, "ysb", [128, D], F32)

                def rstd_lnexp(ssv, rs_, scale):
                    S.op("act", lambda e: e.activation(out=rs_.t[:], in_=ssv.t[:], func=AF.Ln, scale=scale,
                                                       bias=epsb.t[:]), reads=[ssv.b, epsb.b], writes=[rs_.b])
                    S.op("act", lambda e: e.activation(out=rs_.t[:], in_=rs_.t[:], func=AF.Exp, scale=-0.5),
                         reads=[rs_.b], writes=[rs_.b])

                def stage_a(u):
                    i = u["i"]
                    a, b = psA[i % 2], psB[i % 2]
                    q_ap, pb, chunk = u["q"], u["pb"], u["chunk"]
                    if u["loc"] is not None:
                        tok0, nloc, tile0 = u["loc"]
                        kbs = [KT_b[bk] for bk in range(tok0 // 512, (tok0 + nloc - 1) // 512 + 1)]
                        S.op("pe", lambda e: e.matmul(a.t[:, 0:512], lhsT=q_ap,
                                                      rhs=KT.t[:, chunk, tok0:tok0 + 512], start=True, stop=True),
                             reads=[u["qb"]] + kbs, writes=[a.b])
                        if nloc > 512:
                            S.op("pe", lambda e: e.matmul(b.t[:, 0:64], lhsT=q_ap,
                                                          rhs=KT.t[:, chunk, tok0 + 512:tok0 + 576],
                                                          start=True, stop=True),
                                 reads=[u["qb"]] + kbs, writes=[b.b])
                    S.op("pe", lambda e: e.matmul(b.t[:, 64:320], lhsT=q_ap, rhs=KcT.t[:, chunk, :],
                                                  start=True, stop=True), reads=[u["qb"], KcT.b], writes=[b.b])

                def stage_b1(u):
                    i, h = u["i"], u["h"]
                    a, b = psA[i % 2], psB[i % 2]
                    s, mx = Ssb[i % NB], nmx[i % NB]
                    sc_b = Sctx_b[i % NB]
                    nloc = u["loc"][1] if u["loc"] is not None else 0
                    ntot = nloc + CTX
                    S.op("act", lambda e: e.activation(out=s.t[:, nloc:ntot], in_=b.t[:, 64:320], func=AF.Copy),
                         reads=[b.b], writes=[sc_b])
                    if u["loc"] is not None:
                        if u["edge"] is None:
                            bt, bap = nbm.b, nbm.t[:, h, :]
                        else:
                            nb = nbe[u["nbi"] % 4]
                            bt, bap = nb.b, nb.t[:, :]
                        S.op("dve", lambda e: e.tensor_tensor(out=s.t[:, 0:512], in0=a.t[:, 0:512], in1=bap[:, 0:512],
                                                              op=ALU.add), reads=[a.b, bt], writes=[s.b])
                        if nloc > 512:
                            S.op("dve", lambda e: e.tensor_tensor(out=s.t[:, 512:576], in0=b.t[:, 0:64],
                                                                  in1=bap[:, 512:576], op=ALU.add),
                                 reads=[b.b, bt], writes=[s.b])
                    S.op("dve", lambda e: e.reduce_max(out=mx.t[:], in_=s.t[:, 0:ntot], axis=AX.X, negate=True),
                         reads=[s.b, sc_b], writes=[mx.b])

                def stage_b2(u):
                    i, h = u["i"], u["h"]
                    s, pp, mx = Ssb[i % NB], Psb[i % NB], nmx[i % NB]
                    sc_b = Sctx_b[i % NB]
                    rs = rsum[u["tile"] % 3]
                    nloc = u["loc"][1] if u["loc"] is not None else 0
                    ntot = nloc + CTX
                    S.op("act", lambda e: e.activation(out=pp.t[:, 0:ntot], in_=s.t[:, 0:ntot], func=AF.Exp,
                                                       bias=mx.t[:, 0:1], accum_out=rs.t[:, h:h + 1]),
                         reads=[s.b, sc_b, mx.b], writes=[pp.b, rs.b])

                def chunks_of(u):
                    nloc = 0
                    chunks = []
                    if u["loc"] is not None:
                        tok0, nloc, tile0 = u["loc"]
                        for k in range(4):
                            chunks.append((k * 128, 128, V, tile0 + k))
                        if nloc > 512:
                            chunks.append((512, 64, V, tile0 + 4))
                    chunks.append((nloc, 128, Vc, 0))
                    chunks.append((nloc + 128, 128, Vc, 1))
                    return chunks

                def stage_c1(u):
                    i = u["i"]
                    pp = Psb[i % NB]
                    pst = psT[i % 2]
                    for j, (off, sz, vt, ti) in enumerate(chunks_of(u)):
                        S.op("pe", lambda e: e.transpose(out=pst.t[0:sz, j, :], in_=pp.t[:, off:off + sz],
                                                         identity=ident.t[:]),
                             reads=[pp.b, ident.b], writes=[pst.b])

                def stage_c1b(u):
                    i = u["i"]
                    pt = PT[i % NB]
                    pst = psT[i % 2]
                    nch = len(chunks_of(u))
                    if i % 2 == 0:
                        S.op("act", lambda e: e.activation(out=pt.t[:, 0:nch, :], in_=pst.t[:, 0:nch, :], func=AF.Copy),
                             reads=[pst.b], writes=[pt.b])
                    else:
                        S.op("dve", lambda e: e.tensor_copy(out=pt.t[:, 0:nch, :], in_=pst.t[:, 0:nch, :]),
                             reads=[pst.b], writes=[pt.b])

                def stage_c2(u):
                    i, h = u["i"], u["h"]
                    pt = PT[i % NB]
                    chunks = chunks_of(u)
                    nch = len(chunks)
                    for j, (off, sz, vt, ti) in enumerate(chunks):
                        S.op("pe", lambda e: e.matmul(psO.t[:, h * 64:(h + 1) * 64], lhsT=pt.t[0:sz, j, :],
                                                      rhs=vt.t[0:sz, ti, h * 64:(h + 1) * 64],
                                                      start=(j == 0), stop=(j == nch - 1)),
                             reads=[pt.b, (V_b[ti // 8] if vt is V else vt.b)], writes=[psO.b])

                def T1(ti, m, col0, src_rows, dst_rows, ggt, L):
                    k2 = ti % 2
                    rs = rsum[ti % 3]
                    S.dma("sp", xr[k2].t[:], src_rows, writes=[xr[k2].b], sembuf=xr[k2].b)
                    S.op("dve", lambda e: e.reciprocal(out=rinv[k2].t[:], in_=rs.t[:]),
                         reads=[rs.b], writes=[rinv[k2].b])
                    S.op("dve", lambda e: e.tensor_tensor(
                        out=Osb[k2].t[:].rearrange("p (h d) -> p h d", h=8),
                        in0=psO.t[:].rearrange("p (h d) -> p h d", h=8),
                        in1=rinv[k2].t[:].unsqueeze(2).to_broadcast([128, 8, 64]), op=ALU.mult),
                        reads=[psO.b, rinv[k2].b], writes=[Osb[k2].b])

                def T2(ti, m, col0, src_rows, dst_rows, ggt, L):
                    k2 = ti % 2
                    S.op("act", lambda e: e.activation(out=junk.t[:], in_=Osb[k2].t[:], func=AF.Square,
                                                       accum_out=ss1[k2].t[:]),
                         reads=[Osb[k2].b], writes=[junk.b, ss1[k2].b])
                    rstd_lnexp(ss1[k2], rstd[k2], 1.0 / 512)

                def T3(ti, m, col0, src_rows, dst_rows, ggt, L):
                    k2 = ti % 2
                    S.op("dve", lambda e: e.scalar_tensor_tensor(out=On[k2].t[:], in0=Osb[k2].t[:],
                                                                 scalar=rstd[k2].t[:, 0:1], in1=goa.t[:],
                                                                 op0=ALU.mult, op1=ALU.mult),
                         reads=[Osb[k2].b, rstd[k2].b, goa.b], writes=[On[k2].b])

                def T4(ti, m, col0, src_rows, dst_rows, ggt, L):
                    k2 = ti % 2
                    pst = psT[(L + 7) % 2]
                    for c in range(4):
                        S.op("pe", lambda e: e.transpose(out=pst.t[:, c, :], in_=On[k2].t[:, c * 128:(c + 1) * 128],
                                                         identity=ident.t[:]),
                             reads=[On[k2].b, ident.b], writes=[pst.b])

                def T5(ti, m, col0, src_rows, dst_rows, ggt, L):
                    pst = psT[(L + 7) % 2]
                    S.op("act", lambda e: e.activation(out=m.t[:, 0:4, col0:col0 + 128], in_=pst.t[:, 0:4, :], func=AF.Copy),
                         reads=[pst.b], writes=[m.b])

                def T6(hf, ti, m, col0, src_rows, dst_rows, ggt, L):
                    for k in range(8):
                        S.op("pe", lambda e: e.matmul(psY.t[:], lhsT=m.t[:, k, col0:col0 + 128],
                                                      rhs=wo.t[:, k, hf * 512:(hf + 1) * 512],
                                                      start=(k == 0), stop=(k == 7)),
                             reads=[m.b, wo.b], writes=[psY.b])

                def T7(hf, ti, m, col0, src_rows, dst_rows, ggt, L):
                    k2 = ti % 2
                    S.op("act", lambda e: e.activation(out=ysb[k2].t[:, hf * 512:(hf + 1) * 512], in_=psY.t[:],
                                                       func=AF.Copy), reads=[psY.b], writes=[ysb[k2].b])
                    S.op("act", lambda e: e.activation(out=junk.t[:], in_=ysb[k2].t[:, hf * 512:(hf + 1) * 512],
                                                       func=AF.Square, accum_out=ss[k2].t[:, hf:hf + 1]),
                         reads=[ysb[k2].b], writes=[junk.b, ss[k2].b])

                def T10(ti, m, col0, src_rows, dst_rows, ggt, L):
                    k2 = ti % 2
                    S.op("dve", lambda e: e.tensor_tensor(out=ss1[k2].t[:], in0=ss[k2].t[:, 0:1], in1=ss[k2].t[:, 1:2],
                                                          op=ALU.add), reads=[ss[k2].b], writes=[ss1[k2].b])

                def T11(ti, m, col0, src_rows, dst_rows, ggt, L):
                    k2 = ti % 2
                    rstd_lnexp(ss1[k2], rstd[k2], 1.0 / D)

                def T12(ti, m, col0, src_rows, dst_rows, ggt, L):
                    k2 = ti % 2
                    S.op("dve", lambda e: e.scalar_tensor_tensor(
                        out=ysb[k2].t[:], in0=ysb[k2].t[:], scalar=rstd[k2].t[:, 0:1],
                        in1=ggt.t[:], op0=ALU.mult, op1=ALU.mult),
                        reads=[ysb[k2].b, rstd[k2].b, ggt.b], writes=[ysb[k2].b])
                    S.op("pool", lambda e: e.tensor_tensor(out=xr[k2].t[:], in0=ysb[k2].t[:], in1=xr[k2].t[:], op=ALU.add),
                         reads=[ysb[k2].b, xr[k2].b], writes=[xr[k2].b])
                    S.dma("sp", dst_rows, xr[k2].t[:], reads=[xr[k2].b], sembuf=xr[k2].b)

                units = []
                tails = {}
                groups = []
                tile_no = 0
                nbi = 0
                if not last:
                    csrc = ctx_in if l == 0 else ctxs
                    qa, qb_, m = QA[0], QB[0], mT[0]
                    groups.append((len(units),
                                   [(qa, lambda qa=qa: qa.t[0:64, :, 0:CTX], QcTd[:, 0:64, :].rearrange("c p t -> p c t")),
                                    (qb_, lambda qb_=qb_: qb_.t[64:128, :, 0:CTX], QcTd[:, 64:128, :].rearrange("c p t -> p c t"))],
                                   (m, lambda m=m: m.t[:, 4:8, 0:CTX], cncTd.rearrange("c p t -> p c t"))))
                    for ti in range(2):
                        for h in range(8):
                            pb = (h % 2) * 64
                            q = qa if h % 2 == 0 else qb_
                            units.append(dict(q=q.t[:, h // 2, ti * 128:(ti + 1) * 128], qb=q.b, pb=pb, chunk=h // 2,
                                              h=h, loc=None, edge=None, tile=tile_no))
                        tails[len(units) - 1] = (tile_no, m, ti * 128, csrc[ti * 128:(ti + 1) * 128, :],
                                                 ctxs[ti * 128:(ti + 1) * 128, :], ggt_c)
                        tile_no += 1
                xsrc = x_in if l == 0 else y_out
                for g in range(8):
                    qa, qb_, m = QA[(g + 1) % 2], QB[(g + 1) % 2], mT[(g + 1) % 2]
                    groups.append((len(units),
                                   [(qa, lambda qa=qa: qa.t[0:64, :, :],
                                     QTd[:, 0:64, g * 512:(g + 1) * 512].rearrange("c p t -> p c t")),
                                    (qb_, lambda qb_=qb_: qb_.t[64:128, :, :],
                                     QTd[:, 64:128, g * 512:(g + 1) * 512].rearrange("c p t -> p c t"))],
                                   (m, lambda m=m: m.t[:, 4:8, :], cnTd[:, :, g * 512:(g + 1) * 512].rearrange("c p t -> p c t"))))
                    for pi in range(4):
                        p = g * 4 + pi
                        if 2 <= p <= 29:
                            bs, nloc, edge = 2 * p - 4, 576, None
                        elif p < 2:
                            bs, nloc, edge = 0, 512, p
                        else:
                            bs, nloc, edge = 56, 512, p - 28
                        for h in range(8):
                            pb = (h % 2) * 64
                            q = qa if h % 2 == 0 else qb_
                            u = dict(q=q.t[:, h // 2, pi * 128:(pi + 1) * 128], qb=q.b, pb=pb, chunk=h // 2, h=h,
                                     loc=(bs * 64, nloc, bs // 2), edge=edge, tile=tile_no)
                            if edge is not None:
                                u["nbi"] = nbi
                                nbi += 1
                            units.append(u)
                        tails[len(units) - 1] = (tile_no, m, pi * 128, xsrc[p * 128:(p + 1) * 128, :],
                                                 y_out[p * 128:(p + 1) * 128, :], ggt_x)
                        tile_no += 1
                for i, u in enumerate(units):
                    u["i"] = i
                NU = len(units)
                pre = {}

                def at(step, fn):
                    pre.setdefault(max(step, 0), []).append(fn)

                def mk_load(spec):
                    tt, dst_fn, src_ap = spec
                    return lambda: S.dma("sp", dst_fn(), src_ap, writes=[tt.b], sembuf=tt.b)

                for gi_, (first, qspecs, mspec) in enumerate(groups):
                    if gi_ == 0:
                        for qs in qspecs:
                            at(0, mk_load(qs))
                        at(0, mk_load(mspec))
                    else:
                        pf = groups[gi_ - 1][0]
                        for qs in qspecs:
                            at(pf, mk_load(qs))
                        at(pf + 12, mk_load(mspec))
                for u in units:
                    if u["edge"] is not None:
                        def ld(u=u):
                            nb = nbe[u["nbi"] % 4]
                            S.dma("sp", nb.t[:], nab_edge[l, u["edge"], u["h"]], writes=[nb.b], sembuf=nb.b)
                        at(u["i"] - 2, ld)
                post = {}
                pre2 = {}
                for L, targs in tails.items():
                    A_ = lambda d, s, f, *x: d.setdefault(s, []).append(lambda f=f, x=x, targs=targs, L=L: f(*x, *targs, L))
                    A_(post, L + 4, T1)
                    A_(post, L + 5, T2)
                    A_(post, L + 6, T3)
                    A_(post, L + 7, T4)
                    A_(pre2, L + 8, T5)
                    A_(post, L + 8, T6, 0)
                    A_(pre2, L + 9, T7, 0)
                    A_(post, L + 9, T6, 1)
                    A_(pre2, L + 10, T7, 1)
                    A_(post, L + 10, T10)
                    A_(post, L + 11, T11)
                    A_(post, L + 12, T12)
                    post.setdefault(L + 12, []).append(lambda: conv_step(2))
                init_loads_first()
                for st in range(NU + 13):
                    for fn in pre.get(st, []):
                        fn()
                    if st == 0:
                        init_loads_rest()
                    if st == 2:
                        load_wo()
                    for fn in pre2.get(st, []):
                        fn()
                    if 0 <= st - 4 < NU:
                        stage_c1b(units[st - 4])
                    if st < NU:
                        stage_a(units[st])
                    if 0 <= st - 1 < NU:
                        stage_b1(units[st - 1])
                    if 0 <= st - 2 < NU:
                        stage_b2(units[st - 2])
                    if 0 <= st - 3 < NU:
                        stage_c1(units[st - 3])
                    if 0 <= st - 4 < NU:
                        stage_c2(units[st - 4])
                    for fn in post.get(st, []):
                        fn()
                S.end_phase()

        def phase_f(l, last):
            es = ExitStack()
            with es:
                gsT = alloc(es, "fgs", [128, D], F32, dma=True)
                shT = alloc(es, "fsh", [128, D], F32, dma=True)
                ggt_x = load_bc(es, "fggt_x", 5)
                ggt_c = load_bc(es, "fggt_c", 6 + 5) if not last else None

                def load_rows(base):
                    S.dma("sp", gsT.t[:], _bc_rows(mods, base + 4, D), writes=[gsT.b], sembuf=gsT.b)
                    S.dma("sp", shT.t[:], _bc_rows(mods, base + 3, D), writes=[shT.b], sembuf=shT.b)

                tiles = []
                if not last:
                    for t in range(CTX // 128):
                        tiles.append(dict(rows=ctxs[t * 128:(t + 1) * 128, :], ctx=True))
                for p in range(S_LEN // 128):
                    tiles.append(dict(rows=y_out[p * 128:(p + 1) * 128, :], ctx=False))
                ngr = 4
                base_n, extra = len(tiles) // ngr, len(tiles) % ngr
                groups = []
                pos = 0
                for k in range(ngr):
                    n = base_n + (1 if k < extra else 0)
                    groups.append(dict(k=k, tiles=tiles[pos:pos + n]))
                    pos += n
                NTM = max(len(g["tiles"]) for g in groups)
                TM = NTM * 128
                for g in groups:
                    nt = len(g["tiles"])
                    g["nt"] = nt
                    g["splits"] = [(0, 512), (512, 512)] if nt == 8 else [(i * 384, 384) for i in range(nt * 128 // 384)]
                    assert sum(w for _, w in g["splits"]) == nt * 128
                    bl = []
                    for i, td in enumerate(g["tiles"]):
                        if bl and len(bl[-1]) < 4 and g["tiles"][bl[-1][0]]["ctx"] == td["ctx"]:
                            bl[-1].append(i)
                        else:
                            bl.append([i])
                    g["batches"] = bl

                xts = allocn(es, 4, "fxt", [128, D], F32, dma=True)
                junk = alloc(es, "fjunk", [128, D], BF16)
                tmp = allocn(es, 2, "ftmp", [128, D], F32)
                hb = allocn(es, 4, "fhb", [128, D], BF16)
                ss4 = allocn(es, 2, "fss4", [128, 4], F32)
                rs4 = allocn(es, 2, "frs4", [128, 4], F32)
                sse = alloc(es, "fsse", [128, NTM], F32)
                rse = alloc(es, "frse", [128, NTM], F32)
                hT = allocn(es, 2, "fhT", [128, 8, TM], BF16)
                tps = allocn(es, 2, "ftp", [128, 8, 128], BF16, psum=True)
                psG = allocn(es, 2, "psG", [128, 512], F32, psum=True)
                psU = allocn(es, 2, "psU", [128, 512], F32, psum=True)
                psD = allocn(es, 2, "psD", [128, 512], F32, psum=True)
                wg = allocn(es, 2, "wg", [128, 8, 512], BF16, dma="pool")
                wu = allocn(es, 2, "wu", [128, 8, 512], BF16, dma="pool")
                wd = allocn(es, 2, "wd", [128, 4, D], BF16, dma="pool")
                sg = allocn(es, 2, "sg", [128, 512], F32)
                aT = allocn(es, 2, "aT", [128, 4, TM], BF16)
                acc = alloc(es, "facc", [128, NTM, D], F32)
                cnt = dict(x=0, h=0, b=0, w=0, gu=0, d=0, a=0, t=0)
                cur_kind = [None]

                def load_batch(g, bi):
                    xl = []
                    for i in g["batches"][bi]:
                        xt = xts[cnt["x"] % 4]
                        cnt["x"] += 1
                        xl.append(xt)
                        S.dma("sp", xt.t[:], g["tiles"][i]["rows"], writes=[xt.b], sembuf=xt.b)
                    g.setdefault("xl", {})[bi] = xl

                def norm_batch(g, bi):
                    idx = g["batches"][bi]
                    kind = g["tiles"][idx[0]]["ctx"]
                    if cur_kind[0] != kind:
                        load_rows(6 if kind else 0)
                        cur_kind[0] = kind
                    kb = cnt["b"] % 2
                    cnt["b"] += 1
                    ssb, rsb = ss4[kb], rs4[kb]
                    xl = g["xl"][bi]
                    for j, i in enumerate(idx):
                        xt = xl[j]
                        S.op("act", lambda e: e.activation(out=junk.t[:], in_=xt.t[:], func=AF.Square,
                                                           accum_out=ssb.t[:, j:j + 1]),
                             reads=[xt.b], writes=[junk.b, ssb.b])
                    nbt = len(idx)
                    S.op("act", lambda e: e.activation(out=rsb.t[:, 0:nbt], in_=ssb.t[:, 0:nbt], func=AF.Sqrt,
                                                       scale=1.0 / D, bias=epsb.t[:]),
                         reads=[ssb.b, epsb.b], writes=[rsb.b])
                    S.op("dve", lambda e: e.reciprocal(out=rsb.t[:, 0:nbt], in_=rsb.t[:, 0:nbt]),
                         reads=[rsb.b], writes=[rsb.b])
                    hl = []
                    for j, i in enumerate(idx):
                        xt = xl[j]
                        tm = tmp[j % 2]
                        h_ = hb[cnt["h"] % 4]
                        cnt["h"] += 1
                        hl.append((i, h_))
                        S.op("dve", lambda e: e.scalar_tensor_tensor(out=tm.t[:], in0=xt.t[:], scalar=rsb.t[:, j:j + 1],
                                                                     in1=gsT.t[:], op0=ALU.mult, op1=ALU.mult),
                             reads=[xt.b, rsb.b, gsT.b], writes=[tm.b])
                        S.op("pool", lambda e: e.tensor_tensor(out=h_.t[:], in0=tm.t[:], in1=shT.t[:], op=ALU.add),
                             reads=[tm.b, shT.b], writes=[h_.b])
                    g.setdefault("hl", {})[bi] = hl

                def trans_batch(g, bi):
                    h = hT[g["k"] % 2]
                    for (i, h_) in g["hl"][bi]:
                        tp = tps[cnt["t"] % 2]
                        cnt["t"] += 1
                        for c in range(8):
                            S.op("pe", lambda e: e.transpose(out=tp.t[:, c, :], in_=h_.t[:, c * 128:(c + 1) * 128],
                                                             identity=ident.t[:]),
                                 reads=[h_.b, ident.b], writes=[tp.b])
                        S.op("act", lambda e: e.activation(out=h.t[:, :, i * 128:(i + 1) * 128], in_=tp.t[:],
                                                           func=AF.Copy), reads=[tp.b], writes=[h.b])

                def epilogue(g):
                    nt = g["nt"]
                    for i in range(nt):
                        S.op("act", lambda e: e.activation(out=junk.t[:], in_=acc.t[:, i, :], func=AF.Square,
                                                           accum_out=sse.t[:, i:i + 1]),
                             reads=[acc.b], writes=[junk.b, sse.b])
                    S.op("act", lambda e: e.activation(out=rse.t[:, 0:nt], in_=sse.t[:, 0:nt], func=AF.Sqrt,
                                                       scale=1.0 / D, bias=epsb.t[:]),
                         reads=[sse.b, epsb.b], writes=[rse.b])
                    S.op("dve", lambda e: e.reciprocal(out=rse.t[:, 0:nt], in_=rse.t[:, 0:nt]),
                         reads=[rse.b], writes=[rse.b])

                    def issue(i):
                        xt = xts[cnt["x"] % 4]
                        cnt["x"] += 1
                        S.dma("sp", xt.t[:], g["tiles"][i]["rows"], writes=[xt.b], sembuf=xt.b)
                        return xt
                    q = [issue(i) for i in range(min(3, nt))]
                    for i in range(nt):
                        xt = q.pop(0)
                        tm = tmp[i % 2]
                        ggt = ggt_c if g["tiles"][i]["ctx"] else ggt_x
                        S.op("dve", lambda e: e.scalar_tensor_tensor(out=tm.t[:], in0=acc.t[:, i, :],
                                                                     scalar=rse.t[:, i:i + 1], in1=ggt.t[:],
                                                                     op0=ALU.mult, op1=ALU.mult),
                             reads=[acc.b, rse.b, ggt.b], writes=[tm.b])
                        S.op("pool", lambda e: e.tensor_tensor(out=xt.t[:], in0=tm.t[:], in1=xt.t[:], op=ALU.add),
                             reads=[tm.b, xt.b], writes=[xt.b])
                        S.dma("sp", g["tiles"][i]["rows"], xt.t[:], reads=[xt.b], sembuf=xt.b)
                        if i + 3 < nt:
                            q.append(issue(i + 3))

                wgu, wdn = w_gu_d[l // 2], w_dn_d[l // 2]

                def wload(c):
                    kw = cnt["w"] % 2
                    cnt["w"] += 1
                    S.dma("pool", wg[kw].t[:], wgu[:, c * 512:(c + 1) * 512].rearrange("(c p) f -> p c f", p=128),
                          writes=[wg[kw].b], sembuf=wg[kw].b)
                    S.dma("pool", wu[kw].t[:],
                          wgu[:, FF + c * 512:FF + (c + 1) * 512].rearrange("(c p) f -> p c f", p=128),
                          writes=[wu[kw].b], sembuf=wu[kw].b)
                    S.dma("pool", wd[kw].t[:], wdn[c * 512:(c + 1) * 512, :].rearrange("(j p) d -> p j d", p=128),
                          writes=[wd[kw].b], sembuf=wd[kw].b)
                    return kw

                def gu_block(g, kw, a, j, c0, cwd):
                    h = hT[g["k"] % 2]
                    kk = cnt["gu"] % 2
                    cnt["gu"] += 1
                    pg, pu, sgt = psG[kk], psU[kk], sg[kk]
                    for ch in range(8):
                        S.op("pe", lambda e: e.matmul(pg.t[:, 0:cwd], lhsT=wg[kw].t[:, ch, j * 128:(j + 1) * 128],
                                                      rhs=h.t[:, ch, c0:c0 + cwd],
                                                      start=(ch == 0), stop=(ch == 7)),
                             reads=[wg[kw].b, h.b], writes=[pg.b])
                    for ch in range(8):
                        S.op("pe", lambda e: e.matmul(pu.t[:, 0:cwd], lhsT=wu[kw].t[:, ch, j * 128:(j + 1) * 128],
                                                      rhs=h.t[:, ch, c0:c0 + cwd],
                                                      start=(ch == 0), stop=(ch == 7)),
                             reads=[wu[kw].b, h.b], writes=[pu.b])
                    S.op("act", lambda e: e.activation(out=sgt.t[:, 0:cwd], in_=pg.t[:, 0:cwd], func=AF.Silu),
                         reads=[pg.b], writes=[sgt.b])
                    S.op("dve", lambda e: e.tensor_tensor(out=a.t[:, j, c0:c0 + cwd],
                                                          in0=pu.t[:, 0:cwd], in1=sgt.t[:, 0:cwd], op=ALU.mult),
                         reads=[pu.b, sgt.b], writes=[a.b])

                def down(g, c, kw, a):
                    for i in range(g["nt"]):
                        for dh in range(2):
                            pd = psD[cnt["d"] % 2]
                            cnt["d"] += 1
                            for j in range(4):
                                S.op("pe", lambda e: e.matmul(pd.t[:], lhsT=a.t[:, j, i * 128:(i + 1) * 128],
                                                              rhs=wd[kw].t[:, j, dh * 512:(dh + 1) * 512],
                                                              start=(j == 0), stop=(j == 3)),
                                     reads=[a.b, wd[kw].b], writes=[pd.b])
                            av = acc.t[:, i, dh * 512:(dh + 1) * 512]
                            if c == 0:
                                S.op("dve", lambda e: e.tensor_copy(out=av, in_=pd.t[:]), reads=[pd.b], writes=[acc.b])
                            else:
                                S.op("dve", lambda e: e.tensor_tensor(out=av, in0=pd.t[:], in1=av, op=ALU.add),
                                     reads=[pd.b, acc.b], writes=[acc.b])

                NCH = 7 * len(groups)
                kw_of = {0: wload(0), 1: wload(1)}
                g0 = groups[0]
                for bi in range(len(g0["batches"])):
                    load_batch(g0, bi)
                    norm_batch(g0, bi)
                    trans_batch(g0, bi)
                pend_d = None
                for k, g in enumerate(groups):
                    nxt = groups[k + 1] if k + 1 < len(groups) else None
                    nbn = len(nxt["batches"]) if nxt is not None else 0
                    for c in range(7):
                        t = 7 * k + c
                        kw = kw_of[t]
                        a = aT[cnt["a"] % 2]
                        cnt["a"] += 1
                        blocks = [(j, c0, cwd) for j in range(4) for (c0, cwd) in g["splits"]]
                        gu_block(g, kw, a, *blocks[0])
                        if pend_d is not None:
                            down(*pend_d)
                            if t + 1 < NCH:
                                kw_of[t + 1] = wload((t + 1) % 7)
                        conv_step(1)
                        if c == 0 and k > 0:
                            epilogue(groups[k - 1])
                        if 0 <= c - 2 < nbn:
                            trans_batch(nxt, c - 2)
                        for blk in blocks[1:]:
                            gu_block(g, kw, a, *blk)
                        if 0 <= c - 1 < nbn:
                            norm_batch(nxt, c - 1)
                        if c < nbn:
                            load_batch(nxt, c)
                        pend_d = (g, c, kw, a)
                down(*pend_d)
                epilogue(groups[-1])
                S.end_phase()


        def phase_moe(l):
            es0 = ExitStack()
            with es0:
                slot = alloc(es0, "slot", [128, 32, 2], U32)
                gates = alloc(es0, "gates", [128, 32, 2], F32)
                idxw = alloc(es0, "idxw", [128, NSEG * 7], U32)
                es = ExitStack()
                with es:
                    gs_x = load_bc(es, "mgs_x", 4)
                    sh_x = load_bc(es, "msh_x", 3)
                    hb_all = alloc(es, "hb_all", [128, 32, D], BF16)
                    xts = allocn(es, 4, "mxt", [128, D], F32, dma=True)
                    junk = alloc(es, "mjunk", [128, D], BF16)
                    tmp = allocn(es, 2, "mtmp", [128, D], F32)
                    hb32 = allocn(es, 3, "mhb32", [128, D], F32)
                    sss = allocn(es, 2, "mss", [128, 1], F32)
                    rstds = allocn(es, 2, "mrstd", [128, 1], F32)
                    tp32 = allocn(es, 2, "mtp32", [128, 4, 128], F32, psum=True)
                    pl = allocn(es, 2, "mpl", [128, 512], F32, psum=True)
                    hT32 = allocn(es, 2, "mhT32", [128, 8, 128], F32)
                    wr = alloc(es, "mwr", [128, 8, NE], F32, dma=True)
                    S.dma("sp", wr.t[:], w_rt, writes=[wr.b], sembuf=wr.b)
                    M1 = alloc(es, "M1", [128, 32, NE], F32)
                    M2 = alloc(es, "M2", [128, 32, NE], F32)
                    M12 = allocn(es, 2, "M12", [128, NE], F32)
                    rt = alloc(es, "rt", [128, 32, 2 * NE], F32)
                    cum = alloc(es, "cum", [128, 32, NE], F32)
                    rank = alloc(es, "rank", [128, 32, NE], F32)
                    lg = allocn(es, 2, "lg", [128, NE], F32)
                    l2 = allocn(es, 2, "l2", [128, NE], F32)
                    m1 = allocn(es, 2, "m1", [128, 1], F32)
                    m2 = allocn(es, 2, "m2", [128, 1], F32)
                    dd = alloc(es, "dd", [128, 32], F32)
                    scb = S.buf(dma="pool")
                    V_ = lambda fn, r, w: S.op("dve", fn, reads=r, writes=w)
                    G_ = lambda fn, r, w: S.op("pool", fn, reads=r, writes=w)
                    NT = 32

                    def r_load(i):
                        xt = xts[i % 4]
                        S.dma("sp", xt.t[:], y_out[i * 128:(i + 1) * 128, :], writes=[xt.b], sembuf=xt.b)

                    def r_norm(i):
                        k2 = i % 2
                        xt = xts[i % 4]
                        h32 = hb32[i % 3]
                        S.op("act", lambda e: e.activation(out=junk.t[:], in_=xt.t[:], func=AF.Square,
                                                           accum_out=sss[k2].t[:]),
                             reads=[xt.b], writes=[junk.b, sss[k2].b])
                        rstd_from_ss(sss[k2], rstds[k2], 1.0 / D)
                        V_(lambda e: e.scalar_tensor_tensor(out=tmp[k2].t[:], in0=xt.t[:], scalar=rstds[k2].t[:, 0:1],
                                                            in1=gs_x.t[:], op0=ALU.mult, op1=ALU.mult),
                           [xt.b, rstds[k2].b, gs_x.b], [tmp[k2].b])
                        G_(lambda e: e.tensor_tensor(out=h32.t[:], in0=tmp[k2].t[:], in1=sh_x.t[:], op=ALU.add),
                           [tmp[k2].b, sh_x.b], [h32.b])
                        S.op("act", lambda e: e.activation(out=hb_all.t[:, i, :], in_=h32.t[:], func=AF.Copy),
                             reads=[h32.b], writes=[hb_all.b])

                    def r_trans(i):
                        k2 = i % 2
                        h32 = hb32[i % 3]
                        for half in range(2):
                            tpp = tp32[half]
                            for c in range(4):
                                cc = half * 4 + c
                                S.op("pe", lambda e: e.transpose(out=tpp.t[:, c, :], in_=h32.t[:, cc * 128:(cc + 1) * 128],
                                                                 identity=ident32.t[:]),
                                     reads=[h32.b, ident32.b], writes=[tpp.b])
                            S.op("act", lambda e: e.activation(out=hT32[k2].t[:, half * 4:half * 4 + 4, :], in_=tpp.t[:],
                                                               func=AF.Copy), reads=[tpp.b], writes=[hT32[k2].b])

                    def r_route(i):
                        k2 = i % 2
                        p = pl[k2]
                        for c in range(8):
                            S.op("pe", lambda e: e.matmul(p.t[:, 0:NE], lhsT=hT32[k2].t[:, c, :], rhs=wr.t[:, c, :],
                                                          start=(c == 0), stop=(c == 7)),
                                 reads=[hT32[k2].b, wr.b], writes=[p.b])
                        lgk, l2k, m1k, m2k = lg[k2], l2[k2], m1[k2], m2[k2]
                        V_(lambda e: e.tensor_copy(out=lgk.t[:], in_=p.t[:, 0:NE]), [p.b], [lgk.b])
                        V_(lambda e: e.reduce_max(out=m1k.t[:], in_=lgk.t[:], axis=AX.X), [lgk.b], [m1k.b])
                        V_(lambda e: e.tensor_scalar(out=M1.t[:, i, :], in0=lgk.t[:], scalar1=m1k.t[:, 0:1], scalar2=None,
                                                     op0=ALU.is_ge), [lgk.b, m1k.b], [M1.b])
                        V_(lambda e: e.scalar_tensor_tensor(out=l2k.t[:], in0=M1.t[:, i, :], scalar=-1e30, in1=lgk.t[:],
                                                            op0=ALU.mult, op1=ALU.add), [M1.b, lgk.b], [l2k.b])
                        V_(lambda e: e.reduce_max(out=m2k.t[:], in_=l2k.t[:], axis=AX.X), [l2k.b], [m2k.b])
                        V_(lambda e: e.tensor_scalar(out=M2.t[:, i, :], in0=l2k.t[:], scalar1=m2k.t[:, 0:1], scalar2=None,
                                                     op0=ALU.is_ge), [l2k.b, m2k.b], [M2.b])
                        V_(lambda e: e.tensor_tensor(out=dd.t[:, i:i + 1], in0=m1k.t[:], in1=m2k.t[:], op=ALU.subtract),
                           [m1k.b, m2k.b], [dd.b])
                        mk = M12[k2]
                        V_(lambda e: e.tensor_tensor(out=mk.t[:], in0=M1.t[:, i, :], in1=M2.t[:, i, :], op=ALU.add),
                           [M1.b, M2.b], [mk.b])
                        S.op("pe", lambda e: e.matmul(p.t[:, 64:64 + NE], lhsT=tri32.t[:], rhs=mk.t[:], start=True, stop=True),
                             reads=[tri32.b, mk.b], writes=[p.b])
                        S.op("pe", lambda e: e.matmul(p.t[:, 64 + NE:64 + 2 * NE], lhsT=ones32.t[:], rhs=mk.t[:],
                                                      start=True, stop=True),
                             reads=[ones32.b, mk.b], writes=[p.b])
                        V_(lambda e: e.tensor_copy(out=rt.t[:, i, :], in_=p.t[:, 64:64 + 2 * NE]), [p.b], [rt.b])

                    for i in range(min(3, NT)):
                        r_load(i)
                    for st in range(NT + 2):
                        if st + 3 < NT:
                            r_load(st + 3)
                        if st < NT:
                            r_norm(st)
                        if 0 <= st - 1 < NT:
                            r_trans(st - 1)
                        if 0 <= st - 2 < NT:
                            r_route(st - 2)
                    S.op("act", lambda e: e.activation(out=gates.t[:, :, 0], in_=dd.t[:, :], func=AF.Sigmoid),
                         reads=[dd.b], writes=[gates.b])
                    V_(lambda e: e.tensor_scalar(out=gates.t[:, :, 1], in0=gates.t[:, :, 0], scalar1=-1.0, scalar2=1.0,
                                                 op0=ALU.mult, op1=ALU.add), [gates.b], [gates.b])
                    G_(lambda e: e.memset(cum.t[:, 0, :], 0.0), [], [cum.b])
                    for i in range(1, NT):
                        V_(lambda e: e.tensor_tensor(out=cum.t[:, i, :], in0=cum.t[:, i - 1, :], in1=rt.t[:, i - 1, NE:2 * NE],
                                                     op=ALU.add), [cum.b, rt.b], [cum.b])
                    V_(lambda e: e.tensor_tensor(out=rank.t[:], in0=rt.t[:, :, 0:NE], in1=cum.t[:], op=ALU.add),
                       [rt.b, cum.b], [rank.b])
                    nb = alloc(es, "nb", [128, NE], F32)
                    pad = alloc(es, "pad", [128, NE], F32)
                    pend = alloc(es, "pend", [128, NE], F32)
                    off = alloc(es, "off", [128, NE], F32)
                    V_(lambda e: e.tensor_tensor(out=nb.t[:], in0=cum.t[:, NT - 1, :], in1=rt.t[:, NT - 1, NE:2 * NE],
                                                 op=ALU.add), [cum.b, rt.b], [nb.b])
                    cmp0 = alloc(es, "cmp0", [128, NE], F32)
                    V_(lambda e: e.tensor_single_scalar(out=pad.t[:], in_=nb.t[:], scalar=0.0, op=ALU.is_gt), [nb.b], [pad.b])
                    for kq in range(1, 8):
                        V_(lambda e: e.tensor_single_scalar(out=cmp0.t[:], in_=nb.t[:], scalar=float(512 * kq), op=ALU.is_gt),
                           [nb.b], [cmp0.b])
                        V_(lambda e: e.tensor_tensor(out=pad.t[:], in0=pad.t[:], in1=cmp0.t[:], op=ALU.add),
                           [pad.b, cmp0.b], [pad.b])
                    V_(lambda e: e.tensor_single_scalar(out=pad.t[:], in_=pad.t[:], scalar=512.0, op=ALU.mult), [pad.b], [pad.b])
                    V_(lambda e: e.tensor_copy(out=pend.t[:, 0:1], in_=pad.t[:, 0:1]), [pad.b], [pend.b])
                    for ex in range(1, NE):
                        V_(lambda e: e.tensor_tensor(out=pend.t[:, ex:ex + 1], in0=pend.t[:, ex - 1:ex], in1=pad.t[:, ex:ex + 1],
                                                     op=ALU.add), [pend.b, pad.b], [pend.b])
                    V_(lambda e: e.tensor_tensor(out=off.t[:], in0=pend.t[:], in1=pad.t[:], op=ALU.subtract),
                       [pend.b, pad.b], [off.b])
                    V_(lambda e: e.tensor_tensor(out=rank.t[:], in0=rank.t[:],
                                                 in1=off.t[:].unsqueeze(1).to_broadcast([128, 32, NE]), op=ALU.add),
                       [rank.b, off.b], [rank.b])
                    prod = alloc(es, "prod", [128, 32, NE], F32)
                    slotf = alloc(es, "slotf", [128, 32, 2], F32)
                    for k, Mk in enumerate((M1, M2)):
                        V_(lambda e: e.tensor_tensor(out=prod.t[:], in0=rank.t[:], in1=Mk.t[:], op=ALU.mult),
                           [rank.b, Mk.b], [prod.b])
                        V_(lambda e: e.reduce_sum(out=slotf.t[:, :, k], in_=prod.t[:], axis=AX.X), [prod.b], [slotf.b])
                    V_(lambda e: e.tensor_copy(out=slot.t[:], in_=slotf.t[:]), [slotf.b], [slot.b])
                    eseg = alloc(es, "eseg", [128, NSEG], F32)
                    cmpt = alloc(es, "cmpt", [128, NE], F32)
                    pci = alloc(es, "pci", [128, 7], I32)
                    pcf = alloc(es, "pcf", [128, 7], F32)
                    idxf = alloc(es, "idxf", [128, NSEG, 7], F32)
                    for sg_ in range(NSEG):
                        V_(lambda e: e.tensor_single_scalar(out=cmpt.t[:], in_=pend.t[:], scalar=float(512 * sg_), op=ALU.is_le),
                           [pend.b], [cmpt.b])
                        V_(lambda e: e.reduce_sum(out=eseg.t[:, sg_:sg_ + 1], in_=cmpt.t[:], axis=AX.X), [cmpt.b], [eseg.b])
                    V_(lambda e: e.tensor_scalar(out=eseg.t[:], in0=eseg.t[:], scalar1=float(NE - 1), scalar2=896.0,
                                                 op0=ALU.min, op1=ALU.mult), [eseg.b], [eseg.b])
                    G_(lambda e: e.iota(pci.t[:], pattern=[[128, 7]], base=0, channel_multiplier=1), [], [pci.b])
                    V_(lambda e: e.tensor_copy(out=pcf.t[:], in_=pci.t[:]), [pci.b], [pcf.b])
                    for sg_ in range(NSEG):
                        V_(lambda e: e.tensor_scalar(out=idxf.t[:, sg_, :], in0=pcf.t[:], scalar1=eseg.t[:, sg_:sg_ + 1],
                                                     scalar2=None, op0=ALU.add), [pcf.b, eseg.b], [idxf.b])
                    V_(lambda e: e.tensor_copy(out=idxw.t[:], in_=idxf.t[:].rearrange("p s c -> p (s c)")), [idxf.b], [idxw.b])
                    for i in range(32):
                        for k in range(2):
                            S.dma_fn("pool", lambda e: e.indirect_dma_start(
                                out=hs, out_offset=bass.IndirectOffsetOnAxis(ap=slot.t[:, i, k:k + 1], axis=0),
                                in_=hb_all.t[:, i, :], in_offset=None),
                                reads=[slot.b, hb_all.b, hsz], sembuf=scb)
                    if dbg is not None and dbg.get("moe_dump"):
                        S.dma("sp", dbg_out[:, 0:64], slot.t[:].rearrange("p i k -> p (i k)").bitcast(F32), reads=[slot.b], sembuf=xts[0].b)
                        S.dma("sp", dbg_out[:, 64:128], gates.t[:].rearrange("p i k -> p (i k)"), reads=[gates.b], sembuf=xts[0].b)
                        S.dma("sp", dbg_out[:, 128:128 + NSEG * 7], idxw.t[:].bitcast(F32), reads=[idxw.b], sembuf=xts[0].b)
                    S.end_phase()
                conv_step(10 ** 6)
                es = ExitStack()
                with es:
                    hsl = allocn(es, 4, "hsl", [128, D], BF16, dma=True)
                    hT = allocn(es, 2, "shT", [128, 8, 512], BF16)
                    tp = allocn(es, 2, "stp", [128, 8, 128], BF16, psum=True)
                    psG = allocn(es, 2, "spsG", [128, 512], F32, psum=True)
                    psU = allocn(es, 2, "spsU", [128, 512], F32, psum=True)
                    psD = allocn(es, 2, "spsD", [128, 512], F32, psum=True)
                    wg = allocn(es, 2, "swg", [128, 8, 512], BF16, dma="pool")
                    wu = allocn(es, 2, "swu", [128, 8, 512], BF16, dma="pool")
                    wd = allocn(es, 2, "swd", [128, 4, D], BF16, dma="pool")
                    sg = allocn(es, 2, "ssg", [128, 512], F32)
                    aT = allocn(es, 2, "saT", [128, 4, 512], BF16)
                    acc = allocn(es, 2, "sacc", [128, 4, D], F32, dma=True)
                    cnt = dict(x=0, w=0, gu=0, d=0, a=0)

                    def prep_load(seg):
                        for i in range(4):
                            hl = hsl[i]
                            S.dma("sp", hl.t[:], hs[seg * 512 + i * 128:seg * 512 + (i + 1) * 128, :], writes=[hl.b], sembuf=hl.b)

                    def prep_trans(seg):
                        h = hT[seg % 2]
                        for i in range(4):
                            hl = hsl[i]
                            t = tp[i % 2]
                            for c in range(8):
                                S.op("pe", lambda e: e.transpose(out=t.t[:, c, :], in_=hl.t[:, c * 128:(c + 1) * 128],
                                                                 identity=ident.t[:]),
                                     reads=[hl.b, ident.b], writes=[t.b])
                            S.op("act", lambda e: e.activation(out=h.t[:, :, i * 128:(i + 1) * 128], in_=t.t[:], func=AF.Copy),
                                 reads=[t.b], writes=[h.b])

                    def wload(seg, c):
                        k = cnt["w"] % 2
                        cnt["w"] += 1
                        ia = idxw.t[:, seg * 7 + c:seg * 7 + c + 1]
                        for (dst, src) in ((wg[k], WGd), (wu[k], WUd), (wd[k], WDd)):
                            S.dma_fn("pool", lambda e: e.indirect_dma_start(
                                out=dst.t[:].rearrange("p a b -> p (a b)"), out_offset=None, in_=src,
                                in_offset=bass.IndirectOffsetOnAxis(ap=ia, axis=0)),
                                reads=[idxw.b, convb], writes=[dst.b], sembuf=dst.b)
                        return k

                    def gu(seg, k, a, j):
                        h = hT[seg % 2]
                        kk = cnt["gu"] % 2
                        cnt["gu"] += 1
                        pg, pu, sgt = psG[kk], psU[kk], sg[kk]
                        for ch in range(8):
                            S.op("pe", lambda e: e.matmul(pg.t[:], lhsT=wg[k].t[:, ch, j * 128:(j + 1) * 128],
                                                          rhs=h.t[:, ch, :], start=(ch == 0), stop=(ch == 7)),
                                 reads=[wg[k].b, h.b], writes=[pg.b])
                        for ch in range(8):
                            S.op("pe", lambda e: e.matmul(pu.t[:], lhsT=wu[k].t[:, ch, j * 128:(j + 1) * 128],
                                                          rhs=h.t[:, ch, :], start=(ch == 0), stop=(ch == 7)),
                                 reads=[wu[k].b, h.b], writes=[pu.b])
                        S.op("act", lambda e: e.activation(out=sgt.t[:], in_=pg.t[:], func=AF.Silu),
                             reads=[pg.b], writes=[sgt.b])
                        S.op("dve", lambda e: e.tensor_tensor(out=a.t[:, j, :], in0=pu.t[:], in1=sgt.t[:], op=ALU.mult),
                             reads=[pu.b, sgt.b], writes=[a.b])

                    def down(seg, c, k, a):
                        ac = acc[seg % 2]
                        for i in range(4):
                            for dh in range(2):
                                pd = psD[cnt["d"] % 2]
                                cnt["d"] += 1
                                for j in range(4):
                                    S.op("pe", lambda e: e.matmul(pd.t[:], lhsT=a.t[:, j, i * 128:(i + 1) * 128],
                                                                  rhs=wd[k].t[:, j, dh * 512:(dh + 1) * 512],
                                                                  start=(j == 0), stop=(j == 3)),
                                         reads=[a.b, wd[k].b], writes=[pd.b])
                                av = ac.t[:, i, dh * 512:(dh + 1) * 512]
                                if c == 0:
                                    S.op("dve", lambda e: e.tensor_copy(out=av, in_=pd.t[:]), reads=[pd.b], writes=[ac.b])
                                else:
                                    S.op("dve", lambda e: e.tensor_tensor(out=av, in0=pd.t[:], in1=av, op=ALU.add),
                                         reads=[pd.b, ac.b], writes=[ac.b])
                        if c == 6:
                            S.dma("sp", ys[seg * 512:(seg + 1) * 512, :].rearrange("(i p) d -> p i d", p=128), ac.t[:],
                                  reads=[ac.b], sembuf=ac.b)

                    prep_load(0)
                    prep_trans(0)
                    pend_d = None
                    for seg in range(NSEG):
                        for c in range(7):
                            k = wload(seg, c)
                            a = aT[cnt["a"] % 2]
                            cnt["a"] += 1
                            gu(seg, k, a, 0)
                            if pend_d is not None:
                                down(*pend_d)
                            for j in range(1, 4):
                                gu(seg, k, a, j)
                            pend_d = (seg, c, k, a)
                            if seg + 1 < NSEG:
                                if c == 1:
                                    prep_load(seg + 1)
                                if c == 4:
                                    prep_trans(seg + 1)
                    down(*pend_d)
                    S.end_phase()
                es = ExitStack()
                with es:
                    ggt_x = load_bc(es, "cggt_x", 5)
                    NBUF = 4
                    r1 = allocn(es, NBUF, "r1", [128, D], F32, dma="pool")
                    r2 = allocn(es, NBUF, "r2", [128, D], F32, dma="pool")
                    xts = allocn(es, NBUF, "cxt", [128, D], F32, dma=True)
                    a1 = allocn(es, 2, "a1", [128, D], F32)
                    junk = alloc(es, "cjunk", [128, D], BF16)
                    sss = allocn(es, 2, "css", [128, 1], F32)
                    rstds = allocn(es, 2, "crstd", [128, 1], F32)
                    tmp = allocn(es, 2, "ctmp", [128, D], F32)

                    def c_issue(i):
                        kb = i % NBUF
                        for (r, k) in ((r1[kb], 0), (r2[kb], 1)):
                            S.dma_fn("pool", lambda e: e.indirect_dma_start(
                                out=r.t[:], out_offset=None, in_=ys,
                                in_offset=bass.IndirectOffsetOnAxis(ap=slot.t[:, i, k:k + 1], axis=0)),
                                reads=[slot.b], writes=[r.b], sembuf=r.b)
                        xt = xts[kb]
                        S.dma("sp", xt.t[:], y_out[i * 128:(i + 1) * 128, :], writes=[xt.b], sembuf=xt.b)

                    for i in range(NBUF - 1):
                        c_issue(i)
                    for i in range(32):
                        if i + NBUF - 1 < 32:
                            c_issue(i + NBUF - 1)
                        k2 = i % 2
                        kb = i % NBUF
                        xt = xts[kb]
                        S.op("act", lambda e: e.activation(out=a1[k2].t[:], in_=r1[kb].t[:], func=AF.Copy,
                                                           scale=gates.t[:, i, 0:1]),
                             reads=[r1[kb].b, gates.b], writes=[a1[k2].b])
                        S.op("dve", lambda e: e.scalar_tensor_tensor(out=a1[k2].t[:], in0=r2[kb].t[:], scalar=gates.t[:, i, 1:2],
                                                                     in1=a1[k2].t[:], op0=ALU.mult, op1=ALU.add),
                             reads=[r2[kb].b, gates.b, a1[k2].b], writes=[a1[k2].b])
                        S.op("act", lambda e: e.activation(out=junk.t[:], in_=a1[k2].t[:], func=AF.Square,
                                                           accum_out=sss[k2].t[:]),
                             reads=[a1[k2].b], writes=[junk.b, sss[k2].b])
                        rstd_from_ss(sss[k2], rstds[k2], 1.0 / D)
                        S.op("dve", lambda e: e.scalar_tensor_tensor(out=tmp[k2].t[:], in0=a1[k2].t[:],
                                                                     scalar=rstds[k2].t[:, 0:1], in1=ggt_x.t[:],
                                                                     op0=ALU.mult, op1=ALU.mult),
                             reads=[a1[k2].b, rstds[k2].b, ggt_x.b], writes=[tmp[k2].b])
                        S.op("pool", lambda e: e.tensor_tensor(out=xt.t[:], in0=tmp[k2].t[:], in1=xt.t[:], op=ALU.add),
                             reads=[tmp[k2].b, xt.b], writes=[xt.b])
                        S.dma("sp", y_out[i * 128:(i + 1) * 128, :], xt.t[:], reads=[xt.b], sembuf=xt.b)
                    S.end_phase()

        stop_after = dbg.get("stop") if dbg else None
        done = False
        for l in range(DEPTH):
            last = (l == DEPTH - 1)
            for name, fn in (("ada", lambda: phase_ada(l)), ("a", lambda: phase_a(l, last)),
                             ("b", lambda: phase_b(l, last)),
                             ("f", (lambda: phase_moe(l)) if (l % 2 == 1 and last) else (lambda: phase_f(l, last)))):
                fn()
                if stop_after == (l, name):
                    done = True
                    break
            if done:
                break
        if dbg is not None:
            es = ExitStack()
            with es:
                srcd = dict(mods=mods, QTd=QTd, KTd=KTd, Vd=Vd, cnTd=cnTd, QcTd=QcTd, KcTd=KcTd, Vcd=Vcd,
                            cncTd=cncTd, ctxs=ctxs)[dbg["src"]]
                b = S.buf(dma=True)
                S.dma("sp", dbg_out, srcd, sembuf=b)
                S.end_phase()
    return nc


def _host_inputs(inputs):
    f = lambda a: np.ascontiguousarray(np.asarray(a, dtype=np.float32))
    x = f(inputs["x"]); c = f(inputs["c"]); ctx = f(inputs["ctx"]); c_ctx = f(inputs["c_ctx"])
    rpb = f(inputs["rpb"])
    conv_w = f(inputs["conv_w"])
    gout = f(inputs["out_norm_g"])
    q = np.arange(128)
    qr_off = q // 64
    qc = q % 64
    cs = np.clip(qc - 8, 0, 48)

    def table(p, bs, nrows):
        qr = 2 * p + qr_off
        rs = np.clip(qr - 4, 0, 56)
        kk = np.arange(nrows * 64)
        kr = bs + kk // 64
        kc = kk % 64
        valid = ((kr[None, :] >= rs[:, None]) & (kr[None, :] < rs[:, None] + 8) &
                 (kc[None, :] >= cs[:, None]) & (kc[None, :] < cs[:, None] + 16))
        dy = np.clip(kr[None, :] - qr[:, None] + 7, 0, 14)
        dx = np.clip(kc[None, :] - qc[:, None] + 15, 0, 30)
        g = rpb[:, :, dy, dx]
        return np.where(valid[None, None], g, np.float32(NEG)).astype(np.float32)

    mid = table(10, 16, 9)
    nab_mid = np.ascontiguousarray(mid.transpose(0, 2, 1, 3))
    edges = [table(0, 0, 8), table(1, 0, 8), table(30, 56, 8), table(31, 56, 8)]
    nab_edge = np.ascontiguousarray(np.stack(edges, axis=1))
    convw = np.ascontiguousarray(conv_w.reshape(DEPTH, 3, 4, 128).transpose(0, 3, 2, 1).reshape(DEPTH, 128, 12))
    goutc = np.ascontiguousarray(gout[:, 512:].reshape(DEPTH, 4, 128).transpose(0, 2, 1))
    w_rt = np.ascontiguousarray(f(inputs["w_router"])[0].reshape(8, 128, NE).transpose(1, 0, 2))
    shared = {
        "w_ada": f(inputs["w_ada"]), "b_ada": f(inputs["b_ada"]),
        "norm_g": f(inputs["norm_g"]).reshape(DEPTH * 4, D),
        "w_in": f(inputs["w_in"]), "nab_mid": nab_mid, "nab_edge": nab_edge, "convw": convw,
        "gout": gout, "goutc": goutc, "w_out": f(inputs["w_out"]),
        "w_gu_dense": f(inputs["w_gu_dense"]), "w_down_dense": f(inputs["w_down_dense"]),
        "w_router": w_rt, "w_gu_moe": f(inputs["w_gu_moe"])[0], "w_down_moe": f(inputs["w_down_moe"])[0],
    }
    maps = []
    for b in range(x.shape[0]):
        cv = np.concatenate([c[b].reshape(8, 128).T, c_ctx.reshape(8, 128).T], axis=1)
        m = dict(shared)
        m["x"] = x[b]
        m["ctx"] = ctx[b]
        m["cvec"] = np.ascontiguousarray(cv)
        maps.append(m)
    return maps


def kernel(**inputs):
    maps = _host_inputs(inputs)
    nc = build_nc()
    res = run_bass_kernel_spmd(nc, maps, core_ids=list(range(len(maps))))
    return np.stack([np.asarray(r["y"], dtype=np.float32) for r in res.results], axis=0)
```
